# Optimizing a Trainium2 kernel written in Bass

```python
import math
import jax, jax.numpy as jnp
from jax import lax
import numpy as np

D_MODEL = 2048
BATCH = 1
SEQ = 8192
DEPTH = 1

N_HEADS = 16
HEAD_DIM = 128
ATT_WIDTH = N_HEADS * HEAD_DIM
MOBA_BLOCK = 256
MOBA_TOPK = 3
QUERY_CHUNK = 32
LRU_WIDTH = D_MODEL
LRU_BLOCKS = 16
LRU_BLOCK_W = LRU_WIDTH // LRU_BLOCKS
CONV_WIDTH = 4
LRU_C = 8.0
D_FF = int(math.ceil(8 * D_MODEL / 3 / 256)) * 256
IN_COLS = 3 * ATT_WIDTH + 2 * LRU_WIDTH + 2 * D_MODEL
SPLIT_POINTS = (ATT_WIDTH, 2 * ATT_WIDTH, 3 * ATT_WIDTH,
                3 * ATT_WIDTH + LRU_WIDTH, 3 * ATT_WIDTH + 2 * LRU_WIDTH,
                3 * ATT_WIDTH + 2 * LRU_WIDTH + D_MODEL)
EPS = 1e-6
NEG_INF = -1e30

kernel_name = "moba_rglru_gated_hybrid"


def rms_norm(x, w):
    xf = x.astype(jnp.float32)
    y = xf * lax.rsqrt(jnp.mean(xf * xf, axis=-1, keepdims=True) + EPS)
    return (y * w.astype(jnp.float32)).astype(x.dtype)


def alibi_slopes(n_heads):
    h = jnp.arange(1, n_heads + 1, dtype=jnp.float32)
    return jnp.exp2(-8.0 * h / n_heads)


def moba_attention(q, k, v):
    B, S, H, Dh = q.shape
    n_blk = -(-S // MOBA_BLOCK)
    s_pad = n_blk * MOBA_BLOCK
    topk = min(MOBA_TOPK, n_blk)
    pad = ((0, 0), (0, s_pad - S), (0, 0), (0, 0))
    kb = jnp.pad(k, pad).reshape(B, n_blk, MOBA_BLOCK, H, Dh).transpose(0, 3, 1, 2, 4)
    vb = jnp.pad(v, pad).reshape(B, n_blk, MOBA_BLOCK, H, Dh).transpose(0, 3, 1, 2, 4)
    k_mean = jnp.mean(kb.astype(jnp.float32), axis=3).astype(q.dtype)
    slopes = alibi_slopes(H)
    scale = HEAD_DIM ** -0.5
    b_idx = jnp.arange(B)[:, None, None, None]
    h_idx = jnp.arange(H)[None, None, :, None]
    blk_offsets = jnp.arange(MOBA_BLOCK)
    n_chunks = S // QUERY_CHUNK

    def one_chunk(c):
        t0 = c * QUERY_CHUNK
        blk = t0 // MOBA_BLOCK
        qc = lax.dynamic_slice_in_dim(q, t0, QUERY_CHUNK, axis=1)
        pos_q = t0 + jnp.arange(QUERY_CHUNK)
        gate = jnp.einsum('bqhd,bhnd->bqhn', qc, k_mean).astype(jnp.float32)
        gate = jnp.where(jnp.arange(n_blk) < blk, gate, NEG_INF)
        _, sel = lax.top_k(gate, topk)
        valid = jnp.arange(topk) < blk
        k_sel = kb[b_idx, h_idx, sel]
        v_sel = vb[b_idx, h_idx, sel]
        s_sel = jnp.einsum('bqhd,bqhjsd->bqhjs', qc, k_sel).astype(jnp.float32) * scale
        key_pos = sel[..., None] * MOBA_BLOCK + blk_offsets
        dist_sel = (pos_q[None, :, None, None, None] - key_pos).astype(jnp.float32)
        s_sel = s_sel - slopes[None, None, :, None, None] * dist_sel
        s_sel = jnp.where(valid[:, None], s_sel, NEG_INF)
        k_own = lax.dynamic_index_in_dim(kb, blk, axis=2, keepdims=False)
        v_own = lax.dynamic_index_in_dim(vb, blk, axis=2, keepdims=False)
        s_own = jnp.einsum('bqhd,bhsd->bqhs', qc, k_own).astype(jnp.float32) * scale
        dist_own = pos_q[:, None] - (blk * MOBA_BLOCK + blk_offsets)[None, :]
        s_own = jnp.where(dist_own[None, :, None, :] >= 0,
                          s_own - slopes[None, None, :, None] * dist_own.astype(jnp.float32)[None, :, None, :],
                          NEG_INF)
        s_all = jnp.concatenate([s_sel.reshape(B, QUERY_CHUNK, H, topk * MOBA_BLOCK), s_own], axis=-1)
        p = jax.nn.softmax(s_all, axis=-1).astype(v.dtype)
        p_sel = p[..., :topk * MOBA_BLOCK].reshape(B, QUERY_CHUNK, H, topk, MOBA_BLOCK)
        p_own = p[..., topk * MOBA_BLOCK:]
        return (jnp.einsum('bqhjs,bqhjsd->bqhd', p_sel, v_sel)
                + jnp.einsum('bqhs,bhsd->bqhd', p_own, v_own))

    out = lax.map(one_chunk, jnp.arange(n_chunks))
    return out.transpose(1, 0, 2, 3, 4).reshape(B, S, H * Dh)


def _lru_combine(left, right):
    a_l, b_l = left
    a_r, b_r = right
    return a_l * a_r, a_r * b_l + b_r


def rglru_branch(xr, yr, conv_w, conv_b, w_rg_a, b_rg_a, w_rg_x, b_rg_x, lru_lambda):
    B, S, W = xr.shape
    xp = jnp.pad(xr, ((0, 0), (CONV_WIDTH - 1, 0), (0, 0)))
    u = conv_b
    for tap in range(CONV_WIDTH):
        u = u + xp[:, tap:tap + S] * conv_w[tap]
    ub = u.reshape(B, S, LRU_BLOCKS, LRU_BLOCK_W)
    r = jax.nn.sigmoid((jnp.einsum('bsnc,ncd->bsnd', ub, w_rg_a).reshape(B, S, W) + b_rg_a).astype(jnp.float32))
    i = jax.nn.sigmoid((jnp.einsum('bsnc,ncd->bsnd', ub, w_rg_x).reshape(B, S, W) + b_rg_x).astype(jnp.float32))
    log_a = -LRU_C * r * jax.nn.softplus(-lru_lambda.astype(jnp.float32))
    a = jnp.exp(log_a)
    mult = jnp.sqrt(-jnp.expm1(2.0 * log_a))
    bx = mult * i * u.astype(jnp.float32)
    _, h = lax.associative_scan(_lru_combine, (a, bx), axis=1)
    return h.astype(xr.dtype) * jax.nn.gelu(yr)


def hybrid_layer(x, norm1_w, w_in, q_norm_w, k_norm_w, conv_w, conv_b, w_rg_a, b_rg_a,
                 w_rg_x, b_rg_x, lru_lambda, w_proj_attn, w_proj_lru, w_out,
                 norm2_w, w_ffn_gate, w_ffn_up, w_ffn_down):
    B, S, _ = x.shape
    h = rms_norm(x, norm1_w)
    proj = h @ w_in
    q, k, v, xr, yr, g_att, g_lru = jnp.split(proj, SPLIT_POINTS, axis=-1)
    q = rms_norm(q.reshape(B, S, N_HEADS, HEAD_DIM), q_norm_w)
    k = rms_norm(k.reshape(B, S, N_HEADS, HEAD_DIM), k_norm_w)
    v = v.reshape(B, S, N_HEADS, HEAD_DIM)
    att = moba_attention(q, k, v)
    lru = rglru_branch(xr, yr, conv_w, conv_b, w_rg_a, b_rg_a, w_rg_x, b_rg_x, lru_lambda)
    merged = (jax.nn.sigmoid(g_att) * (att @ w_proj_attn)
              + jax.nn.sigmoid(g_lru) * (lru @ w_proj_lru))
    x = x + merged @ w_out
    h2 = rms_norm(x, norm2_w)
    x = x + (jax.nn.silu(h2 @ w_ffn_gate) * (h2 @ w_ffn_up)) @ w_ffn_down
    return x


def setup_inputs(seed: int = 0) -> dict:
    key = jax.random.key(seed)
    ks = jax.random.split(key, 20)
    L = DEPTH
    nrm = lambda k, shape, fan_in: jax.random.normal(k, shape, jnp.float32) * fan_in ** -0.5
    u = jax.random.uniform(ks[11], (L, LRU_WIDTH), jnp.float32, minval=0.9, maxval=0.999)
    a0 = u ** (1.0 / LRU_C)
    lam = jnp.log(a0) - jnp.log1p(-a0)
    return {
        "x": jax.random.normal(ks[0], (BATCH, SEQ, D_MODEL), jnp.float32),
        "norm1_w": 1.0 + 0.05 * jax.random.normal(ks[1], (L, D_MODEL), jnp.float32),
        "w_in": nrm(ks[2], (L, D_MODEL, IN_COLS), D_MODEL),
        "q_norm_w": 1.0 + 0.05 * jax.random.normal(ks[3], (L, HEAD_DIM), jnp.float32),
        "k_norm_w": 1.0 + 0.05 * jax.random.normal(ks[4], (L, HEAD_DIM), jnp.float32),
        "conv_w": nrm(ks[5], (L, CONV_WIDTH, LRU_WIDTH), CONV_WIDTH),
        "conv_b": 0.01 * jax.random.normal(ks[6], (L, LRU_WIDTH), jnp.float32),
        "w_rg_a": nrm(ks[7], (L, LRU_BLOCKS, LRU_BLOCK_W, LRU_BLOCK_W), LRU_BLOCK_W),
        "b_rg_a": 0.01 * jax.random.normal(ks[8], (L, LRU_WIDTH), jnp.float32),
        "w_rg_x": nrm(ks[9], (L, LRU_BLOCKS, LRU_BLOCK_W, LRU_BLOCK_W), LRU_BLOCK_W),
        "b_rg_x": 0.01 * jax.random.normal(ks[10], (L, LRU_WIDTH), jnp.float32),
        "lru_lambda": lam,
        "w_proj_attn": nrm(ks[12], (L, ATT_WIDTH, D_MODEL), ATT_WIDTH),
        "w_proj_lru": nrm(ks[13], (L, LRU_WIDTH, D_MODEL), LRU_WIDTH),
        "w_out": nrm(ks[14], (L, D_MODEL, D_MODEL), D_MODEL),
        "norm2_w": 1.0 + 0.05 * jax.random.normal(ks[15], (L, D_MODEL), jnp.float32),
        "w_ffn_gate": nrm(ks[16], (L, D_MODEL, D_FF), D_MODEL),
        "w_ffn_up": nrm(ks[17], (L, D_MODEL, D_FF), D_MODEL),
        "w_ffn_down": nrm(ks[18], (L, D_FF, D_MODEL), D_FF),
    }


def reference(x, norm1_w, w_in, q_norm_w, k_norm_w, conv_w, conv_b, w_rg_a, b_rg_a,
              w_rg_x, b_rg_x, lru_lambda, w_proj_attn, w_proj_lru, w_out,
              norm2_w, w_ffn_gate, w_ffn_up, w_ffn_down):
    for layer in range(DEPTH):
        x = hybrid_layer(x, norm1_w[layer], w_in[layer], q_norm_w[layer], k_norm_w[layer],
                         conv_w[layer], conv_b[layer], w_rg_a[layer], b_rg_a[layer],
                         w_rg_x[layer], b_rg_x[layer], lru_lambda[layer],
                         w_proj_attn[layer], w_proj_lru[layer], w_out[layer],
                         norm2_w[layer], w_ffn_gate[layer], w_ffn_up[layer], w_ffn_down[layer])
    return x
```

```python
import numpy as np
import concourse.bass as bass
import concourse.mybir as mybir
from concourse.bass_utils import run_bass_kernel_spmd

F32 = mybir.dt.float32
BF16 = mybir.dt.bfloat16
AF = mybir.ActivationFunctionType
ALU = mybir.AluOpType
AX = mybir.AxisListType


class Buf:
    __slots__ = ("name", "last_w", "readers")

    def __init__(self, name):
        self.name = name
        self.last_w = None
        self.readers = []


class Op:
    __slots__ = ("eng", "fn", "deps", "signal", "sem", "val", "inc", "idx", "extra")

    def __init__(self, eng, fn, idx):
        self.eng = eng
        self.fn = fn
        self.deps = []
        self.signal = False
        self.sem = None
        self.val = None
        self.inc = 1
        self.idx = idx
        self.extra = ()


class Prog:
    ENGS = ("pe", "act", "dve", "pool", "sp")

    def __init__(self, nc, same_engine_sync=True):
        self.nc = nc
        self.ops = []
        self.same_engine_sync = same_engine_sync
        self.dma_sem_count = {}

    def op(self, eng, fn, reads=(), writes=(), dma_sem=None, extra=()):
        o = Op(eng, fn, len(self.ops))
        o.extra = tuple(extra)
        deps = {}
        for b in reads:
            if b.last_w is not None:
                deps[b.last_w.idx] = b.last_w
        for b in writes:
            if b.last_w is not None:
                deps[b.last_w.idx] = b.last_w
            for r in b.readers:
                deps[r.idx] = r
        is_dma = dma_sem is not None
        for d in deps.values():
            d_is_dma = d.sem is not None
            if d.eng == eng and not d_is_dma and not is_dma:
                if eng == "pe" or not self.same_engine_sync:
                    continue
            if d is o:
                continue
            o.deps.append(d)
            d.signal = True
        if is_dma:
            o.sem = dma_sem
            o.inc = 16
            o.signal = True
            c = self.dma_sem_count.get(id(dma_sem), 0) + 16
            self.dma_sem_count[id(dma_sem)] = c
            o.val = c
        for b in reads:
            b.readers.append(o)
        for b in writes:
            b.last_w = o
            b.readers = []
        self.ops.append(o)
        return o

    def emit(self, block, eng_sems, final_sems=(), counters=None):
        nc = self.nc
        if counters is None:
            counters = {e: 0 for e in self.ENGS}
        for o in self.ops:
            if o.sem is None:
                if o.signal:
                    counters[o.eng] += 1
                    o.sem = eng_sems[o.eng]
                    o.val = counters[o.eng]
        by_eng = {e: [o for o in self.ops if o.eng == e] for e in self.ENGS}

        def run(engine, ops, is_last_owner):
            seen = {}
            for o in ops:
                need = {}
                for d in o.deps:
                    k = id(d.sem)
                    if k not in need or need[k][1] < d.val:
                        need[k] = (d.sem, d.val)
                for (xs, xv) in o.extra:
                    k = id(xs)
                    if k not in need or need[k][1] < xv:
                        need[k] = (xs, xv)
                for k, (s, v) in need.items():
                    if seen.get(k, 0) >= v:
                        continue
                    engine.wait_ge(s, v)
                    seen[k] = v
                ins = o.fn(engine)
                if o.signal:
                    ins.then_inc(o.sem, o.inc)
            if is_last_owner:
                for s in final_sems:
                    engine.wait_ge(s, self.dma_sem_count[id(s)])

        if by_eng["pe"]:
            @block.tensor
            def _(e):
                run(e, by_eng["pe"], False)
        if by_eng["act"]:
            @block.scalar
            def _(e):
                run(e, by_eng["act"], False)
        if by_eng["dve"]:
            @block.vector
            def _(e):
                run(e, by_eng["dve"], False)
        if by_eng["pool"]:
            @block.gpsimd
            def _(e):
                run(e, by_eng["pool"], False)

        @block.sync
        def _(e):
            run(e, by_eng["sp"], True)


S = 8192
D = 2048
H = 16
DH = 128
BLK = 256
NBLK = S // BLK
DFF = 5632
NFF = DFF // 128
KD = D // 128
TT = 512
NTILE = S // TT
NC = 8
TB = S // NC
EPS = 1e-6
SCALE = DH ** -0.5
FG = 4
FGC = NFF // FG
NEG = -1.0e30

V_N1 = 0
V_CW = 16
V_CB = 24
V_BA = 26
V_BX = 28
V_LAM = 30
V_QW = 32
V_KW = 33
V_N2 = 34
V_HI = 50
NV = 52
C_D = 0
C_KB = 64
C_DD = 66
C_DF = 194
NCT = 322


class Ctx:
    pass


def _ring(n):
    return [Buf(f"r{i}") for i in range(n)]


def build_program(debug=False):
    nc = bass.Bass("TRN2", target_bir_lowering=False)
    dt_i32 = mybir.dt.int32

    def din(name, shape, dt=F32):
        return nc.dram_tensor(name, list(shape), dt, kind="ExternalInput").ap()

    def dout(name, shape, dt=F32):
        return nc.dram_tensor(name, list(shape), dt, kind="ExternalOutput").ap()

    xT = din("xT", [D, S])
    xTs = din("xTs", [D, TB])
    w_a = din("w_a", [128, KD, 1280])
    vecs_d = din("vecs", [128, NV])
    ctab_d = din("ctab", [128, NCT])
    wrg_d = din("w_rg", [128, 2, 2, 128])
    cidx = din("cidx", [1, 1], dt_i32)
    w_g = din("w_g", [32, 128, KD * 128])
    w_pa = din("w_pa", [16, 128, KD * 128])
    w_pl = din("w_pl", [16, 128, KD * 128])
    w_o = din("w_o", [16, 128, KD * 128])
    w_fg = din("w_fg", [NFF, 128, KD * 128])
    w_fu = din("w_fu", [NFF, 128, KD * 128])
    w_fd = din("w_fd", [FG * 16, 128, FGC * 128])
    outT = dout("outT", [D, TB])

    ag_lru_in = nc.dram_tensor("ag_lru_in", [NC, 256, TB], BF16).ap()
    ag_att_in = nc.dram_tensor("ag_att_in", [NC, 256, TB], BF16).ap()
    ag_lru_out = nc.dram_tensor("ag_lru_out", [NC * NC, 256, TB], BF16).ap()
    ag_att_out = nc.dram_tensor("ag_att_out", [NC * NC, 256, TB], BF16).ap()

    dbg = {}
    if debug:
        dbg["lru"] = dout("dbg_lru", [NC, 256, TB], BF16)
        dbg["att"] = dout("dbg_att", [NC, 256, TB], BF16)
        dbg["kt"] = dout("dbg_kt", [128, S], BF16)
        dbg["qt"] = dout("dbg_qt", [128, S], BF16)
        dbg["v"] = dout("dbg_v", [128, 64, 130], BF16)
        dbg["sel"] = dout("dbg_sel", [128, 64, 32], BF16)
        dbg["merged"] = dout("dbg_merged", [128, KD, TB], BF16)
        dbg["x1"] = dout("dbg_x1", [128, KD, TB], F32)

    sem_state = {}
    eng_counts = {e: 0 for e in Prog.ENGS}

    import contextlib
    es = contextlib.ExitStack()
    with es:
        eng_sems = {e: es.enter_context(nc.semaphore(f"s_{e}")) for e in Prog.ENGS}
        NDS = 40
        dsems = [es.enter_context(nc.semaphore(f"d{i}")) for i in range(NDS)]
        cc_sem = es.enter_context(nc.semaphore("cc"))
        creg = es.enter_context(nc.sync.register("creg"))
        ps = es.enter_context(nc.psum_tensor("ps", [128, 8, 512], F32))
        psb = [Buf(f"ps{i}") for i in range(8)]

        vecs = es.enter_context(nc.sbuf_tensor("vecs_sb", [128, NV], F32))
        ctab = es.enter_context(nc.sbuf_tensor("ctab_sb", [128, NCT], F32))
        ident = es.enter_context(nc.sbuf_tensor("ident", [128, 128], BF16))
        ones = es.enter_context(nc.sbuf_tensor("ones", [128, 128], BF16))
        sm = es.enter_context(nc.sbuf_tensor("sm", [128, 32], F32))
        SM_SL, SM_NSL, SM_SP, SM_SP2, SM_NBA, SM_NBX, SM_KB = 0, 2, 4, 6, 8, 10, 12

        class Phase:
            def __init__(self, name):
                self.name = name
                self.P = Prog(nc)
                self.P.dma_sem_count = sem_state
                self.used = []
                self.next_ds = 0
                for b_ in psb:
                    b_.last_w, b_.readers = None, []

            def dsem(self):
                s = dsems[self.next_ds]
                self.next_ds += 1
                self.used.append(s)
                return s

            def finish(self):
                with nc.Block() as block:
                    self.P.emit(block, eng_sems, final_sems=[s for s in self.used if id(s) in sem_state],
                                counters=eng_counts)

        def sc(col, n=1):
            return sm[:, col:col + n]

        def vc(col, n=1):
            return vecs[:, col:col + n]

        ph = Phase("setup")
        P = ph.P
        b_vecs, b_ctab, b_sm, b_id, b_ones = Buf("vecs"), Buf("ctab"), Buf("sm"), Buf("id"), Buf("ones")
        P.op("sp", lambda e: e.dma_start(out=vecs[:], in_=vecs_d), writes=[b_vecs], dma_sem=ph.dsem())
        P.op("sp", lambda e: e.dma_start(out=ctab[:], in_=ctab_d), writes=[b_ctab], dma_sem=ph.dsem())
        P.op("sp", lambda e: e.reg_load(creg, cidx[0:1, 0:1]))
        P.op("dve", lambda e: e.memset(ident[:], 0.0), writes=[b_id])
        P.op("pool", lambda e: e.affine_select(out=ident[:], in_=ident[:], pattern=[[-1, 128]], compare_op=ALU.not_equal,
                                                fill=1.0, base=0, channel_multiplier=1), reads=[b_id], writes=[b_id])
        P.op("dve", lambda e: e.memset(ones[:], 1.0), writes=[b_ones])
        P.op("act", lambda e: e.activation(out=sc(SM_SL, 2), in_=vc(V_HI, 2), func=AF.Exp, scale=-0.5 * float(np.log(2.0))),
             reads=[b_vecs], writes=[b_sm])
        P.op("dve", lambda e: e.tensor_scalar(out=sc(SM_NSL, 2), in0=sc(SM_SL, 2), scalar1=-1.0, scalar2=None, op0=ALU.mult),
             reads=[b_sm], writes=[b_sm])
        P.op("act", lambda e: e.activation(out=sc(SM_SP, 2), in_=vc(V_LAM, 2), func=AF.Exp, scale=-1.0), reads=[b_vecs, b_sm], writes=[b_sm])
        P.op("act", lambda e: e.activation(out=sc(SM_SP, 2), in_=sc(SM_SP, 2), func=AF.Ln, bias=1.0, scale=1.0), reads=[b_sm], writes=[b_sm])
        P.op("dve", lambda e: e.tensor_scalar(out=sc(SM_SP2, 2), in0=sc(SM_SP, 2), scalar1=-16.0, scalar2=None, op0=ALU.mult), reads=[b_sm], writes=[b_sm])
        P.op("dve", lambda e: e.tensor_scalar(out=sc(SM_SP, 2), in0=sc(SM_SP, 2), scalar1=-8.0, scalar2=None, op0=ALU.mult), reads=[b_sm], writes=[b_sm])
        P.op("dve", lambda e: e.tensor_scalar(out=sc(SM_NBA, 2), in0=vc(V_BA, 2), scalar1=-1.0, scalar2=None, op0=ALU.mult), reads=[b_vecs, b_sm], writes=[b_sm])
        P.op("dve", lambda e: e.tensor_scalar(out=sc(SM_NBX, 2), in0=vc(V_BX, 2), scalar1=-1.0, scalar2=None, op0=ALU.mult), reads=[b_vecs, b_sm], writes=[b_sm])
        for hl in range(2):
            P.op("dve", lambda e, hl=hl: e.tensor_scalar(out=sc(SM_KB + 2 * hl, 2), in0=ctab[:, C_KB:C_KB + 2], scalar1=sc(SM_SL + hl),
                                                          scalar2=None, op0=ALU.mult), reads=[b_ctab, b_sm], writes=[b_sm])
        ph.finish()

        def phase_A(which, A=None):
            ph = Phase("A_" + which)
            P = ph.P
            c0, ncol = (0, 512) if which == "lru" else (512, 768)
            nm = ncol // 128
            with contextlib.ExitStack() as st:
                T = lambda name, shape, dt: st.enter_context(nc.sbuf_tensor(which + "_" + name, shape, dt))
                wa = T("wa", [128, KD, ncol], BF16)
                NXF = 6 if which == "lru" else 4
                NWS = 2 if which == "lru" else 1
                wst = T("wst", [128, NWS, ncol], F32)
                xf = T("xf", [128, NXF, TT], F32)
                sqb = T("sqb", [128, 3, TT], BF16)
                xb = T("xb", [128, 2, KD, TT], BF16)
                rstd = T("rstd", [128, 2, TT], F32)
                lnt = T("lnt", [128, TT], F32)
                b_wa = [Buf(f"wa{k}") for k in range(KD)]
                b_wst = _ring(NWS)
                s_wst = [ph.dsem() for _ in range(NWS)]
                b_xf = _ring(NXF)
                s_xf = [ph.dsem() for _ in range(NXF)]
                b_sqb = _ring(3)
                b_xb = [[Buf(f"xb{p}_{k}") for k in range(KD)] for p in range(2)]
                b_rstd = _ring(2)
                b_lnt = Buf("lnt")
                BV = Buf("vecs_ro")

                for k in range(KD):
                    sl = k % NWS
                    P.op("sp", lambda e, k=k, sl=sl: e.dma_start(out=wst[:, sl, :], in_=w_a[:, k, c0:c0 + ncol]),
                         writes=[b_wst[sl]], dma_sem=s_wst[sl])
                    P.op("dve", lambda e, k=k, sl=sl: e.tensor_scalar(out=wa[:, k, :], in0=wst[:, sl, :], scalar1=vc(V_N1 + k),
                                                                     scalar2=None, op0=ALU.mult),
                         reads=[b_wst[sl]], writes=[b_wa[k]])

                xcnt = [0]
                sqcnt = [0]

                def x_tile(t):
                    par = t % 2
                    for k in range(KD):
                        sl = xcnt[0] % NXF
                        xcnt[0] += 1
                        sq = sqcnt[0] % 3
                        sqcnt[0] += 1
                        P.op("sp", lambda e, k=k, sl=sl: e.dma_start(out=xf[:, sl, :], in_=xT[k * 128:(k + 1) * 128, t * TT:(t + 1) * TT]),
                             writes=[b_xf[sl]], dma_sem=s_xf[sl])
                        P.op("act", lambda e, sl=sl, sq=sq: e.activation(out=sqb[:, sq, :], in_=xf[:, sl, :], func=AF.Square),
                             reads=[b_xf[sl]], writes=[b_sqb[sq]])
                        P.op("dve", lambda e, k=k, sl=sl: e.tensor_copy(out=xb[:, par, k, :], in_=xf[:, sl, :]),
                             reads=[b_xf[sl]], writes=[b_xb[par][k]])
                        P.op("pe", lambda e, k=k, sq=sq: e.matmul(ps[:, 0, :], lhsT=ones[:], rhs=sqb[:, sq, :], start=(k == 0), stop=(k == KD - 1)),
                             reads=[b_sqb[sq]], writes=[psb[0]])
                    P.op("act", lambda e: e.activation(out=lnt[:], in_=ps[:, 0, :], func=AF.Ln, scale=1.0 / D, bias=EPS),
                         reads=[psb[0]], writes=[b_lnt])
                    P.op("act", lambda e: e.activation(out=rstd[:, par, :], in_=lnt[:], func=AF.Exp, scale=-0.5),
                         reads=[b_lnt], writes=[b_rstd[par]])

                pcnt = [0]

                def proj(t, m):
                    par = t % 2
                    bank = 1 + pcnt[0] % 3
                    pcnt[0] += 1
                    for k in range(KD):
                        P.op("pe", lambda e, k=k, bank=bank: e.matmul(ps[:, bank, :], lhsT=wa[:, k, m * 128:(m + 1) * 128], rhs=xb[:, par, k, :],
                                                                       start=(k == 0), stop=(k == KD - 1)),
                             reads=[b_wa[k], b_xb[par][k]], writes=[psb[bank]])
                    return bank

                if which == "lru":
                    lru_body(ph, st, x_tile, proj, rstd, b_rstd)
                else:
                    qkv_body(ph, st, x_tile, proj, rstd, b_rstd, A)
                ph.finish()

        def lru_body(ph, st, x_tile, proj, rstd, b_rstd):
            P = ph.P
            T = lambda name, shape, dt: st.enter_context(nc.sbuf_tensor(name, shape, dt))
            wrgf = T("wrgf", [128, 2, 2, 128], F32)
            wrg = T("wrg", [128, 2, 2, 128], BF16)
            xrt = T("xrt", [128, 2, TT + 3], F32)
            u = T("u", [128, TT], F32)
            ub = T("ub", [128, TT], BF16)
            ta = T("ta", [128, TT], F32)
            tb = T("tb", [128, TT], F32)
            tc_ = T("tc", [128, TT], F32)
            td = T("td", [128, TT], F32)
            hs = T("hs", [128, TT], F32)
            yt = T("yt", [128, TT], F32)
            y2 = T("y2", [128, TT], F32)
            carry = T("carry", [128, 2], F32)
            ost = T("ost", [128, 4, TT], BF16)
            b_wrgf, b_wrg = Buf("wrgf"), Buf("wrg")
            b_xrt = [Buf("xrt0"), Buf("xrt1")]
            b_u, b_ub, b_ta, b_tb, b_tc, b_td, b_hs, b_yt, b_y2 = [Buf(n) for n in "u ub ta tb tc td hs yt y2".split()]
            b_carry = [Buf("c0"), Buf("c1")]
            b_ost = _ring(4)
            s_ost = [ph.dsem() for _ in range(4)]
            BV = Buf("ro")
            P.op("sp", lambda e: e.dma_start(out=wrgf[:], in_=wrg_d), writes=[b_wrgf], dma_sem=ph.dsem())
            P.op("dve", lambda e: e.tensor_copy(out=wrg[:], in_=wrgf[:]), reads=[b_wrgf], writes=[b_wrg])
            for ch in range(2):
                P.op("dve", lambda e, ch=ch: e.memset(xrt[:, ch, :], 0.0), writes=[b_xrt[ch]])
            ocnt = [0]

            def lru_step(t, ch):
                if True:
                    par = t % 2
                    cw = lambda tap: vc(V_CW + ch * 4 + tap)
                    if t > 0:
                        P.op("dve", lambda e, ch=ch: e.tensor_copy(out=xrt[:, ch, 0:3], in_=xrt[:, ch, TT:TT + 3]),
                             reads=[b_xrt[ch]], writes=[b_xrt[ch]])
                    bank = proj(t, ch)
                    P.op("dve", lambda e, ch=ch, bank=bank: e.tensor_tensor(out=xrt[:, ch, 3:TT + 3], in0=ps[:, bank, :], in1=rstd[:, par, :], op=ALU.mult),
                         reads=[psb[bank], b_rstd[par], b_xrt[ch]], writes=[b_xrt[ch]])
                    P.op("dve", lambda e, ch=ch: e.tensor_scalar(out=u[:], in0=xrt[:, ch, 3:TT + 3], scalar1=cw(3), scalar2=vc(V_CB + ch), op0=ALU.mult, op1=ALU.add),
                         reads=[b_xrt[ch]], writes=[b_u])
                    for tap in (2, 1, 0):
                        P.op("dve", lambda e, ch=ch, tap=tap: e.scalar_tensor_tensor(out=u[:], in0=xrt[:, ch, tap:tap + TT], scalar=cw(tap), in1=u[:], op0=ALU.mult, op1=ALU.add),
                             reads=[b_xrt[ch], b_u], writes=[b_u])
                    P.op("act", lambda e: e.activation(out=ub[:], in_=u[:], func=AF.Copy), reads=[b_u], writes=[b_ub])
                    P.op("pe", lambda e, ch=ch: e.matmul(ps[:, 4, :], lhsT=wrg[:, 0, ch, :], rhs=ub[:], start=True, stop=True), reads=[b_wrg, b_ub], writes=[psb[4]])
                    P.op("pe", lambda e, ch=ch: e.matmul(ps[:, 5, :], lhsT=wrg[:, 1, ch, :], rhs=ub[:], start=True, stop=True), reads=[b_wrg, b_ub], writes=[psb[5]])
                    P.op("act", lambda e, ch=ch: e.activation(out=ta[:], in_=ps[:, 4, :], func=AF.Exp, scale=-1.0, bias=sc(SM_NBA + ch)), reads=[psb[4]], writes=[b_ta])
                    P.op("act", lambda e: e.activation(out=ta[:], in_=ta[:], func=AF.Ln, scale=1.0, bias=1.0), reads=[b_ta], writes=[b_ta])
                    P.op("act", lambda e: e.activation(out=ta[:], in_=ta[:], func=AF.Exp, scale=-1.0), reads=[b_ta], writes=[b_ta])
                    P.op("act", lambda e, ch=ch: e.activation(out=tb[:], in_=ta[:], func=AF.Exp, scale=sc(SM_SP + ch)), reads=[b_ta], writes=[b_tb])
                    P.op("act", lambda e, ch=ch: e.activation(out=ta[:], in_=ta[:], func=AF.Exp, scale=sc(SM_SP2 + ch)), reads=[b_ta], writes=[b_ta])
                    P.op("act", lambda e: e.activation(out=ta[:], in_=ta[:], func=AF.Ln, scale=-1.0, bias=1.0), reads=[b_ta], writes=[b_ta])
                    P.op("act", lambda e, ch=ch: e.activation(out=tc_[:], in_=ps[:, 5, :], func=AF.Exp, scale=-1.0, bias=sc(SM_NBX + ch)), reads=[psb[5]], writes=[b_tc])
                    P.op("act", lambda e: e.activation(out=tc_[:], in_=tc_[:], func=AF.Ln, scale=1.0, bias=1.0), reads=[b_tc], writes=[b_tc])
                    P.op("dve", lambda e: e.scalar_tensor_tensor(out=tc_[:], in0=ta[:], scalar=0.5, in1=tc_[:], op0=ALU.mult, op1=ALU.subtract), reads=[b_ta, b_tc], writes=[b_tc])
                    P.op("act", lambda e: e.activation(out=tc_[:], in_=tc_[:], func=AF.Exp), reads=[b_tc], writes=[b_tc])
                    P.op("dve", lambda e: e.tensor_tensor(out=tc_[:], in0=tc_[:], in1=u[:], op=ALU.mult), reads=[b_tc, b_u], writes=[b_tc])
                    init = 0.0 if t == 0 else carry[:, ch:ch + 1]
                    P.op("dve", lambda e, init=init: e.tensor_tensor_scan(out=hs[:], data0=tb[:], data1=tc_[:], initial=init, op0=ALU.mult, op1=ALU.add),
                         reads=[b_tb, b_tc, b_carry[ch]], writes=[b_hs])
                    P.op("dve", lambda e, ch=ch: e.tensor_copy(out=carry[:, ch:ch + 1], in_=hs[:, TT - 1:TT]), reads=[b_hs], writes=[b_carry[ch]])
                    bank = proj(t, 2 + ch)
                    P.op("dve", lambda e, bank=bank: e.tensor_tensor(out=yt[:], in0=ps[:, bank, :], in1=rstd[:, par, :], op=ALU.mult),
                         reads=[psb[bank], b_rstd[par]], writes=[b_yt])
                    P.op("act", lambda e: e.activation(out=y2[:], in_=yt[:], func=AF.Square), reads=[b_yt], writes=[b_y2])
                    P.op("dve", lambda e: e.tensor_scalar(out=y2[:], in0=y2[:], scalar1=0.044715, scalar2=1.0, op0=ALU.mult, op1=ALU.add), reads=[b_y2], writes=[b_y2])
                    P.op("dve", lambda e: e.tensor_tensor(out=y2[:], in0=y2[:], in1=yt[:], op=ALU.mult), reads=[b_y2, b_yt], writes=[b_y2])
                    P.op("act", lambda e: e.activation(out=y2[:], in_=y2[:], func=AF.Exp, scale=-1.5957691216), reads=[b_y2], writes=[b_y2])
                    P.op("act", lambda e: e.activation(out=y2[:], in_=y2[:], func=AF.Ln, scale=1.0, bias=1.0), reads=[b_y2], writes=[b_y2])
                    P.op("act", lambda e: e.activation(out=y2[:], in_=y2[:], func=AF.Exp, scale=-1.0), reads=[b_y2], writes=[b_y2])
                    P.op("dve", lambda e: e.tensor_tensor(out=yt[:], in0=yt[:], in1=hs[:], op=ALU.mult), reads=[b_yt, b_hs], writes=[b_yt])
                    sl = ocnt[0] % 4
                    ocnt[0] += 1
                    P.op("dve", lambda e, sl=sl: e.tensor_tensor(out=ost[:, sl, :], in0=yt[:], in1=y2[:], op=ALU.mult), reads=[b_yt, b_y2], writes=[b_ost[sl]])
                    j, off = t // 2, (t % 2) * TT
                    P.op("sp", lambda e, sl=sl, ch=ch, j=j, off=off: e.dma_start(out=ag_lru_in[j, ch * 128:(ch + 1) * 128, off:off + TT], in_=ost[:, sl, :]),
                         reads=[b_ost[sl]], dma_sem=s_ost[sl])

            for t in range(NTILE):
                x_tile(t)
                for ch in range(2):
                    lru_step(t, ch)

        def qkv_body(ph, st, x_tile, proj, rstd, b_rstd, A):
            P = ph.P
            T = lambda name, shape, dt: st.enter_context(nc.sbuf_tensor(name, shape, dt))
            kt = T("kt", [128, TT], F32)
            sqk = T("sqk", [128, TT], BF16)
            lk = T("lk", [128, TT], F32)
            qf = T("qf", [128, TT], F32)
            vt = T("vt", [128, TT], BF16)
            gsb = T("gsb", [128, 2, 32], F32)
            m8 = T("m8", [128, 8], F32)
            b_kt, b_sqk, b_lk, b_qf, b_vt, b_m8 = [Buf(n) for n in "kt sqk lk qf vt m8".split()]
            b_gsb = [Buf("g0"), Buf("g1")]
            psv = ps[:, 6, :].bitcast(BF16)
            for hl in range(2):
                P.op("act", lambda e, hl=hl: e.activation(out=A.Ttab[:, hl, :], in_=ctab[:, C_D:C_D + 64], func=AF.Exp, scale=sc(SM_NSL + hl)), writes=[A.b_tab])
                P.op("dve", lambda e, hl=hl: e.tensor_scalar(out=A.Bdiag[:, hl, :], in0=ctab[:, C_DD:C_DD + 128], scalar1=sc(SM_SL + hl), scalar2=None, op0=ALU.mult), writes=[A.b_tab])
                P.op("dve", lambda e, hl=hl: e.tensor_scalar(out=A.Bfull[:, hl, :], in0=ctab[:, C_DF:C_DF + 128], scalar1=sc(SM_SL + hl), scalar2=None, op0=ALU.mult), writes=[A.b_tab])
                P.op("dve", lambda e, hl=hl: e.memset(A.kmean[:, hl, :], 0.0), writes=[A.b_kmean[hl]])
                P.op("dve", lambda e, hl=hl: e.memset(gsb[:, hl, :], NEG), writes=[b_gsb[hl]])
                P.op("dve", lambda e, hl=hl: e.memset(A.Vext[hl][:, :, 128:130], 1.0), writes=[A.b_V[hl]])
                P.op("dve", lambda e, hl=hl: e.memset(A.selb[hl][:, 0:2, :], 0.0), writes=[A.b_sel[hl]])

            def headnorm(src_bank, par, wcol, dst_f32):
                P.op("dve", lambda e: e.tensor_tensor(out=kt[:], in0=ps[:, src_bank, :], in1=rstd[:, par, :], op=ALU.mult),
                     reads=[psb[src_bank], b_rstd[par]], writes=[b_kt])
                P.op("act", lambda e: e.activation(out=sqk[:], in_=kt[:], func=AF.Square), reads=[b_kt], writes=[b_sqk])
                P.op("pe", lambda e: e.matmul(ps[:, 4, :], lhsT=ones[:], rhs=sqk[:], start=True, stop=True), reads=[b_sqk], writes=[psb[4]])
                P.op("act", lambda e: e.activation(out=lk[:], in_=ps[:, 4, :], func=AF.Ln, scale=1.0 / DH, bias=EPS), reads=[psb[4]], writes=[b_lk])
                P.op("act", lambda e: e.activation(out=lk[:], in_=lk[:], func=AF.Exp, scale=-0.5), reads=[b_lk], writes=[b_lk])

            def qkv_tile(t):
                par = t % 2
                cols = slice(t * TT, (t + 1) * TT)
                x_tile(t)
                for hl in range(2):
                    bank = proj(t, 0 + hl)
                    headnorm(bank, par, V_KW, None)
                    P.op("dve", lambda e, hl=hl: e.scalar_tensor_tensor(out=A.KT[hl][:, cols], in0=kt[:], scalar=vc(V_KW), in1=lk[:], op0=ALU.mult, op1=ALU.mult),
                         reads=[b_kt, b_lk], writes=[A.b_KT[hl]])
                    for bb in range(2):
                        n = 2 * t + bb
                        P.op("dve", lambda e, hl=hl, n=n: e.reduce_sum(out=A.kmean[:, hl, n:n + 1], in_=A.KT[hl][:, n * BLK:(n + 1) * BLK], axis=AX.X),
                             reads=[A.b_KT[hl]], writes=[A.b_kmean[hl]])
                for hl in range(2):
                    bank = proj(t, 2 + hl)
                    P.op("dve", lambda e, bank=bank: e.tensor_tensor(out=vt[:], in0=ps[:, bank, :], in1=rstd[:, par, :], op=ALU.mult),
                         reads=[psb[bank], b_rstd[par]], writes=[b_vt])
                    for c in range(4):
                        P.op("pe", lambda e, c=c: e.transpose(out=psv[:, c * 128:(c + 1) * 128], in_=vt[:, c * 128:(c + 1) * 128], identity=ident[:]),
                             reads=[b_vt], writes=[psb[6]])
                    P.op("act", lambda e, hl=hl: e.activation(out=A.Vext[hl][:, 4 * t:4 * t + 4, 0:128], in_=psv[:, 0:512].rearrange("p (c d) -> p c d", c=4), func=AF.Copy),
                         reads=[psb[6]], writes=[A.b_V[hl]])
                for hl in range(2):
                    bank = proj(t, 4 + hl)
                    headnorm(bank, par, V_QW, None)
                    P.op("dve", lambda e: e.scalar_tensor_tensor(out=qf[:], in0=kt[:], scalar=vc(V_QW), in1=lk[:], op0=ALU.mult, op1=ALU.mult),
                         reads=[b_kt, b_lk], writes=[b_qf])
                    P.op("act", lambda e, hl=hl: e.activation(out=A.QT[hl][:, cols], in_=qf[:], func=AF.Copy), reads=[b_qf], writes=[A.b_QT[hl]])
                    for c in range(4):
                        P.op("pe", lambda e, hl=hl, c=c: e.matmul(ps[:, 5, c * 32:(c + 1) * 32], lhsT=qf[:, c * 128:(c + 1) * 128], rhs=A.kmean[:, hl, :], start=True, stop=True),
                             reads=[b_qf, A.b_kmean[hl]], writes=[psb[5]])
                    for c in range(4):
                        b = 2 * t + c // 2
                        cg = 4 * t + c
                        if b == 0:
                            continue
                        P.op("dve", lambda e, hl=hl, c=c, b=b: e.tensor_copy(out=gsb[:, hl, 0:b], in_=ps[:, 5, c * 32:c * 32 + b]),
                             reads=[psb[5], b_gsb[hl]], writes=[b_gsb[hl]])
                        P.op("dve", lambda e, hl=hl: e.max(out=m8[:], in_=gsb[:, hl, :]), reads=[b_gsb[hl]], writes=[b_m8])
                        P.op("dve", lambda e, hl=hl, cg=cg: e.tensor_scalar(out=A.selb[hl][:, cg, :], in0=gsb[:, hl, :], scalar1=m8[:, 2:3], scalar2=None, op0=ALU.is_ge),
                             reads=[b_gsb[hl], b_m8], writes=[A.b_sel[hl]])
            for t in range(NTILE):
                qkv_tile(t)
            if debug:
                sdb = ph.dsem()
                P.op("sp", lambda e: e.dma_start(out=dbg["kt"], in_=A.KT[0][:]), reads=[A.b_KT[0]], dma_sem=sdb)
                P.op("sp", lambda e: e.dma_start(out=dbg["qt"], in_=A.QT[0][:]), reads=[A.b_QT[0]], dma_sem=sdb)
                P.op("sp", lambda e: e.dma_start(out=dbg["v"], in_=A.Vext[0][:]), reads=[A.b_V[0]], dma_sem=sdb)
                P.op("sp", lambda e: e.dma_start(out=dbg["sel"], in_=A.selb[0][:]), reads=[A.b_sel[0]], dma_sem=sdb)

        def phase_att(A):
            ph = Phase("att")
            P = ph.P
            with contextlib.ExitStack() as st:
                T = lambda name, shape, dt: st.enter_context(nc.sbuf_tensor(name, shape, dt))
                pt = T("pt", [128, 3, 512], BF16)
                tmpd = T("tmpd", [128, 2, 384], F32)
                acc = T("acc", [128, 2, 2, 130], F32)
                mfac = T("mfac", [128, 2, 2, 32], F32)
                rden = T("rden", [128, 2, 2], F32)
                ob = T("ob", [128, 2, 2, 128], BF16)
                ast_ = T("attst", [128, 4, BLK], BF16)
                b_pt = [[Buf(f"pt{i}a"), Buf(f"pt{i}b")] for i in range(3)]
                b_tmpd = _ring(2)
                b_acc = [[Buf("acc00"), Buf("acc01")], [Buf("acc10"), Buf("acc11")]]
                b_mfac = _ring(2)
                b_rden = _ring(2)
                b_ob = _ring(2)
                b_ast = _ring(4)
                s_ast = [ph.dsem() for _ in range(4)]
                pst = ps[:, 6, :].bitcast(BF16)
                sbank = [0]
                obank = [0]
                ptc = [0]
                def att_block(hl, b, it):
                    KT, QT, V = A.KT[hl], A.QT[hl], A.Vext[hl]
                    bK, bQ, bV = A.b_KT[hl], A.b_QT[hl], A.b_V[hl]
                    kb0, kb1 = sc(SM_KB + 2 * hl), sc(SM_KB + 2 * hl + 1)
                    if True:
                        ip = it % 2
                        it += 1
                        q0 = b * BLK
                        if b > 0:
                            for j in range(2):
                                P.op("dve", lambda e, hl=hl, b=b, j=j, ip=ip: e.tensor_tensor(
                                    out=mfac[:, ip, j, 0:b], in0=A.selb[hl][:, 2 * b + j, 0:b], in1=A.Ttab[:, hl, j * 32 + 31 - b:j * 32 + 31], op=ALU.mult),
                                    reads=[A.b_sel[hl], A.b_tab], writes=[b_mfac[ip]])
                        sb = sbank[0] % 3
                        sbank[0] += 1
                        P.op("pe", lambda e, sb=sb: e.matmul(ps[:, sb, 0:128], lhsT=KT[:, q0:q0 + 128], rhs=QT[:, q0:q0 + 128], start=True, stop=True), reads=[bK, bQ], writes=[psb[sb]])
                        P.op("pe", lambda e, sb=sb: e.matmul(ps[:, sb, 128:256], lhsT=KT[:, q0 + 128:q0 + 256], rhs=QT[:, q0 + 128:q0 + 256], start=True, stop=True), reads=[bK, bQ], writes=[psb[sb]])
                        P.op("pe", lambda e, sb=sb: e.matmul(ps[:, sb, 256:384], lhsT=KT[:, q0:q0 + 128], rhs=QT[:, q0 + 128:q0 + 256], start=True, stop=True), reads=[bK, bQ], writes=[psb[sb]])
                        for r, tabl in ((0, A.Bdiag), (1, A.Bdiag), (2, A.Bfull)):
                            P.op("dve", lambda e, sb=sb, r=r, tabl=tabl, ip=ip: e.scalar_tensor_tensor(
                                out=tmpd[:, ip, r * 128:(r + 1) * 128], in0=ps[:, sb, r * 128:(r + 1) * 128], scalar=SCALE, in1=tabl[:, hl, :], op0=ALU.mult, op1=ALU.add),
                                reads=[psb[sb], A.b_tab], writes=[b_tmpd[ip]])
                        pi = ptc[0] % 3
                        ptc[0] += 1
                        P.op("act", lambda e, ip=ip, pi=pi: e.activation(out=pt[:, pi, 0:384], in_=tmpd[:, ip, :], func=AF.Exp), reads=[b_tmpd[ip]], writes=b_pt[pi])
                        ob_ = 3 + obank[0] % 3
                        obank[0] += 1
                        P.op("pe", lambda e, pi=pi, ob_=ob_: e.matmul(ps[:, ob_, 0:129], lhsT=pt[:, pi, 0:128], rhs=V[:, 2 * b, 0:129], start=True, stop=True), reads=b_pt[pi] + [bV], writes=[psb[ob_]])
                        P.op("pe", lambda e, pi=pi, ob_=ob_: e.matmul(ps[:, ob_, 256:385], lhsT=pt[:, pi, 256:384], rhs=V[:, 2 * b, 0:129], start=True, stop=False), reads=b_pt[pi] + [bV], writes=[psb[ob_]])
                        P.op("pe", lambda e, pi=pi, ob_=ob_: e.matmul(ps[:, ob_, 256:385], lhsT=pt[:, pi, 128:256], rhs=V[:, 2 * b + 1, 0:129], start=False, stop=True), reads=b_pt[pi] + [bV], writes=[psb[ob_]])
                        for j in range(2):
                            P.op("dve", lambda e, j=j, ob_=ob_, ip=ip: e.tensor_copy(out=acc[:, ip, j, 0:129], in_=ps[:, ob_, j * 256:j * 256 + 129]),
                                 reads=[psb[ob_]], writes=[b_acc[ip][j]])
                        for n in range(b):
                            k0 = n * BLK
                            sb = sbank[0] % 3
                            sbank[0] += 1
                            P.op("pe", lambda e, sb=sb, k0=k0: e.matmul(ps[:, sb, 0:256], lhsT=KT[:, k0:k0 + 128], rhs=QT[:, q0:q0 + 256], start=True, stop=True), reads=[bK, bQ], writes=[psb[sb]])
                            P.op("pe", lambda e, sb=sb, k0=k0: e.matmul(ps[:, sb, 256:512], lhsT=KT[:, k0 + 128:k0 + 256], rhs=QT[:, q0:q0 + 256], start=True, stop=True), reads=[bK, bQ], writes=[psb[sb]])
                            pi = ptc[0] % 3
                            ptc[0] += 1
                            P.op("act", lambda e, sb=sb, pi=pi: e.activation(out=pt[:, pi, 0:256], in_=ps[:, sb, 0:256], func=AF.Exp, scale=SCALE, bias=kb0), reads=[psb[sb]], writes=[b_pt[pi][0]])
                            P.op("act", lambda e, sb=sb, pi=pi: e.activation(out=pt[:, pi, 256:512], in_=ps[:, sb, 256:512], func=AF.Exp, scale=SCALE, bias=kb1), reads=[psb[sb]], writes=[b_pt[pi][1]])
                            ob_ = 3 + obank[0] % 3
                            obank[0] += 1
                            for j in range(2):
                                P.op("pe", lambda e, pi=pi, ob_=ob_, j=j, n=n: e.matmul(ps[:, ob_, j * 256:j * 256 + 129], lhsT=pt[:, pi, j * 128:(j + 1) * 128], rhs=V[:, 2 * n, 0:129], start=True, stop=False),
                                     reads=[b_pt[pi][0], bV], writes=[psb[ob_]])
                                P.op("pe", lambda e, pi=pi, ob_=ob_, j=j, n=n: e.matmul(ps[:, ob_, j * 256:j * 256 + 129], lhsT=pt[:, pi, 256 + j * 128:256 + (j + 1) * 128], rhs=V[:, 2 * n + 1, 0:129], start=False, stop=True),
                                     reads=[b_pt[pi][1], bV], writes=[psb[ob_]])
                            for j in range(2):
                                P.op("dve", lambda e, j=j, ob_=ob_, ip=ip, n=n: e.scalar_tensor_tensor(
                                    out=acc[:, ip, j, 0:129], in0=ps[:, ob_, j * 256:j * 256 + 129], scalar=mfac[:, ip, j, n:n + 1], in1=acc[:, ip, j, 0:129], op0=ALU.mult, op1=ALU.add),
                                    reads=[psb[ob_], b_mfac[ip], b_acc[ip][j]], writes=[b_acc[ip][j]])
                        for j in range(2):
                            P.op("dve", lambda e, j=j, ip=ip: e.reciprocal(out=rden[:, ip, j:j + 1], in_=acc[:, ip, j, 128:129]), reads=[b_acc[ip][j], b_rden[ip]], writes=[b_rden[ip]])
                            P.op("dve", lambda e, j=j, ip=ip: e.tensor_scalar(out=ob[:, ip, j, :], in0=acc[:, ip, j, 0:128], scalar1=rden[:, ip, j:j + 1], scalar2=None, op0=ALU.mult),
                                 reads=[b_acc[ip][j], b_rden[ip], b_ob[ip]], writes=[b_ob[ip]])
                        for j in range(2):
                            P.op("pe", lambda e, j=j, ip=ip: e.transpose(out=pst[:, j * 128:(j + 1) * 128], in_=ob[:, ip, j, :], identity=ident[:]), reads=[b_ob[ip]], writes=[psb[6]])
                        ai = (it - 1) % 4
                        P.op("act", lambda e, ai=ai: e.activation(out=ast_[:, ai, :], in_=pst[:, 0:BLK], func=AF.Copy), reads=[psb[6]], writes=[b_ast[ai]])
                        jd, off = b // 4, (b % 4) * BLK
                        P.op("sp", lambda e, ai=ai, jd=jd, off=off, hl=hl: e.dma_start(out=ag_att_in[jd, hl * 128:(hl + 1) * 128, off:off + BLK], in_=ast_[:, ai, :]),
                             reads=[b_ast[ai]], dma_sem=s_ast[ai])

                it = 0
                for hl in range(2):
                    for b in range(NBLK):
                        att_block(hl, b, it)
                        it += 1
                ph.finish()

        cc_vals = {}

        def issue_ag(name, src, dst):
            ph = Phase("ag_" + name)
            o = ph.P.op("pool", lambda e: e.collective_compute("AllGather", ALU.bypass, replica_groups=[list(range(NC))],
                                                                ins=[src.rearrange("j f t -> (j f) t")], outs=[dst.rearrange("a f t -> (a f) t")]))
            v = sem_state.get(id(cc_sem), 0) + 1
            sem_state[id(cc_sem)] = v
            o.sem, o.inc, o.val, o.signal = cc_sem, 1, v, True
            cc_vals[name] = v
            ph.finish()

        def phase_B():
            with contextlib.ExitStack() as so:
                TO = lambda name, shape, dt: so.enter_context(nc.sbuf_tensor(name, shape, dt))
                NW = 7
                wr = TO("wr", [128, NW, KD * 128], BF16)
                merged = TO("merged", [128, KD, TB], BF16)
                rstd1 = TO("rstd1", [128, TB], F32)
                lnb = TO("lnb", [128, TB], F32)

                class WStream:
                    def __init__(self, ph, jobs, pf=6):
                        self.ph, self.jobs, self.pf = ph, jobs, pf
                        self.b = _ring(NW)
                        self.s = [ph.dsem() for _ in range(NW)]
                        self.issued = 0

                    def get(self, i):
                        while self.issued < min(len(self.jobs), i + 1 + self.pf):
                            src, L = self.jobs[self.issued]
                            sl = self.issued % NW
                            self.ph.P.op("pool", lambda e, src=src, L=L, sl=sl: e.dma_start(out=wr[:, sl, 0:L], in_=src),
                                         writes=[self.b[sl]], dma_sem=self.s[sl])
                            self.issued += 1
                        sl = i % NW
                        return sl, self.b[sl]

                bankc = [0]

                def nb():
                    b = bankc[0] % 8
                    bankc[0] += 1
                    return b

                ph = Phase("B12")
                P = ph.P
                with contextlib.ExitStack() as st:
                    T = lambda name, shape, dt: st.enter_context(nc.sbuf_tensor(name, shape, dt))
                    attlru = T("attlru", [128, 32, TB], BF16)
                    hb = T("hb", [128, KD, TB], BF16)
                    xring = T("xring", [128, 2, TB], F32)
                    sq1 = T("sq1", [128, 2, TB], BF16)
                    tmp = T("tmpB", [128, 2, 4, TT], F32)
                    b_al = [Buf(f"al{i}") for i in range(16)]
                    b_hb = [Buf(f"hb{k}") for k in range(KD)]
                    b_xr = _ring(2)
                    s_xr = [ph.dsem() for _ in range(2)]
                    b_sq1 = _ring(2)
                    b_tmp = [[Buf(f"t{p}{i}") for i in range(4)] for p in range(2)]
                    b_rstd1, b_lnb = Buf("rstd1"), Buf("lnb")
                    b_mg = [Buf(f"mg{k}") for k in range(KD)]
                    jobs = []
                    for m in range(KD):
                        jobs += [(w_g[m], KD * 128), (w_g[16 + m], KD * 128), (w_pl[m], KD * 128), (w_pa[m], KD * 128)]
                    W = WStream(ph, jobs, pf=3)
                    W.get(0)
                    s_al = ph.dsem()
                    vcache = {}
                    for name, base, src in (("lru", 16, ag_lru_out), ("att", 0, ag_att_out)):
                        src2 = src.rearrange("(c j) (h p) t -> j c h p t", j=NC, p=128)
                        for c in range(NC):
                            for h in range(2):
                                def ld(e, c=c, h=h, base=base, src2=src2):
                                    if "v" not in vcache:
                                        vcache["v"] = e.snap(creg, min_val=0, max_val=NC - 1)
                                    return e.dma_start(out=attlru[:, base + 2 * c + h, :], in_=src2[bass.ds(vcache["v"], 1), c, h].squeeze(0))
                                P.op("sp", ld, dma_sem=s_al, extra=[(cc_sem, cc_vals[name])])
                        P.op("sp", lambda e: e.nop(), writes=b_al[(base // 16) * 8:(base // 16) * 8 + 8], extra=[(s_al, sem_state[id(s_al)])])
                    for k in range(KD):
                        sl = k % 2
                        P.op("sp", lambda e, k=k, sl=sl: e.dma_start(out=xring[:, sl, :], in_=xTs[k * 128:(k + 1) * 128, :]), writes=[b_xr[sl]], dma_sem=s_xr[sl])
                        P.op("act", lambda e, sl=sl: e.activation(out=sq1[:, sl, :], in_=xring[:, sl, :], func=AF.Square), reads=[b_xr[sl]], writes=[b_sq1[sl]])
                        P.op("dve", lambda e, k=k, sl=sl: e.tensor_scalar(out=hb[:, k, :], in0=xring[:, sl, :], scalar1=vc(V_N1 + k), scalar2=None, op0=ALU.mult),
                             reads=[b_xr[sl]], writes=[b_hb[k]])
                        for hf in range(2):
                            P.op("pe", lambda e, k=k, sl=sl, hf=hf: e.matmul(ps[:, hf, :], lhsT=ones[:], rhs=sq1[:, sl, hf * TT:(hf + 1) * TT], start=(k == 0), stop=(k == KD - 1)),
                                 reads=[b_sq1[sl]], writes=[psb[hf]])
                    for hf in range(2):
                        P.op("act", lambda e, hf=hf: e.activation(out=lnb[:, hf * TT:(hf + 1) * TT], in_=ps[:, hf, :], func=AF.Ln, scale=1.0 / D, bias=EPS), reads=[psb[hf]], writes=[b_lnb])
                    P.op("act", lambda e: e.activation(out=rstd1[:], in_=lnb[:], func=AF.Exp, scale=-0.5), reads=[b_lnb], writes=[b_rstd1])
                    bankc[0] = 2
                    def b2_step(m, hf, tp, slots):
                        if True:
                            hs_ = slice(hf * TT, (hf + 1) * TT)
                            banks = []
                            for i, (sl, bw) in enumerate(slots):
                                bk = nb()
                                banks.append(bk)
                                for k in range(KD):
                                    if i < 2:
                                        rhs, rb = hb[:, k, hs_], b_hb[k]
                                    elif i == 2:
                                        rhs, rb = attlru[:, 16 + k, hs_], b_al[8 + k // 2]
                                    else:
                                        rhs, rb = attlru[:, k, hs_], b_al[k // 2]
                                    P.op("pe", lambda e, sl=sl, k=k, bk=bk, rhs=rhs: e.matmul(ps[:, bk, :], lhsT=wr[:, sl, k * 128:(k + 1) * 128], rhs=rhs, start=(k == 0), stop=(k == KD - 1)),
                                         reads=[bw, rb], writes=[psb[bk]])
                            for i in range(2):
                                gb, pb = banks[i], banks[3 - i]
                                P.op("dve", lambda e, gb=gb, tp=tp, i=i: e.tensor_tensor(out=tmp[:, tp, i, :], in0=ps[:, gb, :], in1=rstd1[:, hs_], op=ALU.mult),
                                     reads=[psb[gb], b_rstd1], writes=[b_tmp[tp][i]])
                                P.op("act", lambda e, tp=tp, i=i: e.activation(out=tmp[:, tp, i, :], in_=tmp[:, tp, i, :], func=AF.Tanh, scale=0.5), reads=[b_tmp[tp][i]], writes=[b_tmp[tp][i]])
                                P.op("dve", lambda e, pb=pb, tp=tp, i=i: e.scalar_tensor_tensor(out=tmp[:, tp, 2 + i, :], in0=tmp[:, tp, i, :], scalar=1.0, in1=ps[:, pb, :], op0=ALU.add, op1=ALU.mult),
                                     reads=[b_tmp[tp][i], psb[pb]], writes=[b_tmp[tp][2 + i]])
                            P.op("dve", lambda e, tp=tp: e.tensor_tensor(out=tmp[:, tp, 2, :], in0=tmp[:, tp, 2, :], in1=tmp[:, tp, 3, :], op=ALU.add),
                                 reads=[b_tmp[tp][2], b_tmp[tp][3]], writes=[b_tmp[tp][2]])
                            P.op("act", lambda e, tp=tp, m=m: e.activation(out=merged[:, m, hs_], in_=tmp[:, tp, 2, :], func=AF.Copy, scale=0.5), reads=[b_tmp[tp][2]], writes=[b_mg[m]])

                    it = 0
                    for m in range(KD):
                        slots = [W.get(4 * m + i) for i in range(4)]
                        for hf in range(2):
                            b2_step(m, hf, it % 2, slots)
                            it += 1
                    if debug:
                        P.op("sp", lambda e: e.dma_start(out=dbg["merged"], in_=merged[:]), reads=b_mg, dma_sem=ph.dsem())
                        sdb = ph.dsem()
                        P.op("sp", lambda e: e.dma_start(out=dbg["lru"], in_=ag_lru_in), dma_sem=sdb)
                        P.op("sp", lambda e: e.dma_start(out=dbg["att"], in_=ag_att_in), dma_sem=sdb)
                    ph.finish()

                ph = Phase("B36")
                P = ph.P
                with contextlib.ExitStack() as st:
                    T = lambda name, shape, dt: st.enter_context(nc.sbuf_tensor(name, shape, dt))
                    x1 = T("x1", [128, KD, TB], F32)
                    h2b = T("h2b", [128, KD, TB], BF16)
                    actT = T("actT", [128, FGC, TB], BF16)
                    sq2 = T("sq2", [128, 2, TB], BF16)
                    tmp = T("tmpF", [128, 2, 2, TT], F32)
                    rstd2 = T("rstd2", [128, TB], F32)
                    b_x1 = [[Buf(f"x1_{m}_{h}") for h in range(2)] for m in range(KD)]
                    b_h2 = [Buf(f"h2{k}") for k in range(KD)]
                    b_act = [[Buf(f"a{f}_{h}") for h in range(2)] for f in range(FGC)]
                    b_sq2 = _ring(2)
                    b_tmp = [[Buf(f"tf{p}{i}") for i in range(2)] for p in range(2)]
                    b_rstd2, b_lnb = Buf("rstd2"), Buf("lnb")
                    b_mg = Buf("mg")
                    s_x1 = ph.dsem()
                    jobs = [(w_o[m], KD * 128) for m in range(KD)]
                    for g in range(FG):
                        for f in range(FGC):
                            jobs += [(w_fg[g * FGC + f], KD * 128), (w_fu[g * FGC + f], KD * 128)]
                        jobs += [(w_fd[g * 16 + m], FGC * 128) for m in range(KD)]
                    W = WStream(ph, jobs, pf=4)
                    W.get(0)
                    for m in range(KD):
                        P.op("sp", lambda e, m=m: e.dma_start(out=x1[:, m, :], in_=xTs[m * 128:(m + 1) * 128, :]), dma_sem=s_x1)
                    P.op("sp", lambda e: e.nop(), writes=[b for bb in b_x1 for b in bb], extra=[(s_x1, sem_state[id(s_x1)])])
                    ji = 0
                    for m in range(KD):
                        sl, bw = W.get(ji)
                        ji += 1
                        for hf in range(2):
                            hs_ = slice(hf * TT, (hf + 1) * TT)
                            bk = nb()
                            for k in range(KD):
                                P.op("pe", lambda e, sl=sl, k=k, bk=bk, hs_=hs_: e.matmul(ps[:, bk, :], lhsT=wr[:, sl, k * 128:(k + 1) * 128], rhs=merged[:, k, hs_], start=(k == 0), stop=(k == KD - 1)),
                                     reads=[bw, b_mg], writes=[psb[bk]])
                            P.op("dve", lambda e, m=m, bk=bk, hs_=hs_: e.tensor_tensor(out=x1[:, m, hs_], in0=x1[:, m, hs_], in1=ps[:, bk, :], op=ALU.add),
                                 reads=[psb[bk], b_x1[m][hf]], writes=[b_x1[m][hf]])
                    if debug:
                        P.op("sp", lambda e: e.dma_start(out=dbg["x1"], in_=x1[:]), reads=[b for bb in b_x1 for b in bb], dma_sem=ph.dsem())
                    sb0, sb1 = nb(), nb()
                    for k in range(KD):
                        sl = k % 2
                        P.op("act", lambda e, k=k, sl=sl: e.activation(out=sq2[:, sl, :], in_=x1[:, k, :], func=AF.Square), reads=b_x1[k], writes=[b_sq2[sl]])
                        for hf, bk in ((0, sb0), (1, sb1)):
                            P.op("pe", lambda e, k=k, sl=sl, hf=hf, bk=bk: e.matmul(ps[:, bk, :], lhsT=ones[:], rhs=sq2[:, sl, hf * TT:(hf + 1) * TT], start=(k == 0), stop=(k == KD - 1)),
                                 reads=[b_sq2[sl]], writes=[psb[bk]])
                    for hf, bk in ((0, sb0), (1, sb1)):
                        P.op("act", lambda e, hf=hf, bk=bk: e.activation(out=lnb[:, hf * TT:(hf + 1) * TT], in_=ps[:, bk, :], func=AF.Ln, scale=1.0 / D, bias=EPS), reads=[psb[bk]], writes=[b_lnb])
                    P.op("act", lambda e: e.activation(out=rstd2[:], in_=lnb[:], func=AF.Exp, scale=-0.5), reads=[b_lnb], writes=[b_rstd2])
                    for k in range(KD):
                        P.op("dve", lambda e, k=k: e.scalar_tensor_tensor(out=h2b[:, k, :], in0=x1[:, k, :], scalar=vc(V_N2 + k), in1=rstd2[:], op0=ALU.mult, op1=ALU.mult),
                             reads=b_x1[k] + [b_rstd2], writes=[b_h2[k]])
                    it = 0
                    for g in range(FG):
                        for f in range(FGC):
                            (slg, bwg), (slu, bwu) = W.get(ji), W.get(ji + 1)
                            ji += 2
                            for hf in range(2):
                                tp = it % 2
                                it += 1
                                hs_ = slice(hf * TT, (hf + 1) * TT)
                                bg, bu = nb(), nb()
                                for sl, bw, bk in ((slg, bwg, bg), (slu, bwu, bu)):
                                    for k in range(KD):
                                        P.op("pe", lambda e, sl=sl, k=k, bk=bk, hs_=hs_: e.matmul(ps[:, bk, :], lhsT=wr[:, sl, k * 128:(k + 1) * 128], rhs=h2b[:, k, hs_], start=(k == 0), stop=(k == KD - 1)),
                                             reads=[bw, b_h2[k]], writes=[psb[bk]])
                                P.op("act", lambda e, tp=tp, bg=bg: e.activation(out=tmp[:, tp, 0, :], in_=ps[:, bg, :], func=AF.Tanh, scale=0.5), reads=[psb[bg]], writes=[b_tmp[tp][0]])
                                P.op("dve", lambda e, tp=tp, bg=bg: e.scalar_tensor_tensor(out=tmp[:, tp, 1, :], in0=tmp[:, tp, 0, :], scalar=1.0, in1=ps[:, bg, :], op0=ALU.add, op1=ALU.mult),
                                     reads=[b_tmp[tp][0], psb[bg]], writes=[b_tmp[tp][1]])
                                P.op("dve", lambda e, tp=tp, bu=bu, f=f, hs_=hs_: e.scalar_tensor_tensor(out=actT[:, f, hs_], in0=tmp[:, tp, 1, :], scalar=0.5, in1=ps[:, bu, :], op0=ALU.mult, op1=ALU.mult),
                                     reads=[b_tmp[tp][1], psb[bu]], writes=[b_act[f][hf]])
                        for m in range(KD):
                            sl, bw = W.get(ji)
                            ji += 1
                            for hf in range(2):
                                hs_ = slice(hf * TT, (hf + 1) * TT)
                                bk = nb()
                                for f in range(FGC):
                                    P.op("pe", lambda e, sl=sl, f=f, bk=bk, hs_=hs_: e.matmul(ps[:, bk, :], lhsT=wr[:, sl, f * 128:(f + 1) * 128], rhs=actT[:, f, hs_], start=(f == 0), stop=(f == FGC - 1)),
                                         reads=[bw, b_act[f][hf]], writes=[psb[bk]])
                                P.op("dve", lambda e, m=m, bk=bk, hs_=hs_: e.tensor_tensor(out=x1[:, m, hs_], in0=x1[:, m, hs_], in1=ps[:, bk, :], op=ALU.add),
                                     reads=[psb[bk], b_x1[m][hf]], writes=[b_x1[m][hf]])
                    s_out = ph.dsem()
                    for m in range(KD):
                        P.op("sp", lambda e, m=m: e.dma_start(out=outT[m * 128:(m + 1) * 128, :], in_=x1[:, m, :]), reads=b_x1[m], dma_sem=s_out)
                    ph.finish()

        phase_A("lru")
        issue_ag("lru", ag_lru_in, ag_lru_out)
        with contextlib.ExitStack() as sa:
            A = Ctx()
            TA = lambda name, shape, dt: sa.enter_context(nc.sbuf_tensor(name, shape, dt))
            A.QT = [TA(f"QT{h}", [128, S], BF16) for h in range(2)]
            A.KT = [TA(f"KT{h}", [128, S], BF16) for h in range(2)]
            A.Vext = [TA(f"V{h}", [128, 64, 130], BF16) for h in range(2)]
            A.selb = [TA(f"sel{h}", [128, 64, 32], BF16) for h in range(2)]
            A.kmean = TA("kmean", [128, 2, 32], F32)
            A.Ttab = TA("Ttab", [128, 2, 64], F32)
            A.Bdiag = TA("Bdiag", [128, 2, 128], F32)
            A.Bfull = TA("Bfull", [128, 2, 128], F32)
            A.b_QT = [Buf("QT0"), Buf("QT1")]
            A.b_KT = [Buf("KT0"), Buf("KT1")]
            A.b_V = [Buf("V0"), Buf("V1")]
            A.b_sel = [Buf("sel0"), Buf("sel1")]
            A.b_kmean = [Buf("km0"), Buf("km1")]
            A.b_tab = Buf("tab")
            phase_A("qkv", A)
            for lst in (A.b_QT, A.b_KT, A.b_V, A.b_sel, A.b_kmean, [A.b_tab]):
                for b_ in lst:
                    b_.last_w, b_.readers = None, []
            phase_att(A)
        issue_ag("att", ag_att_in, ag_att_out)
        phase_B()
    return nc


def _slabs(W):
    K, N = W.shape
    return np.ascontiguousarray(W.reshape(K // 128, 128, N // 128, 128).transpose(2, 1, 0, 3).reshape(N // 128, 128, (K // 128) * 128))


def _const_tables():
    ct = np.zeros((128, NCT), np.float32)
    q = np.arange(128, dtype=np.float32)[:, None]
    for j in range(2):
        i = np.arange(32, dtype=np.float32)[None, :]
        ct[:, C_D + j * 32:C_D + (j + 1) * 32] = 256.0 * (31 - i) + 128.0 * j + q - 255.0
    ct[:, C_D + 31] = 0.0
    ct[:, C_D + 63] = 0.0
    for kc in range(2):
        ct[:, C_KB + kc] = 128.0 * kc + q[:, 0] - 255.0
    p = np.arange(128, dtype=np.float32)[:, None]
    qq = np.arange(128, dtype=np.float32)[None, :]
    ct[:, C_DD:C_DD + 128] = np.where(p <= qq, p - qq, -1.0e9)
    ct[:, C_DF:C_DF + 128] = p - qq - 128.0
    return ct


def make_in_maps(inp):
    f32 = lambda a: np.ascontiguousarray(np.asarray(a, dtype=np.float32))
    x = f32(inp["x"])[0]
    xT = np.ascontiguousarray(x.T)
    w_in = f32(inp["w_in"])[0]
    ctab = _const_tables()
    shared = dict(
        xT=xT, ctab=ctab,
        w_g=_slabs(w_in[:, 10240:14336]),
        w_pa=_slabs(f32(inp["w_proj_attn"])[0]),
        w_pl=_slabs(f32(inp["w_proj_lru"])[0]),
        w_o=_slabs(f32(inp["w_out"])[0]),
        w_fg=_slabs(f32(inp["w_ffn_gate"])[0]),
        w_fu=_slabs(f32(inp["w_ffn_up"])[0]),
    )
    wfd = f32(inp["w_ffn_down"])[0]
    shared["w_fd"] = np.ascontiguousarray(
        wfd.reshape(FG, FGC, 128, KD, 128).transpose(0, 3, 2, 1, 4).reshape(FG * KD, 128, FGC * 128))
    n1, n2 = f32(inp["norm1_w"])[0], f32(inp["norm2_w"])[0]
    cw, cb = f32(inp["conv_w"])[0], f32(inp["conv_b"])[0]
    ba, bx, lam = f32(inp["b_rg_a"])[0], f32(inp["b_rg_x"])[0], f32(inp["lru_lambda"])[0]
    qw, kw = f32(inp["q_norm_w"])[0], f32(inp["k_norm_w"])[0]
    wra, wrx = f32(inp["w_rg_a"])[0], f32(inp["w_rg_x"])[0]
    maps = []
    for c in range(NC):
        cols = np.concatenate([np.arange(256) + base + 256 * c for base in (6144, 8192, 2048, 4096, 0)])
        w_a = np.ascontiguousarray(w_in[:, cols].reshape(KD, 128, 1280).transpose(1, 0, 2))
        vecs = np.zeros((128, NV), np.float32)
        vecs[:, V_N1:V_N1 + KD] = n1.reshape(KD, 128).T
        vecs[:, V_N2:V_N2 + KD] = n2.reshape(KD, 128).T
        for ch in range(2):
            sl = slice(256 * c + 128 * ch, 256 * c + 128 * ch + 128)
            for tap in range(4):
                vecs[:, V_CW + ch * 4 + tap] = cw[tap, sl]
            vecs[:, V_CB + ch] = cb[sl]
            vecs[:, V_BA + ch] = ba[sl]
            vecs[:, V_BX + ch] = bx[sl]
            vecs[:, V_LAM + ch] = lam[sl]
            vecs[:, V_HI + ch] = float(2 * c + ch + 1)
        vecs[:, V_QW] = qw
        vecs[:, V_KW] = kw
        w_rg = np.zeros((128, 2, 2, 128), np.float32)
        for ch in range(2):
            w_rg[:, 0, ch, :] = wra[2 * c + ch]
            w_rg[:, 1, ch, :] = wrx[2 * c + ch]
        m = dict(shared)
        m.update(w_a=w_a, vecs=vecs, w_rg=w_rg, xTs=np.ascontiguousarray(xT[:, c * TB:(c + 1) * TB]),
                 cidx=np.array([[c]], np.int32))
        maps.append(m)
    return maps


_NC_CACHE = {}


def kernel(**inputs):
    if "nc" not in _NC_CACHE:
        _NC_CACHE["nc"] = build_program(debug=True)
    nc = _NC_CACHE["nc"]
    in_maps = make_in_maps(inputs)
    res = run_bass_kernel_spmd(nc, in_maps, core_ids=list(range(NC)))
    out = np.empty((1, S, D), np.float32)
    for c in range(NC):
        out[0, c * TB:(c + 1) * TB, :] = res.results[c]["outT"].T
    return out
```

```python
import numpy as np
import concourse.bass as bass
import concourse.mybir as mybir
from concourse.bass_utils import run_bass_kernel_spmd

F32 = mybir.dt.float32
BF16 = mybir.dt.bfloat16
AF = mybir.ActivationFunctionType
ALU = mybir.AluOpType
AX = mybir.AxisListType


class Buf:
    __slots__ = ("name", "last_w", "readers")

    def __init__(self, name):
        self.name = name
        self.last_w = None
        self.readers = []


class Op:
    __slots__ = ("eng", "fn", "deps", "signal", "sem", "val", "inc", "idx", "extra")

    def __init__(self, eng, fn, idx):
        self.eng = eng
        self.fn = fn
        self.deps = []
        self.signal = False
        self.sem = None
        self.val = None
        self.inc = 1
        self.idx = idx
        self.extra = ()


class Prog:
    ENGS = ("pe", "act", "dve", "pool", "sp")

    def __init__(self, nc, same_engine_sync=True):
        self.nc = nc
        self.ops = []
        self.same_engine_sync = same_engine_sync
        self.dma_sem_count = {}

    def op(self, eng, fn, reads=(), writes=(), dma_sem=None, extra=()):
        o = Op(eng, fn, len(self.ops))
        o.extra = tuple(extra)
        deps = {}
        for b in reads:
            if b.last_w is not None:
                deps[b.last_w.idx] = b.last_w
        for b in writes:
            if b.last_w is not None:
                deps[b.last_w.idx] = b.last_w
            for r in b.readers:
                deps[r.idx] = r
        is_dma = dma_sem is not None
        for d in deps.values():
            d_is_dma = d.sem is not None
            if d.eng == eng and not d_is_dma and not is_dma:
                if eng == "pe" or not self.same_engine_sync:
                    continue
            if d is o:
                continue
            o.deps.append(d)
            d.signal = True
        if is_dma:
            o.sem = dma_sem
            o.inc = 16
            o.signal = True
            c = self.dma_sem_count.get(id(dma_sem), 0) + 16
            self.dma_sem_count[id(dma_sem)] = c
            o.val = c
        for b in reads:
            b.readers.append(o)
        for b in writes:
            b.last_w = o
            b.readers = []
        self.ops.append(o)
        return o

    def emit(self, block, eng_sems, final_sems=(), counters=None):
        nc = self.nc
        if counters is None:
            counters = {e: 0 for e in self.ENGS}
        for o in self.ops:
            if o.sem is None:
                if o.signal:
                    counters[o.eng] += 1
                    o.sem = eng_sems[o.eng]
                    o.val = counters[o.eng]
        by_eng = {e: [o for o in self.ops if o.eng == e] for e in self.ENGS}

        def run(engine, ops, is_last_owner):
            seen = {}
            for o in ops:
                need = {}
                for d in o.deps:
                    k = id(d.sem)
                    if k not in need or need[k][1] < d.val:
                        need[k] = (d.sem, d.val)
                for (xs, xv) in o.extra:
                    k = id(xs)
                    if k not in need or need[k][1] < xv:
                        need[k] = (xs, xv)
                for k, (s, v) in need.items():
                    if seen.get(k, 0) >= v:
                        continue
                    engine.wait_ge(s, v)
                    seen[k] = v
                ins = o.fn(engine)
                if o.signal:
                    ins.then_inc(o.sem, o.inc)
            if is_last_owner:
                for s in final_sems:
                    engine.wait_ge(s, self.dma_sem_count[id(s)])

        if by_eng["pe"]:
            @block.tensor
            def _(e):
                run(e, by_eng["pe"], False)
        if by_eng["act"]:
            @block.scalar
            def _(e):
                run(e, by_eng["act"], False)
        if by_eng["dve"]:
            @block.vector
            def _(e):
                run(e, by_eng["dve"], False)
        if by_eng["pool"]:
            @block.gpsimd
            def _(e):
                run(e, by_eng["pool"], False)

        @block.sync
        def _(e):
            run(e, by_eng["sp"], True)


S = 8192
D = 2048
H = 16
DH = 128
BLK = 256
NBLK = S // BLK
DFF = 5632
NFF = DFF // 128
KD = D // 128
TT = 512
NTILE = S // TT
NC = 8
TB = S // NC
EPS = 1e-6
SCALE = DH ** -0.5
FG = 4
FGC = NFF // FG
NEG = -1.0e30

V_N1 = 0
V_CW = 16
V_CB = 24
V_BA = 26
V_BX = 28
V_LAM = 30
V_QW = 32
V_KW = 33
V_N2 = 34
V_HI = 50
NV = 52
C_D = 0
C_KB = 64
C_DD = 66
C_DF = 194
NCT = 322


class Ctx:
    pass


def _ring(n):
    return [Buf(f"r{i}") for i in range(n)]


def build_program(debug=False):
    nc = bass.Bass("TRN2", target_bir_lowering=False)
    dt_i32 = mybir.dt.int32

    def din(name, shape, dt=F32):
        return nc.dram_tensor(name, list(shape), dt, kind="ExternalInput").ap()

    def dout(name, shape, dt=F32):
        return nc.dram_tensor(name, list(shape), dt, kind="ExternalOutput").ap()

    xT = din("xT", [D, S])
    xTs = din("xTs", [D, TB])
    w_a = din("w_a", [128, KD, 1280])
    vecs_d = din("vecs", [128, NV])
    ctab_d = din("ctab", [128, NCT])
    wrg_d = din("w_rg", [128, 2, 2, 128])
    cidx = din("cidx", [1, 1], dt_i32)
    w_g = din("w_g", [32, 128, KD * 128])
    w_pa = din("w_pa", [16, 128, KD * 128])
    w_pl = din("w_pl", [16, 128, KD * 128])
    w_o = din("w_o", [16, 128, KD * 128])
    w_fg = din("w_fg", [NFF, 128, KD * 128])
    w_fu = din("w_fu", [NFF, 128, KD * 128])
    w_fd = din("w_fd", [FG * 16, 128, FGC * 128])
    outT = dout("outT", [D, TB])

    ag_lru_in = nc.dram_tensor("ag_lru_in", [NC, 256, TB], BF16).ap()
    ag_att_in = nc.dram_tensor("ag_att_in", [NC, 256, TB], BF16).ap()
    ag_lru_out = nc.dram_tensor("ag_lru_out", [NC * NC, 256, TB], BF16).ap()
    ag_att_out = nc.dram_tensor("ag_att_out", [NC * NC, 256, TB], BF16).ap()

    dbg = {}
    if debug:
        dbg["lru"] = dout("dbg_lru", [NC, 256, TB], BF16)
        dbg["att"] = dout("dbg_att", [NC, 256, TB], BF16)
        dbg["kt"] = dout("dbg_kt", [128, S], BF16)
        dbg["qt"] = dout("dbg_qt", [128, S], BF16)
        dbg["v"] = dout("dbg_v", [128, 64, 130], BF16)
        dbg["sel"] = dout("dbg_sel", [128, 64, 32], BF16)
        dbg["merged"] = dout("dbg_merged", [128, KD, TB], BF16)
        dbg["x1"] = dout("dbg_x1", [128, KD, TB], F32)

    sem_state = {}
    eng_counts = {e: 0 for e in Prog.ENGS}

    import contextlib
    es = contextlib.ExitStack()
    with es:
        eng_sems = {e: es.enter_context(nc.semaphore(f"s_{e}")) for e in Prog.ENGS}
        NDS = 40
        dsems = [es.enter_context(nc.semaphore(f"d{i}")) for i in range(NDS)]
        cc_sem = es.enter_context(nc.semaphore("cc"))
        creg = es.enter_context(nc.sync.register("creg"))
        ps = es.enter_context(nc.psum_tensor("ps", [128, 8, 512], F32))
        psb = [Buf(f"ps{i}") for i in range(8)]

        vecs = es.enter_context(nc.sbuf_tensor("vecs_sb", [128, NV], F32))
        ctab = es.enter_context(nc.sbuf_tensor("ctab_sb", [128, NCT], F32))
        ident = es.enter_context(nc.sbuf_tensor("ident", [128, 128], BF16))
        ones = es.enter_context(nc.sbuf_tensor("ones", [128, 128], BF16))
        sm = es.enter_context(nc.sbuf_tensor("sm", [128, 32], F32))
        SM_SL, SM_NSL, SM_SP, SM_SP2, SM_NBA, SM_NBX, SM_KB = 0, 2, 4, 6, 8, 10, 12

        class Phase:
            def __init__(self, name):
                self.name = name
                self.P = Prog(nc)
                self.P.dma_sem_count = sem_state
                self.used = []
                self.next_ds = 0
                for b_ in psb:
                    b_.last_w, b_.readers = None, []

            def dsem(self):
                s = dsems[self.next_ds]
                self.next_ds += 1
                self.used.append(s)
                return s

            def finish(self):
                with nc.Block() as block:
                    self.P.emit(block, eng_sems, final_sems=[s for s in self.used if id(s) in sem_state],
                                counters=eng_counts)

        def sc(col, n=1):
            return sm[:, col:col + n]

        def vc(col, n=1):
            return vecs[:, col:col + n]

        ph = Phase("setup")
        P = ph.P
        b_vecs, b_ctab, b_sm, b_id, b_ones = Buf("vecs"), Buf("ctab"), Buf("sm"), Buf("id"), Buf("ones")
        P.op("sp", lambda e: e.dma_start(out=vecs[:], in_=vecs_d), writes=[b_vecs], dma_sem=ph.dsem())
        P.op("sp", lambda e: e.dma_start(out=ctab[:], in_=ctab_d), writes=[b_ctab], dma_sem=ph.dsem())
        P.op("sp", lambda e: e.reg_load(creg, cidx[0:1, 0:1]))
        P.op("dve", lambda e: e.memset(ident[:], 0.0), writes=[b_id])
        P.op("pool", lambda e: e.affine_select(out=ident[:], in_=ident[:], pattern=[[-1, 128]], compare_op=ALU.not_equal,
                                                fill=1.0, base=0, channel_multiplier=1), reads=[b_id], writes=[b_id])
        P.op("dve", lambda e: e.memset(ones[:], 1.0), writes=[b_ones])
        P.op("act", lambda e: e.activation(out=sc(SM_SL, 2), in_=vc(V_HI, 2), func=AF.Exp, scale=-0.5 * float(np.log(2.0))),
             reads=[b_vecs], writes=[b_sm])
        P.op("dve", lambda e: e.tensor_scalar(out=sc(SM_NSL, 2), in0=sc(SM_SL, 2), scalar1=-1.0, scalar2=None, op0=ALU.mult),
             reads=[b_sm], writes=[b_sm])
        P.op("act", lambda e: e.activation(out=sc(SM_SP, 2), in_=vc(V_LAM, 2), func=AF.Exp, scale=-1.0), reads=[b_vecs, b_sm], writes=[b_sm])
        P.op("act", lambda e: e.activation(out=sc(SM_SP, 2), in_=sc(SM_SP, 2), func=AF.Ln, bias=1.0, scale=1.0), reads=[b_sm], writes=[b_sm])
        P.op("dve", lambda e: e.tensor_scalar(out=sc(SM_SP2, 2), in0=sc(SM_SP, 2), scalar1=-16.0, scalar2=None, op0=ALU.mult), reads=[b_sm], writes=[b_sm])
        P.op("dve", lambda e: e.tensor_scalar(out=sc(SM_SP, 2), in0=sc(SM_SP, 2), scalar1=-8.0, scalar2=None, op0=ALU.mult), reads=[b_sm], writes=[b_sm])
        P.op("dve", lambda e: e.tensor_scalar(out=sc(SM_NBA, 2), in0=vc(V_BA, 2), scalar1=-1.0, scalar2=None, op0=ALU.mult), reads=[b_vecs, b_sm], writes=[b_sm])
        P.op("dve", lambda e: e.tensor_scalar(out=sc(SM_NBX, 2), in0=vc(V_BX, 2), scalar1=-1.0, scalar2=None, op0=ALU.mult), reads=[b_vecs, b_sm], writes=[b_sm])
        for hl in range(2):
            P.op("dve", lambda e, hl=hl: e.tensor_scalar(out=sc(SM_KB + 2 * hl, 2), in0=ctab[:, C_KB:C_KB + 2], scalar1=sc(SM_SL + hl),
                                                          scalar2=None, op0=ALU.mult), reads=[b_ctab, b_sm], writes=[b_sm])
        ph.finish()

        def phase_A(which, A=None):
            ph = Phase("A_" + which)
            P = ph.P
            if which == "qkv":
                issue_ag(ph, "lru", ag_lru_in, ag_lru_out)
            c0, ncol = (0, 512) if which == "lru" else (512, 768)
            nm = ncol // 128
            with contextlib.ExitStack() as st:
                T = lambda name, shape, dt: st.enter_context(nc.sbuf_tensor(which + "_" + name, shape, dt))
                wa = T("wa", [128, KD, ncol], BF16)
                NXF = 6 if which == "lru" else 4
                NWS = 2 if which == "lru" else 1
                wst = T("wst", [128, NWS, ncol], F32)
                xf = T("xf", [128, NXF, TT], F32)
                sqb = T("sqb", [128, 3, TT], BF16)
                xb = T("xb", [128, 2, KD, TT], BF16)
                rstd = T("rstd", [128, 2, TT], F32)
                lnt = T("lnt", [128, TT], F32)
                b_wa = [Buf(f"wa{k}") for k in range(KD)]
                b_wst = _ring(NWS)
                s_wst = [ph.dsem() for _ in range(NWS)]
                b_xf = _ring(NXF)
                s_xf = [ph.dsem() for _ in range(NXF)]
                b_sqb = _ring(3)
                b_xb = [[Buf(f"xb{p}_{k}") for k in range(KD)] for p in range(2)]
                b_rstd = _ring(2)
                b_lnt = Buf("lnt")
                BV = Buf("vecs_ro")

                for k in range(KD):
                    sl = k % NWS
                    P.op("sp", lambda e, k=k, sl=sl: e.dma_start(out=wst[:, sl, :], in_=w_a[:, k, c0:c0 + ncol]),
                         writes=[b_wst[sl]], dma_sem=s_wst[sl])
                    P.op("dve", lambda e, k=k, sl=sl: e.tensor_scalar(out=wa[:, k, :], in0=wst[:, sl, :], scalar1=vc(V_N1 + k),
                                                                     scalar2=None, op0=ALU.mult),
                         reads=[b_wst[sl]], writes=[b_wa[k]])

                xcnt = [0]
                sqcnt = [0]

                def x_tile(t):
                    par = t % 2
                    for k in range(KD):
                        sl = xcnt[0] % NXF
                        xcnt[0] += 1
                        sq = sqcnt[0] % 3
                        sqcnt[0] += 1
                        P.op("sp", lambda e, k=k, sl=sl: e.dma_start(out=xf[:, sl, :], in_=xT[k * 128:(k + 1) * 128, t * TT:(t + 1) * TT]),
                             writes=[b_xf[sl]], dma_sem=s_xf[sl])
                        P.op("act", lambda e, sl=sl, sq=sq: e.activation(out=sqb[:, sq, :], in_=xf[:, sl, :], func=AF.Square),
                             reads=[b_xf[sl]], writes=[b_sqb[sq]])
                        P.op("dve", lambda e, k=k, sl=sl: e.tensor_copy(out=xb[:, par, k, :], in_=xf[:, sl, :]),
                             reads=[b_xf[sl]], writes=[b_xb[par][k]])
                        P.op("pe", lambda e, k=k, sq=sq: e.matmul(ps[:, 0, :], lhsT=ones[:], rhs=sqb[:, sq, :], start=(k == 0), stop=(k == KD - 1)),
                             reads=[b_sqb[sq]], writes=[psb[0]])
                    P.op("act", lambda e: e.activation(out=lnt[:], in_=ps[:, 0, :], func=AF.Ln, scale=1.0 / D, bias=EPS),
                         reads=[psb[0]], writes=[b_lnt])
                    P.op("act", lambda e: e.activation(out=rstd[:, par, :], in_=lnt[:], func=AF.Exp, scale=-0.5),
                         reads=[b_lnt], writes=[b_rstd[par]])

                pcnt = [0]

                def proj(t, m):
                    par = t % 2
                    bank = 1 + pcnt[0] % 3
                    pcnt[0] += 1
                    for k in range(KD):
                        P.op("pe", lambda e, k=k, bank=bank: e.matmul(ps[:, bank, :], lhsT=wa[:, k, m * 128:(m + 1) * 128], rhs=xb[:, par, k, :],
                                                                       start=(k == 0), stop=(k == KD - 1)),
                             reads=[b_wa[k], b_xb[par][k]], writes=[psb[bank]])
                    return bank

                if which == "lru":
                    lru_body(ph, st, x_tile, proj, rstd, b_rstd)
                else:
                    qkv_body(ph, st, x_tile, proj, rstd, b_rstd, A)
                ph.finish()

        def lru_body(ph, st, x_tile, proj, rstd, b_rstd):
            P = ph.P
            T = lambda name, shape, dt: st.enter_context(nc.sbuf_tensor(name, shape, dt))
            wrgf = T("wrgf", [128, 2, 2, 128], F32)
            wrg = T("wrg", [128, 2, 2, 128], BF16)
            xrt = T("xrt", [128, 2, TT + 3], F32)
            u = T("u", [128, TT], F32)
            ub = T("ub", [128, TT], BF16)
            ta = T("ta", [128, TT], F32)
            tb = T("tb", [128, TT], F32)
            tc_ = T("tc", [128, TT], F32)
            td = T("td", [128, TT], F32)
            hs = T("hs", [128, TT], F32)
            yt = T("yt", [128, TT], F32)
            y2 = T("y2", [128, TT], F32)
            carry = T("carry", [128, 2], F32)
            ost = T("ost", [128, 4, TT], BF16)
            b_wrgf, b_wrg = Buf("wrgf"), Buf("wrg")
            b_xrt = [Buf("xrt0"), Buf("xrt1")]
            b_u, b_ub, b_ta, b_tb, b_tc, b_td, b_hs, b_yt, b_y2 = [Buf(n) for n in "u ub ta tb tc td hs yt y2".split()]
            b_carry = [Buf("c0"), Buf("c1")]
            b_ost = _ring(4)
            s_ost = [ph.dsem() for _ in range(4)]
            BV = Buf("ro")
            P.op("sp", lambda e: e.dma_start(out=wrgf[:], in_=wrg_d), writes=[b_wrgf], dma_sem=ph.dsem())
            P.op("dve", lambda e: e.tensor_copy(out=wrg[:], in_=wrgf[:]), reads=[b_wrgf], writes=[b_wrg])
            for ch in range(2):
                P.op("dve", lambda e, ch=ch: e.memset(xrt[:, ch, :], 0.0), writes=[b_xrt[ch]])
            ocnt = [0]

            def lru_step(t, ch):
                if True:
                    par = t % 2
                    cw = lambda tap: vc(V_CW + ch * 4 + tap)
                    if t > 0:
                        P.op("dve", lambda e, ch=ch: e.tensor_copy(out=xrt[:, ch, 0:3], in_=xrt[:, ch, TT:TT + 3]),
                             reads=[b_xrt[ch]], writes=[b_xrt[ch]])
                    bank = proj(t, ch)
                    P.op("dve", lambda e, ch=ch, bank=bank: e.tensor_tensor(out=xrt[:, ch, 3:TT + 3], in0=ps[:, bank, :], in1=rstd[:, par, :], op=ALU.mult),
                         reads=[psb[bank], b_rstd[par], b_xrt[ch]], writes=[b_xrt[ch]])
                    P.op("dve", lambda e, ch=ch: e.tensor_scalar(out=u[:], in0=xrt[:, ch, 3:TT + 3], scalar1=cw(3), scalar2=vc(V_CB + ch), op0=ALU.mult, op1=ALU.add),
                         reads=[b_xrt[ch]], writes=[b_u])
                    for tap in (2, 1, 0):
                        P.op("dve", lambda e, ch=ch, tap=tap: e.scalar_tensor_tensor(out=u[:], in0=xrt[:, ch, tap:tap + TT], scalar=cw(tap), in1=u[:], op0=ALU.mult, op1=ALU.add),
                             reads=[b_xrt[ch], b_u], writes=[b_u])
                    P.op("act", lambda e: e.activation(out=ub[:], in_=u[:], func=AF.Copy), reads=[b_u], writes=[b_ub])
                    P.op("pe", lambda e, ch=ch: e.matmul(ps[:, 4, :], lhsT=wrg[:, 0, ch, :], rhs=ub[:], start=True, stop=True), reads=[b_wrg, b_ub], writes=[psb[4]])
                    P.op("pe", lambda e, ch=ch: e.matmul(ps[:, 5, :], lhsT=wrg[:, 1, ch, :], rhs=ub[:], start=True, stop=True), reads=[b_wrg, b_ub], writes=[psb[5]])
                    P.op("act", lambda e, ch=ch: e.activation(out=ta[:], in_=ps[:, 4, :], func=AF.Exp, scale=-1.0, bias=sc(SM_NBA + ch)), reads=[psb[4]], writes=[b_ta])
                    P.op("act", lambda e: e.activation(out=ta[:], in_=ta[:], func=AF.Ln, scale=1.0, bias=1.0), reads=[b_ta], writes=[b_ta])
                    P.op("act", lambda e: e.activation(out=ta[:], in_=ta[:], func=AF.Exp, scale=-1.0), reads=[b_ta], writes=[b_ta])
                    P.op("act", lambda e, ch=ch: e.activation(out=tb[:], in_=ta[:], func=AF.Exp, scale=sc(SM_SP + ch)), reads=[b_ta], writes=[b_tb])
                    P.op("act", lambda e, ch=ch: e.activation(out=ta[:], in_=ta[:], func=AF.Exp, scale=sc(SM_SP2 + ch)), reads=[b_ta], writes=[b_ta])
                    P.op("act", lambda e: e.activation(out=ta[:], in_=ta[:], func=AF.Ln, scale=-1.0, bias=1.0), reads=[b_ta], writes=[b_ta])
                    P.op("act", lambda e, ch=ch: e.activation(out=tc_[:], in_=ps[:, 5, :], func=AF.Exp, scale=-1.0, bias=sc(SM_NBX + ch)), reads=[psb[5]], writes=[b_tc])
                    P.op("act", lambda e: e.activation(out=tc_[:], in_=tc_[:], func=AF.Ln, scale=1.0, bias=1.0), reads=[b_tc], writes=[b_tc])
                    P.op("dve", lambda e: e.scalar_tensor_tensor(out=tc_[:], in0=ta[:], scalar=0.5, in1=tc_[:], op0=ALU.mult, op1=ALU.subtract), reads=[b_ta, b_tc], writes=[b_tc])
                    P.op("act", lambda e: e.activation(out=tc_[:], in_=tc_[:], func=AF.Exp), reads=[b_tc], writes=[b_tc])
                    P.op("dve", lambda e: e.tensor_tensor(out=tc_[:], in0=tc_[:], in1=u[:], op=ALU.mult), reads=[b_tc, b_u], writes=[b_tc])
                    init = 0.0 if t == 0 else carry[:, ch:ch + 1]
                    P.op("dve", lambda e, init=init: e.tensor_tensor_scan(out=hs[:], data0=tb[:], data1=tc_[:], initial=init, op0=ALU.mult, op1=ALU.add),
                         reads=[b_tb, b_tc, b_carry[ch]], writes=[b_hs])
                    P.op("dve", lambda e, ch=ch: e.tensor_copy(out=carry[:, ch:ch + 1], in_=hs[:, TT - 1:TT]), reads=[b_hs], writes=[b_carry[ch]])
                    bank = proj(t, 2 + ch)
                    P.op("dve", lambda e, bank=bank: e.tensor_tensor(out=yt[:], in0=ps[:, bank, :], in1=rstd[:, par, :], op=ALU.mult),
                         reads=[psb[bank], b_rstd[par]], writes=[b_yt])
                    P.op("act", lambda e: e.activation(out=y2[:], in_=yt[:], func=AF.Square), reads=[b_yt], writes=[b_y2])
                    P.op("dve", lambda e: e.tensor_scalar(out=y2[:], in0=y2[:], scalar1=0.044715, scalar2=1.0, op0=ALU.mult, op1=ALU.add), reads=[b_y2], writes=[b_y2])
                    P.op("dve", lambda e: e.tensor_tensor(out=y2[:], in0=y2[:], in1=yt[:], op=ALU.mult), reads=[b_y2, b_yt], writes=[b_y2])
                    P.op("act", lambda e: e.activation(out=y2[:], in_=y2[:], func=AF.Exp, scale=-1.5957691216), reads=[b_y2], writes=[b_y2])
                    P.op("act", lambda e: e.activation(out=y2[:], in_=y2[:], func=AF.Ln, scale=1.0, bias=1.0), reads=[b_y2], writes=[b_y2])
                    P.op("act", lambda e: e.activation(out=y2[:], in_=y2[:], func=AF.Exp, scale=-1.0), reads=[b_y2], writes=[b_y2])
                    P.op("dve", lambda e: e.tensor_tensor(out=yt[:], in0=yt[:], in1=hs[:], op=ALU.mult), reads=[b_yt, b_hs], writes=[b_yt])
                    sl = ocnt[0] % 4
                    ocnt[0] += 1
                    P.op("dve", lambda e, sl=sl: e.tensor_tensor(out=ost[:, sl, :], in0=yt[:], in1=y2[:], op=ALU.mult), reads=[b_yt, b_y2], writes=[b_ost[sl]])
                    j, off = t // 2, (t % 2) * TT
                    P.op("sp", lambda e, sl=sl, ch=ch, j=j, off=off: e.dma_start(out=ag_lru_in[j, ch * 128:(ch + 1) * 128, off:off + TT], in_=ost[:, sl, :]),
                         reads=[b_ost[sl]], dma_sem=s_ost[sl])

            for t in range(NTILE):
                x_tile(t)
                for ch in range(2):
                    lru_step(t, ch)

        def qkv_body(ph, st, x_tile, proj, rstd, b_rstd, A):
            P = ph.P
            T = lambda name, shape, dt: st.enter_context(nc.sbuf_tensor(name, shape, dt))
            kt = T("kt", [128, TT], F32)
            sqk = T("sqk", [128, TT], BF16)
            lk = T("lk", [128, TT], F32)
            qf = T("qf", [128, TT], F32)
            vt = T("vt", [128, TT], BF16)
            gsb = T("gsb", [128, 2, 32], F32)
            m8 = T("m8", [128, 8], F32)
            b_kt, b_sqk, b_lk, b_qf, b_vt, b_m8 = [Buf(n) for n in "kt sqk lk qf vt m8".split()]
            b_gsb = [Buf("g0"), Buf("g1")]
            psv = ps[:, 6, :].bitcast(BF16)
            for hl in range(2):
                P.op("act", lambda e, hl=hl: e.activation(out=A.Ttab[:, hl, :], in_=ctab[:, C_D:C_D + 64], func=AF.Exp, scale=sc(SM_NSL + hl)), writes=[A.b_tab])
                P.op("dve", lambda e, hl=hl: e.tensor_scalar(out=A.Bdiag[:, hl, :], in0=ctab[:, C_DD:C_DD + 128], scalar1=sc(SM_SL + hl), scalar2=None, op0=ALU.mult), writes=[A.b_tab])
                P.op("dve", lambda e, hl=hl: e.tensor_scalar(out=A.Bfull[:, hl, :], in0=ctab[:, C_DF:C_DF + 128], scalar1=sc(SM_SL + hl), scalar2=None, op0=ALU.mult), writes=[A.b_tab])
                P.op("dve", lambda e, hl=hl: e.memset(A.kmean[:, hl, :], 0.0), writes=[A.b_kmean[hl]])
                P.op("dve", lambda e, hl=hl: e.memset(gsb[:, hl, :], NEG), writes=[b_gsb[hl]])
                P.op("dve", lambda e, hl=hl: e.memset(A.Vext[hl][:, :, 128:130], 1.0), writes=[A.b_V[hl]])
                P.op("dve", lambda e, hl=hl: e.memset(A.selb[hl][:, 0:2, :], 0.0), writes=[A.b_sel[hl]])

            def headnorm(src_bank, par, wcol, dst_f32):
                P.op("dve", lambda e: e.tensor_tensor(out=kt[:], in0=ps[:, src_bank, :], in1=rstd[:, par, :], op=ALU.mult),
                     reads=[psb[src_bank], b_rstd[par]], writes=[b_kt])
                P.op("act", lambda e: e.activation(out=sqk[:], in_=kt[:], func=AF.Square), reads=[b_kt], writes=[b_sqk])
                P.op("pe", lambda e: e.matmul(ps[:, 4, :], lhsT=ones[:], rhs=sqk[:], start=True, stop=True), reads=[b_sqk], writes=[psb[4]])
                P.op("act", lambda e: e.activation(out=lk[:], in_=ps[:, 4, :], func=AF.Ln, scale=1.0 / DH, bias=EPS), reads=[psb[4]], writes=[b_lk])
                P.op("act", lambda e: e.activation(out=lk[:], in_=lk[:], func=AF.Exp, scale=-0.5), reads=[b_lk], writes=[b_lk])

            def qkv_tile(t):
                par = t % 2
                cols = slice(t * TT, (t + 1) * TT)
                x_tile(t)
                for hl in range(2):
                    bank = proj(t, 0 + hl)
                    headnorm(bank, par, V_KW, None)
                    P.op("dve", lambda e, hl=hl: e.scalar_tensor_tensor(out=A.KT[hl][:, cols], in0=kt[:], scalar=vc(V_KW), in1=lk[:], op0=ALU.mult, op1=ALU.mult),
                         reads=[b_kt, b_lk], writes=[A.b_KT[hl]])
                    for bb in range(2):
                        n = 2 * t + bb
                        P.op("dve", lambda e, hl=hl, n=n: e.reduce_sum(out=A.kmean[:, hl, n:n + 1], in_=A.KT[hl][:, n * BLK:(n + 1) * BLK], axis=AX.X),
                             reads=[A.b_KT[hl]], writes=[A.b_kmean[hl]])
                for hl in range(2):
                    bank = proj(t, 2 + hl)
                    P.op("dve", lambda e, bank=bank: e.tensor_tensor(out=vt[:], in0=ps[:, bank, :], in1=rstd[:, par, :], op=ALU.mult),
                         reads=[psb[bank], b_rstd[par]], writes=[b_vt])
                    for c in range(4):
                        P.op("pe", lambda e, c=c: e.transpose(out=psv[:, c * 128:(c + 1) * 128], in_=vt[:, c * 128:(c + 1) * 128], identity=ident[:]),
                             reads=[b_vt], writes=[psb[6]])
                    P.op("act", lambda e, hl=hl: e.activation(out=A.Vext[hl][:, 4 * t:4 * t + 4, 0:128], in_=psv[:, 0:512].rearrange("p (c d) -> p c d", c=4), func=AF.Copy),
                         reads=[psb[6]], writes=[A.b_V[hl]])
                for hl in range(2):
                    bank = proj(t, 4 + hl)
                    headnorm(bank, par, V_QW, None)
                    P.op("dve", lambda e: e.scalar_tensor_tensor(out=qf[:], in0=kt[:], scalar=vc(V_QW), in1=lk[:], op0=ALU.mult, op1=ALU.mult),
                         reads=[b_kt, b_lk], writes=[b_qf])
                    P.op("act", lambda e, hl=hl: e.activation(out=A.QT[hl][:, cols], in_=qf[:], func=AF.Copy), reads=[b_qf], writes=[A.b_QT[hl]])
                    for c in range(4):
                        P.op("pe", lambda e, hl=hl, c=c: e.matmul(ps[:, 5, c * 32:(c + 1) * 32], lhsT=qf[:, c * 128:(c + 1) * 128], rhs=A.kmean[:, hl, :], start=True, stop=True),
                             reads=[b_qf, A.b_kmean[hl]], writes=[psb[5]])
                    for c in range(4):
                        b = 2 * t + c // 2
                        cg = 4 * t + c
                        if b == 0:
                            continue
                        P.op("dve", lambda e, hl=hl, c=c, b=b: e.tensor_copy(out=gsb[:, hl, 0:b], in_=ps[:, 5, c * 32:c * 32 + b]),
                             reads=[psb[5], b_gsb[hl]], writes=[b_gsb[hl]])
                        P.op("dve", lambda e, hl=hl: e.max(out=m8[:], in_=gsb[:, hl, :]), reads=[b_gsb[hl]], writes=[b_m8])
                        P.op("dve", lambda e, hl=hl, cg=cg: e.tensor_scalar(out=A.selb[hl][:, cg, :], in0=gsb[:, hl, :], scalar1=m8[:, 2:3], scalar2=None, op0=ALU.is_ge),
                             reads=[b_gsb[hl], b_m8], writes=[A.b_sel[hl]])
            for t in range(NTILE):
                qkv_tile(t)
            if debug:
                sdb = ph.dsem()
                P.op("sp", lambda e: e.dma_start(out=dbg["kt"], in_=A.KT[0][:]), reads=[A.b_KT[0]], dma_sem=sdb)
                P.op("sp", lambda e: e.dma_start(out=dbg["qt"], in_=A.QT[0][:]), reads=[A.b_QT[0]], dma_sem=sdb)
                P.op("sp", lambda e: e.dma_start(out=dbg["v"], in_=A.Vext[0][:]), reads=[A.b_V[0]], dma_sem=sdb)
                P.op("sp", lambda e: e.dma_start(out=dbg["sel"], in_=A.selb[0][:]), reads=[A.b_sel[0]], dma_sem=sdb)

        def phase_att(A):
            ph = Phase("att")
            P = ph.P
            NA = 4
            LA = 2
            with contextlib.ExitStack() as st:
                T = lambda name, shape, dt: st.enter_context(nc.sbuf_tensor(name, shape, dt))
                pt = T("pt", [128, 3, 512], BF16)
                tmpd = T("tmpd", [128, 2, 384], F32)
                acc = T("acc", [128, 2, NA, 2, 130], F32)
                mfac = T("mfac", [128, 2, 2, 32], F32)
                rden = T("rden", [128, 2, 2], F32)
                ob = T("ob", [128, 2, 2, 128], BF16)
                ast_ = T("attst", [128, 4, BLK], BF16)
                b_pt = [[Buf(f"pt{i}a"), Buf(f"pt{i}b")] for i in range(3)]
                b_tmpd = _ring(2)
                b_acc = [[[Buf(f"acc{p}{a}{j}") for j in range(2)] for a in range(NA)] for p in range(2)]
                b_mfac = _ring(2)
                b_rden = _ring(2)
                b_ob = _ring(2)
                b_ast = _ring(4)
                s_ast = [ph.dsem() for _ in range(4)]
                pst = ps[:, 6, :].bitcast(BF16)

                items = []
                for hl in range(2):
                    for b in range(NBLK):
                        items.append((hl, b, -1))
                        for n in range(b):
                            items.append((hl, b, n))
                N = len(items)
                blk_index = {}
                for hl in range(2):
                    for b in range(NBLK):
                        blk_index[(hl, b)] = hl * NBLK + b

                def stage_qk(i):
                    hl, b, n = items[i]
                    KT, QT = A.KT[hl], A.QT[hl]
                    bK, bQ = A.b_KT[hl], A.b_QT[hl]
                    kb0, kb1 = sc(SM_KB + 2 * hl), sc(SM_KB + 2 * hl + 1)
                    q0 = b * BLK
                    sb = i % 3
                    pi = i % 3
                    ip = blk_index[(hl, b)] % 2
                    if n < 0:
                        P.op("pe", lambda e: e.matmul(ps[:, sb, 0:128], lhsT=KT[:, q0:q0 + 128], rhs=QT[:, q0:q0 + 128], start=True, stop=True), reads=[bK, bQ], writes=[psb[sb]])
                        P.op("pe", lambda e: e.matmul(ps[:, sb, 128:256], lhsT=KT[:, q0 + 128:q0 + 256], rhs=QT[:, q0 + 128:q0 + 256], start=True, stop=True), reads=[bK, bQ], writes=[psb[sb]])
                        P.op("pe", lambda e: e.matmul(ps[:, sb, 256:384], lhsT=KT[:, q0:q0 + 128], rhs=QT[:, q0 + 128:q0 + 256], start=True, stop=True), reads=[bK, bQ], writes=[psb[sb]])
                        for r, tabl in ((0, A.Bdiag), (1, A.Bdiag), (2, A.Bfull)):
                            P.op("dve", lambda e, r=r, tabl=tabl: e.scalar_tensor_tensor(
                                out=tmpd[:, ip, r * 128:(r + 1) * 128], in0=ps[:, sb, r * 128:(r + 1) * 128], scalar=SCALE, in1=tabl[:, hl, :], op0=ALU.mult, op1=ALU.add),
                                reads=[psb[sb], A.b_tab], writes=[b_tmpd[ip]])
                        P.op("act", lambda e: e.activation(out=pt[:, pi, 0:384], in_=tmpd[:, ip, :], func=AF.Exp), reads=[b_tmpd[ip]], writes=b_pt[pi])
                    else:
                        k0 = n * BLK
                        P.op("pe", lambda e: e.matmul(ps[:, sb, 0:256], lhsT=KT[:, k0:k0 + 128], rhs=QT[:, q0:q0 + 256], start=True, stop=True), reads=[bK, bQ], writes=[psb[sb]])
                        P.op("pe", lambda e: e.matmul(ps[:, sb, 256:512], lhsT=KT[:, k0 + 128:k0 + 256], rhs=QT[:, q0:q0 + 256], start=True, stop=True), reads=[bK, bQ], writes=[psb[sb]])
                        P.op("act", lambda e: e.activation(out=pt[:, pi, 0:256], in_=ps[:, sb, 0:256], func=AF.Exp, scale=SCALE, bias=kb0), reads=[psb[sb]], writes=[b_pt[pi][0]])
                        P.op("act", lambda e: e.activation(out=pt[:, pi, 256:512], in_=ps[:, sb, 256:512], func=AF.Exp, scale=SCALE, bias=kb1), reads=[psb[sb]], writes=[b_pt[pi][1]])

                def stage_pv(i):
                    hl, b, n = items[i]
                    V, bV = A.Vext[hl], A.b_V[hl]
                    pi = i % 3
                    ob_ = 3 + i % 3
                    bi = blk_index[(hl, b)]
                    ip = bi % 2
                    if n < 0:
                        if b > 0:
                            for j in range(2):
                                P.op("dve", lambda e, j=j: e.tensor_tensor(
                                    out=mfac[:, ip, j, 0:b], in0=A.selb[hl][:, 2 * b + j, 0:b], in1=A.Ttab[:, hl, j * 32 + 31 - b:j * 32 + 31], op=ALU.mult),
                                    reads=[A.b_sel[hl], A.b_tab], writes=[b_mfac[ip]])
                        P.op("pe", lambda e: e.matmul(ps[:, ob_, 0:129], lhsT=pt[:, pi, 0:128], rhs=V[:, 2 * b, 0:129], start=True, stop=True), reads=b_pt[pi] + [bV], writes=[psb[ob_]])
                        P.op("pe", lambda e: e.matmul(ps[:, ob_, 256:385], lhsT=pt[:, pi, 256:384], rhs=V[:, 2 * b, 0:129], start=True, stop=False), reads=b_pt[pi] + [bV], writes=[psb[ob_]])
                        P.op("pe", lambda e: e.matmul(ps[:, ob_, 256:385], lhsT=pt[:, pi, 128:256], rhs=V[:, 2 * b + 1, 0:129], start=False, stop=True), reads=b_pt[pi] + [bV], writes=[psb[ob_]])
                        for j in range(2):
                            P.op("dve", lambda e, j=j: e.tensor_copy(out=acc[:, ip, 0, j, 0:129], in_=ps[:, ob_, j * 256:j * 256 + 129]),
                                 reads=[psb[ob_]], writes=[b_acc[ip][0][j]])
                    else:
                        for j in range(2):
                            P.op("pe", lambda e, j=j: e.matmul(ps[:, ob_, j * 256:j * 256 + 129], lhsT=pt[:, pi, j * 128:(j + 1) * 128], rhs=V[:, 2 * n, 0:129], start=True, stop=False),
                                 reads=[b_pt[pi][0], bV], writes=[psb[ob_]])
                            P.op("pe", lambda e, j=j: e.matmul(ps[:, ob_, j * 256:j * 256 + 129], lhsT=pt[:, pi, 256 + j * 128:256 + (j + 1) * 128], rhs=V[:, 2 * n + 1, 0:129], start=False, stop=True),
                                 reads=[b_pt[pi][1], bV], writes=[psb[ob_]])
                        a = (n + 1) % NA
                        first = (n + 1) < NA
                        for j in range(2):
                            if first:
                                P.op("dve", lambda e, j=j: e.tensor_scalar(out=acc[:, ip, a, j, 0:129], in0=ps[:, ob_, j * 256:j * 256 + 129], scalar1=mfac[:, ip, j, n:n + 1], scalar2=None, op0=ALU.mult),
                                     reads=[psb[ob_], b_mfac[ip]], writes=[b_acc[ip][a][j]])
                            else:
                                P.op("dve", lambda e, j=j: e.scalar_tensor_tensor(
                                    out=acc[:, ip, a, j, 0:129], in0=ps[:, ob_, j * 256:j * 256 + 129], scalar=mfac[:, ip, j, n:n + 1], in1=acc[:, ip, a, j, 0:129], op0=ALU.mult, op1=ALU.add),
                                    reads=[psb[ob_], b_mfac[ip], b_acc[ip][a][j]], writes=[b_acc[ip][a][j]])
                    if n == b - 1:
                        nacc = min(NA, b + 1)
                        for j in range(2):
                            for a in range(1, nacc):
                                P.op("dve", lambda e, j=j, a=a: e.tensor_tensor(out=acc[:, ip, 0, j, 0:129], in0=acc[:, ip, 0, j, 0:129], in1=acc[:, ip, a, j, 0:129], op=ALU.add),
                                     reads=[b_acc[ip][0][j], b_acc[ip][a][j]], writes=[b_acc[ip][0][j]])
                            P.op("dve", lambda e, j=j: e.reciprocal(out=rden[:, ip, j:j + 1], in_=acc[:, ip, 0, j, 128:129]), reads=[b_acc[ip][0][j], b_rden[ip]], writes=[b_rden[ip]])
                            P.op("dve", lambda e, j=j: e.tensor_scalar(out=ob[:, ip, j, :], in0=acc[:, ip, 0, j, 0:128], scalar1=rden[:, ip, j:j + 1], scalar2=None, op0=ALU.mult),
                                 reads=[b_acc[ip][0][j], b_rden[ip], b_ob[ip]], writes=[b_ob[ip]])
                        for j in range(2):
                            P.op("pe", lambda e, j=j: e.transpose(out=pst[:, j * 128:(j + 1) * 128], in_=ob[:, ip, j, :], identity=ident[:]), reads=[b_ob[ip]], writes=[psb[6]])
                        ai = bi % 4
                        P.op("act", lambda e: e.activation(out=ast_[:, ai, :], in_=pst[:, 0:BLK], func=AF.Copy), reads=[psb[6]], writes=[b_ast[ai]])
                        jd, off = b // 4, (b % 4) * BLK
                        P.op("sp", lambda e: e.dma_start(out=ag_att_in[jd, hl * 128:(hl + 1) * 128, off:off + BLK], in_=ast_[:, ai, :]),
                             reads=[b_ast[ai]], dma_sem=s_ast[ai])

                for i in range(-LA, N):
                    if i + LA < N:
                        stage_qk(i + LA)
                    if i >= 0:
                        stage_pv(i)
                ph.finish()

        cc_vals = {}

        def issue_ag(ph, name, src, dst):
            o = ph.P.op("pool", lambda e: e.collective_compute("AllGather", ALU.bypass, replica_groups=[list(range(NC))],
                                                                ins=[src.rearrange("j f t -> (j f) t")], outs=[dst.rearrange("a f t -> (a f) t")]))
            v = sem_state.get(id(cc_sem), 0) + 1
            sem_state[id(cc_sem)] = v
            o.sem, o.inc, o.val, o.signal = cc_sem, 1, v, True
            cc_vals[name] = v

        def phase_B():
            with contextlib.ExitStack() as so:
                TO = lambda name, shape, dt: so.enter_context(nc.sbuf_tensor(name, shape, dt))
                NW = 7
                wr = TO("wr", [128, NW, KD * 128], BF16)
                merged = TO("merged", [128, KD, TB], BF16)
                rstd1 = TO("rstd1", [128, TB], F32)
                lnb = TO("lnb", [128, TB], F32)

                class WStream:
                    def __init__(self, ph, jobs, pf=6):
                        self.ph, self.jobs, self.pf = ph, jobs, pf
                        self.b = _ring(NW)
                        self.s = [ph.dsem() for _ in range(NW)]
                        self.issued = 0

                    def get(self, i):
                        while self.issued < min(len(self.jobs), i + 1 + self.pf):
                            src, L = self.jobs[self.issued]
                            sl = self.issued % NW
                            self.ph.P.op("pool", lambda e, src=src, L=L, sl=sl: e.dma_start(out=wr[:, sl, 0:L], in_=src),
                                         writes=[self.b[sl]], dma_sem=self.s[sl])
                            self.issued += 1
                        sl = i % NW
                        return sl, self.b[sl]

                bankc = [0]

                def nb():
                    b = bankc[0] % 8
                    bankc[0] += 1
                    return b

                ph = Phase("B12")
                P = ph.P
                issue_ag(ph, "att", ag_att_in, ag_att_out)
                with contextlib.ExitStack() as st:
                    T = lambda name, shape, dt: st.enter_context(nc.sbuf_tensor(name, shape, dt))
                    attlru = T("attlru", [128, 32, TB], BF16)
                    hb = T("hb", [128, KD, TB], BF16)
                    xring = T("xring", [128, 2, TB], F32)
                    sq1 = T("sq1", [128, 2, TB], BF16)
                    tmp = T("tmpB", [128, 2, 4, TT], F32)
                    b_al = [Buf(f"al{i}") for i in range(16)]
                    b_hb = [Buf(f"hb{k}") for k in range(KD)]
                    b_xr = _ring(2)
                    s_xr = [ph.dsem() for _ in range(2)]
                    b_sq1 = _ring(2)
                    b_tmp = [[Buf(f"t{p}{i}") for i in range(4)] for p in range(2)]
                    b_rstd1, b_lnb = Buf("rstd1"), Buf("lnb")
                    b_mg = [Buf(f"mg{k}") for k in range(KD)]
                    jobs = []
                    for m in range(KD):
                        jobs += [(w_g[m], KD * 128), (w_g[16 + m], KD * 128), (w_pl[m], KD * 128), (w_pa[m], KD * 128)]
                    W = WStream(ph, jobs, pf=3)
                    W.get(0)
                    s_al = ph.dsem()
                    vcache = {}
                    for name, base, src in (("lru", 16, ag_lru_out), ("att", 0, ag_att_out)):
                        src2 = src.rearrange("(c j) (h p) t -> j c h p t", j=NC, p=128)
                        for c in range(NC):
                            for h in range(2):
                                def ld(e, c=c, h=h, base=base, src2=src2):
                                    if "v" not in vcache:
                                        vcache["v"] = e.snap(creg, min_val=0, max_val=NC - 1)
                                    return e.dma_start(out=attlru[:, base + 2 * c + h, :], in_=src2[bass.ds(vcache["v"], 1), c, h].squeeze(0))
                                P.op("sp", ld, dma_sem=s_al, extra=[(cc_sem, cc_vals[name])])
                        P.op("sp", lambda e: e.nop(), writes=b_al[(base // 16) * 8:(base // 16) * 8 + 8], extra=[(s_al, sem_state[id(s_al)])])
                    for k in range(KD):
                        sl = k % 2
                        P.op("sp", lambda e, k=k, sl=sl: e.dma_start(out=xring[:, sl, :], in_=xTs[k * 128:(k + 1) * 128, :]), writes=[b_xr[sl]], dma_sem=s_xr[sl])
                        P.op("act", lambda e, sl=sl: e.activation(out=sq1[:, sl, :], in_=xring[:, sl, :], func=AF.Square), reads=[b_xr[sl]], writes=[b_sq1[sl]])
                        P.op("dve", lambda e, k=k, sl=sl: e.tensor_scalar(out=hb[:, k, :], in0=xring[:, sl, :], scalar1=vc(V_N1 + k), scalar2=None, op0=ALU.mult),
                             reads=[b_xr[sl]], writes=[b_hb[k]])
                        for hf in range(2):
                            P.op("pe", lambda e, k=k, sl=sl, hf=hf: e.matmul(ps[:, hf, :], lhsT=ones[:], rhs=sq1[:, sl, hf * TT:(hf + 1) * TT], start=(k == 0), stop=(k == KD - 1)),
                                 reads=[b_sq1[sl]], writes=[psb[hf]])
                    for hf in range(2):
                        P.op("act", lambda e, hf=hf: e.activation(out=lnb[:, hf * TT:(hf + 1) * TT], in_=ps[:, hf, :], func=AF.Ln, scale=1.0 / D, bias=EPS), reads=[psb[hf]], writes=[b_lnb])
                    P.op("act", lambda e: e.activation(out=rstd1[:], in_=lnb[:], func=AF.Exp, scale=-0.5), reads=[b_lnb], writes=[b_rstd1])
                    bankc[0] = 2
                    def b2_step(m, hf, tp, slots):
                        if True:
                            hs_ = slice(hf * TT, (hf + 1) * TT)
                            banks = []
                            for i, (sl, bw) in enumerate(slots):
                                bk = nb()
                                banks.append(bk)
                                for k in range(KD):
                                    if i < 2:
                                        rhs, rb = hb[:, k, hs_], b_hb[k]
                                    elif i == 2:
                                        rhs, rb = attlru[:, 16 + k, hs_], b_al[8 + k // 2]
                                    else:
                                        rhs, rb = attlru[:, k, hs_], b_al[k // 2]
                                    P.op("pe", lambda e, sl=sl, k=k, bk=bk, rhs=rhs: e.matmul(ps[:, bk, :], lhsT=wr[:, sl, k * 128:(k + 1) * 128], rhs=rhs, start=(k == 0), stop=(k == KD - 1)),
                                         reads=[bw, rb], writes=[psb[bk]])
                            for i in range(2):
                                gb, pb = banks[i], banks[3 - i]
                                P.op("dve", lambda e, gb=gb, tp=tp, i=i: e.tensor_tensor(out=tmp[:, tp, i, :], in0=ps[:, gb, :], in1=rstd1[:, hs_], op=ALU.mult),
                                     reads=[psb[gb], b_rstd1], writes=[b_tmp[tp][i]])
                                P.op("act", lambda e, tp=tp, i=i: e.activation(out=tmp[:, tp, i, :], in_=tmp[:, tp, i, :], func=AF.Tanh, scale=0.5), reads=[b_tmp[tp][i]], writes=[b_tmp[tp][i]])
                                P.op("dve", lambda e, pb=pb, tp=tp, i=i: e.scalar_tensor_tensor(out=tmp[:, tp, 2 + i, :], in0=tmp[:, tp, i, :], scalar=1.0, in1=ps[:, pb, :], op0=ALU.add, op1=ALU.mult),
                                     reads=[b_tmp[tp][i], psb[pb]], writes=[b_tmp[tp][2 + i]])
                            P.op("dve", lambda e, tp=tp: e.tensor_tensor(out=tmp[:, tp, 2, :], in0=tmp[:, tp, 2, :], in1=tmp[:, tp, 3, :], op=ALU.add),
                                 reads=[b_tmp[tp][2], b_tmp[tp][3]], writes=[b_tmp[tp][2]])
                            P.op("act", lambda e, tp=tp, m=m: e.activation(out=merged[:, m, hs_], in_=tmp[:, tp, 2, :], func=AF.Copy, scale=0.5), reads=[b_tmp[tp][2]], writes=[b_mg[m]])

                    it = 0
                    for m in range(KD):
                        slots = [W.get(4 * m + i) for i in range(4)]
                        for hf in range(2):
                            b2_step(m, hf, it % 2, slots)
                            it += 1
                    if debug:
                        P.op("sp", lambda e: e.dma_start(out=dbg["merged"], in_=merged[:]), reads=b_mg, dma_sem=ph.dsem())
                        sdb = ph.dsem()
                        P.op("sp", lambda e: e.dma_start(out=dbg["lru"], in_=ag_lru_in), dma_sem=sdb)
                        P.op("sp", lambda e: e.dma_start(out=dbg["att"], in_=ag_att_in), dma_sem=sdb)
                    ph.finish()

                ph = Phase("B36")
                P = ph.P
                with contextlib.ExitStack() as st:
                    T = lambda name, shape, dt: st.enter_context(nc.sbuf_tensor(name, shape, dt))
                    x1 = T("x1", [128, KD, TB], F32)
                    h2b = T("h2b", [128, KD, TB], BF16)
                    actT = T("actT", [128, FGC, TB], BF16)
                    sq2 = T("sq2", [128, 2, TB], BF16)
                    tmp = T("tmpF", [128, 2, 2, TT], F32)
                    rstd2 = T("rstd2", [128, TB], F32)
                    b_x1 = [[Buf(f"x1_{m}_{h}") for h in range(2)] for m in range(KD)]
                    b_h2 = [Buf(f"h2{k}") for k in range(KD)]
                    b_act = [[Buf(f"a{f}_{h}") for h in range(2)] for f in range(FGC)]
                    b_sq2 = _ring(2)
                    b_tmp = [[Buf(f"tf{p}{i}") for i in range(2)] for p in range(2)]
                    b_rstd2, b_lnb = Buf("rstd2"), Buf("lnb")
                    b_mg = Buf("mg")
                    s_x1 = ph.dsem()
                    jobs = [(w_o[m], KD * 128) for m in range(KD)]
                    for g in range(FG):
                        for f in range(FGC):
                            jobs += [(w_fg[g * FGC + f], KD * 128), (w_fu[g * FGC + f], KD * 128)]
                        jobs += [(w_fd[g * 16 + m], FGC * 128) for m in range(KD)]
                    W = WStream(ph, jobs, pf=4)
                    W.get(0)
                    for m in range(KD):
                        P.op("sp", lambda e, m=m: e.dma_start(out=x1[:, m, :], in_=xTs[m * 128:(m + 1) * 128, :]), dma_sem=s_x1)
                    P.op("sp", lambda e: e.nop(), writes=[b for bb in b_x1 for b in bb], extra=[(s_x1, sem_state[id(s_x1)])])
                    ji = 0
                    for m in range(KD):
                        sl, bw = W.get(ji)
                        ji += 1
                        for hf in range(2):
                            hs_ = slice(hf * TT, (hf + 1) * TT)
                            bk = nb()
                            for k in range(KD):
                                P.op("pe", lambda e, sl=sl, k=k, bk=bk, hs_=hs_: e.matmul(ps[:, bk, :], lhsT=wr[:, sl, k * 128:(k + 1) * 128], rhs=merged[:, k, hs_], start=(k == 0), stop=(k == KD - 1)),
                                     reads=[bw, b_mg], writes=[psb[bk]])
                            P.op("dve", lambda e, m=m, bk=bk, hs_=hs_: e.tensor_tensor(out=x1[:, m, hs_], in0=x1[:, m, hs_], in1=ps[:, bk, :], op=ALU.add),
                                 reads=[psb[bk], b_x1[m][hf]], writes=[b_x1[m][hf]])
                    if debug:
                        P.op("sp", lambda e: e.dma_start(out=dbg["x1"], in_=x1[:]), reads=[b for bb in b_x1 for b in bb], dma_sem=ph.dsem())
                    sb0, sb1 = nb(), nb()
                    for k in range(KD):
                        sl = k % 2
                        P.op("act", lambda e, k=k, sl=sl: e.activation(out=sq2[:, sl, :], in_=x1[:, k, :], func=AF.Square), reads=b_x1[k], writes=[b_sq2[sl]])
                        for hf, bk in ((0, sb0), (1, sb1)):
                            P.op("pe", lambda e, k=k, sl=sl, hf=hf, bk=bk: e.matmul(ps[:, bk, :], lhsT=ones[:], rhs=sq2[:, sl, hf * TT:(hf + 1) * TT], start=(k == 0), stop=(k == KD - 1)),
                                 reads=[b_sq2[sl]], writes=[psb[bk]])
                    for hf, bk in ((0, sb0), (1, sb1)):
                        P.op("act", lambda e, hf=hf, bk=bk: e.activation(out=lnb[:, hf * TT:(hf + 1) * TT], in_=ps[:, bk, :], func=AF.Ln, scale=1.0 / D, bias=EPS), reads=[psb[bk]], writes=[b_lnb])
                    P.op("act", lambda e: e.activation(out=rstd2[:], in_=lnb[:], func=AF.Exp, scale=-0.5), reads=[b_lnb], writes=[b_rstd2])
                    for k in range(KD):
                        P.op("dve", lambda e, k=k: e.scalar_tensor_tensor(out=h2b[:, k, :], in0=x1[:, k, :], scalar=vc(V_N2 + k), in1=rstd2[:], op0=ALU.mult, op1=ALU.mult),
                             reads=b_x1[k] + [b_rstd2], writes=[b_h2[k]])
                    it = 0
                    for g in range(FG):
                        for f in range(FGC):
                            (slg, bwg), (slu, bwu) = W.get(ji), W.get(ji + 1)
                            ji += 2
                            for hf in range(2):
                                tp = it % 2
                                it += 1
                                hs_ = slice(hf * TT, (hf + 1) * TT)
                                bg, bu = nb(), nb()
                                for sl, bw, bk in ((slg, bwg, bg), (slu, bwu, bu)):
                                    for k in range(KD):
                                        P.op("pe", lambda e, sl=sl, k=k, bk=bk, hs_=hs_: e.matmul(ps[:, bk, :], lhsT=wr[:, sl, k * 128:(k + 1) * 128], rhs=h2b[:, k, hs_], start=(k == 0), stop=(k == KD - 1)),
                                             reads=[bw, b_h2[k]], writes=[psb[bk]])
                                P.op("act", lambda e, tp=tp, bg=bg: e.activation(out=tmp[:, tp, 0, :], in_=ps[:, bg, :], func=AF.Tanh, scale=0.5), reads=[psb[bg]], writes=[b_tmp[tp][0]])
                                P.op("dve", lambda e, tp=tp, bg=bg: e.scalar_tensor_tensor(out=tmp[:, tp, 1, :], in0=tmp[:, tp, 0, :], scalar=1.0, in1=ps[:, bg, :], op0=ALU.add, op1=ALU.mult),
                                     reads=[b_tmp[tp][0], psb[bg]], writes=[b_tmp[tp][1]])
                                P.op("dve", lambda e, tp=tp, bu=bu, f=f, hs_=hs_: e.scalar_tensor_tensor(out=actT[:, f, hs_], in0=tmp[:, tp, 1, :], scalar=0.5, in1=ps[:, bu, :], op0=ALU.mult, op1=ALU.mult),
                                     reads=[b_tmp[tp][1], psb[bu]], writes=[b_act[f][hf]])
                        for m in range(KD):
                            sl, bw = W.get(ji)
                            ji += 1
                            for hf in range(2):
                                hs_ = slice(hf * TT, (hf + 1) * TT)
                                bk = nb()
                                for f in range(FGC):
                                    P.op("pe", lambda e, sl=sl, f=f, bk=bk, hs_=hs_: e.matmul(ps[:, bk, :], lhsT=wr[:, sl, f * 128:(f + 1) * 128], rhs=actT[:, f, hs_], start=(f == 0), stop=(f == FGC - 1)),
                                         reads=[bw, b_act[f][hf]], writes=[psb[bk]])
                                P.op("dve", lambda e, m=m, bk=bk, hs_=hs_: e.tensor_tensor(out=x1[:, m, hs_], in0=x1[:, m, hs_], in1=ps[:, bk, :], op=ALU.add),
                                     reads=[psb[bk], b_x1[m][hf]], writes=[b_x1[m][hf]])
                    s_out = ph.dsem()
                    for m in range(KD):
                        P.op("sp", lambda e, m=m: e.dma_start(out=outT[m * 128:(m + 1) * 128, :], in_=x1[:, m, :]), reads=b_x1[m], dma_sem=s_out)
                    ph.finish()

        phase_A("lru")
        with contextlib.ExitStack() as sa:
            A = Ctx()
            TA = lambda name, shape, dt: sa.enter_context(nc.sbuf_tensor(name, shape, dt))
            A.QT = [TA(f"QT{h}", [128, S], BF16) for h in range(2)]
            A.KT = [TA(f"KT{h}", [128, S], BF16) for h in range(2)]
            A.Vext = [TA(f"V{h}", [128, 64, 130], BF16) for h in range(2)]
            A.selb = [TA(f"sel{h}", [128, 64, 32], BF16) for h in range(2)]
            A.kmean = TA("kmean", [128, 2, 32], F32)
            A.Ttab = TA("Ttab", [128, 2, 64], F32)
            A.Bdiag = TA("Bdiag", [128, 2, 128], F32)
            A.Bfull = TA("Bfull", [128, 2, 128], F32)
            A.b_QT = [Buf("QT0"), Buf("QT1")]
            A.b_KT = [Buf("KT0"), Buf("KT1")]
            A.b_V = [Buf("V0"), Buf("V1")]
            A.b_sel = [Buf("sel0"), Buf("sel1")]
            A.b_kmean = [Buf("km0"), Buf("km1")]
            A.b_tab = Buf("tab")
            phase_A("qkv", A)
            for lst in (A.b_QT, A.b_KT, A.b_V, A.b_sel, A.b_kmean, [A.b_tab]):
                for b_ in lst:
                    b_.last_w, b_.readers = None, []
            phase_att(A)
        phase_B()
    return nc


def _slabs(W):
    K, N = W.shape
    return np.ascontiguousarray(W.reshape(K // 128, 128, N // 128, 128).transpose(2, 1, 0, 3).reshape(N // 128, 128, (K // 128) * 128))


def _const_tables():
    ct = np.zeros((128, NCT), np.float32)
    q = np.arange(128, dtype=np.float32)[:, None]
    for j in range(2):
        i = np.arange(32, dtype=np.float32)[None, :]
        ct[:, C_D + j * 32:C_D + (j + 1) * 32] = 256.0 * (31 - i) + 128.0 * j + q - 255.0
    ct[:, C_D + 31] = 0.0
    ct[:, C_D + 63] = 0.0
    for kc in range(2):
        ct[:, C_KB + kc] = 128.0 * kc + q[:, 0] - 255.0
    p = np.arange(128, dtype=np.float32)[:, None]
    qq = np.arange(128, dtype=np.float32)[None, :]
    ct[:, C_DD:C_DD + 128] = np.where(p <= qq, p - qq, -1.0e9)
    ct[:, C_DF:C_DF + 128] = p - qq - 128.0
    return ct


def make_in_maps(inp):
    f32 = lambda a: np.ascontiguousarray(np.asarray(a, dtype=np.float32))
    x = f32(inp["x"])[0]
    xT = np.ascontiguousarray(x.T)
    w_in = f32(inp["w_in"])[0]
    ctab = _const_tables()
    shared = dict(
        xT=xT, ctab=ctab,
        w_g=_slabs(w_in[:, 10240:14336]),
        w_pa=_slabs(f32(inp["w_proj_attn"])[0]),
        w_pl=_slabs(f32(inp["w_proj_lru"])[0]),
        w_o=_slabs(f32(inp["w_out"])[0]),
        w_fg=_slabs(f32(inp["w_ffn_gate"])[0]),
        w_fu=_slabs(f32(inp["w_ffn_up"])[0]),
    )
    wfd = f32(inp["w_ffn_down"])[0]
    shared["w_fd"] = np.ascontiguousarray(
        wfd.reshape(FG, FGC, 128, KD, 128).transpose(0, 3, 2, 1, 4).reshape(FG * KD, 128, FGC * 128))
    n1, n2 = f32(inp["norm1_w"])[0], f32(inp["norm2_w"])[0]
    cw, cb = f32(inp["conv_w"])[0], f32(inp["conv_b"])[0]
    ba, bx, lam = f32(inp["b_rg_a"])[0], f32(inp["b_rg_x"])[0], f32(inp["lru_lambda"])[0]
    qw, kw = f32(inp["q_norm_w"])[0], f32(inp["k_norm_w"])[0]
    wra, wrx = f32(inp["w_rg_a"])[0], f32(inp["w_rg_x"])[0]
    maps = []
    for c in range(NC):
        cols = np.concatenate([np.arange(256) + base + 256 * c for base in (6144, 8192, 2048, 4096, 0)])
        w_a = np.ascontiguousarray(w_in[:, cols].reshape(KD, 128, 1280).transpose(1, 0, 2))
        vecs = np.zeros((128, NV), np.float32)
        vecs[:, V_N1:V_N1 + KD] = n1.reshape(KD, 128).T
        vecs[:, V_N2:V_N2 + KD] = n2.reshape(KD, 128).T
        for ch in range(2):
            sl = slice(256 * c + 128 * ch, 256 * c + 128 * ch + 128)
            for tap in range(4):
                vecs[:, V_CW + ch * 4 + tap] = cw[tap, sl]
            vecs[:, V_CB + ch] = cb[sl]
            vecs[:, V_BA + ch] = ba[sl]
            vecs[:, V_BX + ch] = bx[sl]
            vecs[:, V_LAM + ch] = lam[sl]
            vecs[:, V_HI + ch] = float(2 * c + ch + 1)
        vecs[:, V_QW] = qw
        vecs[:, V_KW] = kw
        w_rg = np.zeros((128, 2, 2, 128), np.float32)
        for ch in range(2):
            w_rg[:, 0, ch, :] = wra[2 * c + ch]
            w_rg[:, 1, ch, :] = wrx[2 * c + ch]
        m = dict(shared)
        m.update(w_a=w_a, vecs=vecs, w_rg=w_rg, xTs=np.ascontiguousarray(xT[:, c * TB:(c + 1) * TB]),
                 cidx=np.array([[c]], np.int32))
        maps.append(m)
    return maps


_NC_CACHE = {}


def kernel(**inputs):
    if "nc" not in _NC_CACHE:
        _NC_CACHE["nc"] = build_program(debug=True)
    nc = _NC_CACHE["nc"]
    in_maps = make_in_maps(inputs)
    res = run_bass_kernel_spmd(nc, in_maps, core_ids=list(range(NC)))
    out = np.empty((1, S, D), np.float32)
    for c in range(NC):
        out[0, c * TB:(c + 1) * TB, :] = res.results[c]["outT"].T
    return out
```

```python
import numpy as np
import concourse.bass as bass
import concourse.mybir as mybir
from concourse.bass_utils import run_bass_kernel_spmd

F32 = mybir.dt.float32
BF16 = mybir.dt.bfloat16
AF = mybir.ActivationFunctionType
ALU = mybir.AluOpType
AX = mybir.AxisListType


class Buf:
    __slots__ = ("name", "last_w", "readers")

    def __init__(self, name):
        self.name = name
        self.last_w = None
        self.readers = []


class Op:
    __slots__ = ("eng", "fn", "deps", "signal", "sem", "val", "inc", "idx", "extra")

    def __init__(self, eng, fn, idx):
        self.eng = eng
        self.fn = fn
        self.deps = []
        self.signal = False
        self.sem = None
        self.val = None
        self.inc = 1
        self.idx = idx
        self.extra = ()


class Prog:
    ENGS = ("pe", "act", "dve", "pool", "sp")

    def __init__(self, nc, same_engine_sync=True):
        self.nc = nc
        self.ops = []
        self.same_engine_sync = same_engine_sync
        self.dma_sem_count = {}

    def op(self, eng, fn, reads=(), writes=(), dma_sem=None, extra=()):
        o = Op(eng, fn, len(self.ops))
        o.extra = tuple(extra)
        deps = {}
        for b in reads:
            if b.last_w is not None:
                deps[b.last_w.idx] = b.last_w
        for b in writes:
            if b.last_w is not None:
                deps[b.last_w.idx] = b.last_w
            for r in b.readers:
                deps[r.idx] = r
        is_dma = dma_sem is not None
        for d in deps.values():
            d_is_dma = d.sem is not None
            if d.eng == eng and not d_is_dma and not is_dma:
                if eng == "pe" or not self.same_engine_sync:
                    continue
            if d is o:
                continue
            o.deps.append(d)
            d.signal = True
        if is_dma:
            o.sem = dma_sem
            o.inc = 16
            o.signal = True
            c = self.dma_sem_count.get(id(dma_sem), 0) + 16
            self.dma_sem_count[id(dma_sem)] = c
            o.val = c
        for b in reads:
            b.readers.append(o)
        for b in writes:
            b.last_w = o
            b.readers = []
        self.ops.append(o)
        return o

    def emit(self, block, eng_sems, final_sems=(), counters=None):
        nc = self.nc
        if counters is None:
            counters = {e: 0 for e in self.ENGS}
        for o in self.ops:
            if o.sem is None:
                if o.signal:
                    counters[o.eng] += 1
                    o.sem = eng_sems[o.eng]
                    o.val = counters[o.eng]
        by_eng = {e: [o for o in self.ops if o.eng == e] for e in self.ENGS}

        def run(engine, ops, is_last_owner):
            seen = {}
            for o in ops:
                need = {}
                for d in o.deps:
                    k = id(d.sem)
                    if k not in need or need[k][1] < d.val:
                        need[k] = (d.sem, d.val)
                for (xs, xv) in o.extra:
                    k = id(xs)
                    if k not in need or need[k][1] < xv:
                        need[k] = (xs, xv)
                for k, (s, v) in need.items():
                    if seen.get(k, 0) >= v:
                        continue
                    engine.wait_ge(s, v)
                    seen[k] = v
                ins = o.fn(engine)
                if o.signal:
                    ins.then_inc(o.sem, o.inc)
            if is_last_owner:
                for s in final_sems:
                    engine.wait_ge(s, self.dma_sem_count[id(s)])

        if by_eng["pe"]:
            @block.tensor
            def _(e):
                run(e, by_eng["pe"], False)
        if by_eng["act"]:
            @block.scalar
            def _(e):
                run(e, by_eng["act"], False)
        if by_eng["dve"]:
            @block.vector
            def _(e):
                run(e, by_eng["dve"], False)
        if by_eng["pool"]:
            @block.gpsimd
            def _(e):
                run(e, by_eng["pool"], False)

        @block.sync
        def _(e):
            run(e, by_eng["sp"], True)


S = 8192
D = 2048
H = 16
DH = 128
BLK = 256
NBLK = S // BLK
DFF = 5632
NFF = DFF // 128
KD = D // 128
TT = 512
NTILE = S // TT
NC = 8
TB = S // NC
EPS = 1e-6
SCALE = DH ** -0.5
FG = 4
FGC = NFF // FG
NEG = -1.0e30

V_N1 = 0
V_CW = 16
V_CB = 24
V_BA = 26
V_BX = 28
V_LAM = 30
V_QW = 32
V_KW = 33
V_N2 = 34
V_HI = 50
NV = 52
C_D = 0
C_KB = 64
C_DD = 66
C_DF = 194
NCT = 322


class Ctx:
    pass


def _ring(n):
    return [Buf(f"r{i}") for i in range(n)]


def build_program(debug=False):
    nc = bass.Bass("TRN2", target_bir_lowering=False)
    dt_i32 = mybir.dt.int32

    def din(name, shape, dt=F32):
        return nc.dram_tensor(name, list(shape), dt, kind="ExternalInput").ap()

    def dout(name, shape, dt=F32):
        return nc.dram_tensor(name, list(shape), dt, kind="ExternalOutput").ap()

    xT = din("xT", [D, S])
    xTs = din("xTs", [D, TB])
    w_a = din("w_a", [128, KD, 1280])
    vecs_d = din("vecs", [128, NV])
    ctab_d = din("ctab", [128, NCT])
    wrg_d = din("w_rg", [128, 2, 2, 128])
    cidx = din("cidx", [1, 1], dt_i32)
    w_g = din("w_g", [32, 128, KD * 128])
    w_pa = din("w_pa", [16, 128, KD * 128])
    w_pl = din("w_pl", [16, 128, KD * 128])
    w_o = din("w_o", [16, 128, KD * 128])
    w_fg = din("w_fg", [NFF, 128, KD * 128])
    w_fu = din("w_fu", [NFF, 128, KD * 128])
    w_fd = din("w_fd", [FG * 16, 128, FGC * 128])
    outT = dout("outT", [D, TB])

    ag_lru_in = nc.dram_tensor("ag_lru_in", [NC, 256, TB], BF16).ap()
    ag_att_in = nc.dram_tensor("ag_att_in", [NC, 256, TB], BF16).ap()
    ag_lru_out = nc.dram_tensor("ag_lru_out", [NC * NC, 256, TB], BF16).ap()
    ag_att_out = nc.dram_tensor("ag_att_out", [NC * NC, 256, TB], BF16).ap()

    dbg = {}
    if debug:
        dbg["lru"] = dout("dbg_lru", [NC, 256, TB], BF16)
        dbg["att"] = dout("dbg_att", [NC, 256, TB], BF16)
        dbg["kt"] = dout("dbg_kt", [128, S], BF16)
        dbg["qt"] = dout("dbg_qt", [128, S], BF16)
        dbg["v"] = dout("dbg_v", [128, 64, 130], BF16)
        dbg["sel"] = dout("dbg_sel", [128, 64, 32], BF16)
        dbg["merged"] = dout("dbg_merged", [128, KD, TB], BF16)
        dbg["x1"] = dout("dbg_x1", [128, KD, TB], F32)

    sem_state = {}
    eng_counts = {e: 0 for e in Prog.ENGS}

    import contextlib
    es = contextlib.ExitStack()
    with es:
        eng_sems = {e: es.enter_context(nc.semaphore(f"s_{e}")) for e in Prog.ENGS}
        NDS = 40
        dsems = [es.enter_context(nc.semaphore(f"d{i}")) for i in range(NDS)]
        cc_sem = es.enter_context(nc.semaphore("cc"))
        creg = es.enter_context(nc.sync.register("creg"))
        ps = es.enter_context(nc.psum_tensor("ps", [128, 8, 512], F32))
        psb = [Buf(f"ps{i}") for i in range(8)]

        vecs = es.enter_context(nc.sbuf_tensor("vecs_sb", [128, NV], F32))
        ctab = es.enter_context(nc.sbuf_tensor("ctab_sb", [128, NCT], F32))
        ident = es.enter_context(nc.sbuf_tensor("ident", [128, 128], BF16))
        ones = es.enter_context(nc.sbuf_tensor("ones", [128, 128], BF16))
        sm = es.enter_context(nc.sbuf_tensor("sm", [128, 32], F32))
        SM_SL, SM_NSL, SM_SP, SM_SP2, SM_NBA, SM_NBX, SM_KB = 0, 2, 4, 6, 8, 10, 12

        class Phase:
            def __init__(self, name):
                self.name = name
                self.P = Prog(nc)
                self.P.dma_sem_count = sem_state
                self.used = []
                self.next_ds = 0
                for b_ in psb:
                    b_.last_w, b_.readers = None, []

            def dsem(self):
                s = dsems[self.next_ds]
                self.next_ds += 1
                self.used.append(s)
                return s

            def finish(self):
                with nc.Block() as block:
                    self.P.emit(block, eng_sems, final_sems=[s for s in self.used if id(s) in sem_state],
                                counters=eng_counts)

        def sc(col, n=1):
            return sm[:, col:col + n]

        def vc(col, n=1):
            return vecs[:, col:col + n]

        ph = Phase("setup")
        P = ph.P
        b_vecs, b_ctab, b_sm, b_id, b_ones = Buf("vecs"), Buf("ctab"), Buf("sm"), Buf("id"), Buf("ones")
        P.op("sp", lambda e: e.dma_start(out=vecs[:], in_=vecs_d), writes=[b_vecs], dma_sem=ph.dsem())
        P.op("sp", lambda e: e.dma_start(out=ctab[:], in_=ctab_d), writes=[b_ctab], dma_sem=ph.dsem())
        P.op("sp", lambda e: e.reg_load(creg, cidx[0:1, 0:1]))
        P.op("dve", lambda e: e.memset(ident[:], 0.0), writes=[b_id])
        P.op("pool", lambda e: e.affine_select(out=ident[:], in_=ident[:], pattern=[[-1, 128]], compare_op=ALU.not_equal,
                                                fill=1.0, base=0, channel_multiplier=1), reads=[b_id], writes=[b_id])
        P.op("dve", lambda e: e.memset(ones[:], 1.0), writes=[b_ones])
        P.op("act", lambda e: e.activation(out=sc(SM_SL, 2), in_=vc(V_HI, 2), func=AF.Exp, scale=-0.5 * float(np.log(2.0))),
             reads=[b_vecs], writes=[b_sm])
        P.op("dve", lambda e: e.tensor_scalar(out=sc(SM_NSL, 2), in0=sc(SM_SL, 2), scalar1=-1.0, scalar2=None, op0=ALU.mult),
             reads=[b_sm], writes=[b_sm])
        P.op("act", lambda e: e.activation(out=sc(SM_SP, 2), in_=vc(V_LAM, 2), func=AF.Exp, scale=-1.0), reads=[b_vecs, b_sm], writes=[b_sm])
        P.op("act", lambda e: e.activation(out=sc(SM_SP, 2), in_=sc(SM_SP, 2), func=AF.Ln, bias=1.0, scale=1.0), reads=[b_sm], writes=[b_sm])
        P.op("dve", lambda e: e.tensor_scalar(out=sc(SM_SP2, 2), in0=sc(SM_SP, 2), scalar1=-16.0, scalar2=None, op0=ALU.mult), reads=[b_sm], writes=[b_sm])
        P.op("dve", lambda e: e.tensor_scalar(out=sc(SM_SP, 2), in0=sc(SM_SP, 2), scalar1=-8.0, scalar2=None, op0=ALU.mult), reads=[b_sm], writes=[b_sm])
        P.op("dve", lambda e: e.tensor_scalar(out=sc(SM_NBA, 2), in0=vc(V_BA, 2), scalar1=-1.0, scalar2=None, op0=ALU.mult), reads=[b_vecs, b_sm], writes=[b_sm])
        P.op("dve", lambda e: e.tensor_scalar(out=sc(SM_NBX, 2), in0=vc(V_BX, 2), scalar1=-1.0, scalar2=None, op0=ALU.mult), reads=[b_vecs, b_sm], writes=[b_sm])
        for hl in range(2):
            P.op("dve", lambda e, hl=hl: e.tensor_scalar(out=sc(SM_KB + 2 * hl, 2), in0=ctab[:, C_KB:C_KB + 2], scalar1=sc(SM_SL + hl),
                                                          scalar2=None, op0=ALU.mult), reads=[b_ctab, b_sm], writes=[b_sm])
        ph.finish()

        def phase_A(which, A=None):
            ph = Phase("A_" + which)
            P = ph.P
            if which == "qkv":
                issue_ag(ph, "lru", ag_lru_in, ag_lru_out)
            c0, ncol = (0, 512) if which == "lru" else (512, 768)
            nm = ncol // 128
            with contextlib.ExitStack() as st:
                T = lambda name, shape, dt: st.enter_context(nc.sbuf_tensor(which + "_" + name, shape, dt))
                wa = T("wa", [128, KD, ncol], BF16)
                NXF = 6 if which == "lru" else 4
                NWS = 2 if which == "lru" else 1
                wst = T("wst", [128, NWS, ncol], F32)
                xf = T("xf", [128, NXF, TT], F32)
                sqb = T("sqb", [128, 3, TT], BF16)
                xb = T("xb", [128, 2, KD, TT], BF16)
                rstd = T("rstd", [128, 2, TT], F32)
                lnt = T("lnt", [128, TT], F32)
                b_wa = [Buf(f"wa{k}") for k in range(KD)]
                b_wst = _ring(NWS)
                s_wst = [ph.dsem() for _ in range(NWS)]
                b_xf = _ring(NXF)
                s_xf = [ph.dsem() for _ in range(NXF)]
                b_sqb = _ring(3)
                b_xb = [[Buf(f"xb{p}_{k}") for k in range(KD)] for p in range(2)]
                b_rstd = _ring(2)
                b_lnt = Buf("lnt")
                BV = Buf("vecs_ro")

                for k in range(KD):
                    sl = k % NWS
                    P.op("sp", lambda e, k=k, sl=sl: e.dma_start(out=wst[:, sl, :], in_=w_a[:, k, c0:c0 + ncol]),
                         writes=[b_wst[sl]], dma_sem=s_wst[sl])
                    P.op("dve", lambda e, k=k, sl=sl: e.tensor_scalar(out=wa[:, k, :], in0=wst[:, sl, :], scalar1=vc(V_N1 + k),
                                                                     scalar2=None, op0=ALU.mult),
                         reads=[b_wst[sl]], writes=[b_wa[k]])

                xcnt = [0]
                sqcnt = [0]

                def x_tile(t):
                    par = t % 2
                    for k in range(KD):
                        sl = xcnt[0] % NXF
                        xcnt[0] += 1
                        sq = sqcnt[0] % 3
                        sqcnt[0] += 1
                        P.op("sp", lambda e, k=k, sl=sl: e.dma_start(out=xf[:, sl, :], in_=xT[k * 128:(k + 1) * 128, t * TT:(t + 1) * TT]),
                             writes=[b_xf[sl]], dma_sem=s_xf[sl])
                        P.op("act", lambda e, sl=sl, sq=sq: e.activation(out=sqb[:, sq, :], in_=xf[:, sl, :], func=AF.Square),
                             reads=[b_xf[sl]], writes=[b_sqb[sq]])
                        P.op("dve", lambda e, k=k, sl=sl: e.tensor_copy(out=xb[:, par, k, :], in_=xf[:, sl, :]),
                             reads=[b_xf[sl]], writes=[b_xb[par][k]])
                        P.op("pe", lambda e, k=k, sq=sq: e.matmul(ps[:, 0, :], lhsT=ones[:], rhs=sqb[:, sq, :], start=(k == 0), stop=(k == KD - 1)),
                             reads=[b_sqb[sq]], writes=[psb[0]])
                    P.op("act", lambda e: e.activation(out=lnt[:], in_=ps[:, 0, :], func=AF.Ln, scale=1.0 / D, bias=EPS),
                         reads=[psb[0]], writes=[b_lnt])
                    P.op("act", lambda e: e.activation(out=rstd[:, par, :], in_=lnt[:], func=AF.Exp, scale=-0.5),
                         reads=[b_lnt], writes=[b_rstd[par]])

                pcnt = [0]

                def proj(t, m):
                    par = t % 2
                    bank = 1 + pcnt[0] % 3
                    pcnt[0] += 1
                    for k in range(KD):
                        P.op("pe", lambda e, k=k, bank=bank: e.matmul(ps[:, bank, :], lhsT=wa[:, k, m * 128:(m + 1) * 128], rhs=xb[:, par, k, :],
                                                                       start=(k == 0), stop=(k == KD - 1)),
                             reads=[b_wa[k], b_xb[par][k]], writes=[psb[bank]])
                    return bank

                if which == "lru":
                    lru_body(ph, st, x_tile, proj, rstd, b_rstd)
                else:
                    qkv_body(ph, st, x_tile, proj, rstd, b_rstd, A)
                ph.finish()

        def run_interleaved(gens, hook_after=None, hook=None):
            alive = list(gens)
            step = 0
            while alive:
                for g in list(alive):
                    try:
                        next(g)
                    except StopIteration:
                        alive.remove(g)
                step += 1
                if hook is not None and step == hook_after:
                    hook()
                    hook = None
            if hook is not None:
                hook()

        def lru_body(ph, st, x_tile, proj, rstd, b_rstd):
            P = ph.P
            T = lambda name, shape, dt: st.enter_context(nc.sbuf_tensor(name, shape, dt))
            wrgf = T("wrgf", [128, 2, 2, 128], F32)
            wrg = T("wrg", [128, 2, 2, 128], BF16)
            xrt = T("xrt", [128, 2, TT + 3], F32)
            u = T("u", [128, 2, TT], F32)
            ub = T("ub", [128, 2, TT], BF16)
            ta = T("ta", [128, 2, TT], F32)
            tb = T("tb", [128, 2, TT], F32)
            tc_ = T("tc", [128, 2, TT], F32)
            hs = T("hs", [128, 2, TT], F32)
            yt = T("yt", [128, 2, TT], F32)
            y2 = T("y2", [128, 2, TT], F32)
            carry = T("carry", [128, 2], F32)
            ost = T("ost", [128, 4, TT], BF16)
            b_wrgf, b_wrg = Buf("wrgf"), Buf("wrg")
            b_xrt = [Buf("xrt0"), Buf("xrt1")]
            mk = lambda n: [Buf(n + "0"), Buf(n + "1")]
            b_u, b_ub, b_ta, b_tb, b_tc, b_hs, b_yt, b_y2 = [mk(n) for n in "u ub ta tb tc hs yt y2".split()]
            b_carry = [Buf("c0"), Buf("c1")]
            b_ost = _ring(4)
            s_ost = [ph.dsem() for _ in range(4)]
            P.op("sp", lambda e: e.dma_start(out=wrgf[:], in_=wrg_d), writes=[b_wrgf], dma_sem=ph.dsem())
            P.op("dve", lambda e: e.tensor_copy(out=wrg[:], in_=wrgf[:]), reads=[b_wrgf], writes=[b_wrg])
            for ch in range(2):
                P.op("dve", lambda e, ch=ch: e.memset(xrt[:, ch, :], 0.0), writes=[b_xrt[ch]])
            ocnt = [0]

            def chain(t, ch):
                par = t % 2
                gb0, gb1 = (4, 5) if ch == 0 else (6, 7)
                cw = lambda tap: vc(V_CW + ch * 4 + tap)
                U, UB, TA, TB_, TC, HS, YT, Y2 = u[:, ch, :], ub[:, ch, :], ta[:, ch, :], tb[:, ch, :], tc_[:, ch, :], hs[:, ch, :], yt[:, ch, :], y2[:, ch, :]
                bu, bub, bta, btb, btc, bhs, byt, by2 = b_u[ch], b_ub[ch], b_ta[ch], b_tb[ch], b_tc[ch], b_hs[ch], b_yt[ch], b_y2[ch]
                if t > 0:
                    P.op("dve", lambda e: e.tensor_copy(out=xrt[:, ch, 0:3], in_=xrt[:, ch, TT:TT + 3]), reads=[b_xrt[ch]], writes=[b_xrt[ch]])
                bank = proj(t, ch)
                P.op("dve", lambda e: e.tensor_tensor(out=xrt[:, ch, 3:TT + 3], in0=ps[:, bank, :], in1=rstd[:, par, :], op=ALU.mult),
                     reads=[psb[bank], b_rstd[par], b_xrt[ch]], writes=[b_xrt[ch]])
                yield
                P.op("dve", lambda e: e.tensor_scalar(out=U, in0=xrt[:, ch, 3:TT + 3], scalar1=cw(3), scalar2=vc(V_CB + ch), op0=ALU.mult, op1=ALU.add),
                     reads=[b_xrt[ch]], writes=[bu])
                for tap in (2, 1, 0):
                    P.op("dve", lambda e, tap=tap: e.scalar_tensor_tensor(out=U, in0=xrt[:, ch, tap:tap + TT], scalar=cw(tap), in1=U, op0=ALU.mult, op1=ALU.add),
                         reads=[b_xrt[ch], bu], writes=[bu])
                P.op("act", lambda e: e.activation(out=UB, in_=U, func=AF.Copy), reads=[bu], writes=[bub])
                yield
                P.op("pe", lambda e: e.matmul(ps[:, gb0, :], lhsT=wrg[:, 0, ch, :], rhs=UB, start=True, stop=True), reads=[b_wrg, bub], writes=[psb[gb0]])
                P.op("pe", lambda e: e.matmul(ps[:, gb1, :], lhsT=wrg[:, 1, ch, :], rhs=UB, start=True, stop=True), reads=[b_wrg, bub], writes=[psb[gb1]])
                ybank = proj(t, 2 + ch)
                yield
                P.op("act", lambda e: e.activation(out=TA, in_=ps[:, gb0, :], func=AF.Exp, scale=-1.0, bias=sc(SM_NBA + ch)), reads=[psb[gb0]], writes=[bta])
                P.op("act", lambda e: e.activation(out=TC, in_=ps[:, gb1, :], func=AF.Exp, scale=-1.0, bias=sc(SM_NBX + ch)), reads=[psb[gb1]], writes=[btc])
                P.op("dve", lambda e: e.tensor_tensor(out=YT, in0=ps[:, ybank, :], in1=rstd[:, par, :], op=ALU.mult), reads=[psb[ybank], b_rstd[par]], writes=[byt])
                yield
                P.op("act", lambda e: e.activation(out=TA, in_=TA, func=AF.Ln, scale=1.0, bias=1.0), reads=[bta], writes=[bta])
                P.op("act", lambda e: e.activation(out=TC, in_=TC, func=AF.Ln, scale=1.0, bias=1.0), reads=[btc], writes=[btc])
                yield
                P.op("act", lambda e: e.activation(out=TA, in_=TA, func=AF.Exp, scale=-1.0), reads=[bta], writes=[bta])
                P.op("act", lambda e: e.activation(out=Y2, in_=YT, func=AF.Square), reads=[byt], writes=[by2])
                yield
                P.op("act", lambda e: e.activation(out=TB_, in_=TA, func=AF.Exp, scale=sc(SM_SP + ch)), reads=[bta], writes=[btb])
                P.op("dve", lambda e: e.tensor_scalar(out=Y2, in0=Y2, scalar1=0.044715, scalar2=1.0, op0=ALU.mult, op1=ALU.add), reads=[by2], writes=[by2])
                yield
                P.op("act", lambda e: e.activation(out=TA, in_=TA, func=AF.Exp, scale=sc(SM_SP2 + ch)), reads=[bta], writes=[bta])
                P.op("dve", lambda e: e.tensor_tensor(out=Y2, in0=Y2, in1=YT, op=ALU.mult), reads=[by2, byt], writes=[by2])
                yield
                P.op("act", lambda e: e.activation(out=TA, in_=TA, func=AF.Ln, scale=-1.0, bias=1.0), reads=[bta], writes=[bta])
                yield
                P.op("dve", lambda e: e.scalar_tensor_tensor(out=TC, in0=TA, scalar=0.5, in1=TC, op0=ALU.mult, op1=ALU.subtract), reads=[bta, btc], writes=[btc])
                P.op("act", lambda e: e.activation(out=Y2, in_=Y2, func=AF.Exp, scale=-1.5957691216), reads=[by2], writes=[by2])
                yield
                P.op("act", lambda e: e.activation(out=TC, in_=TC, func=AF.Exp), reads=[btc], writes=[btc])
                yield
                P.op("dve", lambda e: e.tensor_tensor(out=TC, in0=TC, in1=U, op=ALU.mult), reads=[btc, bu], writes=[btc])
                P.op("act", lambda e: e.activation(out=Y2, in_=Y2, func=AF.Ln, scale=1.0, bias=1.0), reads=[by2], writes=[by2])
                yield
                init = 0.0 if t == 0 else carry[:, ch:ch + 1]
                P.op("dve", lambda e: e.tensor_tensor_scan(out=HS, data0=TB_, data1=TC, initial=init, op0=ALU.mult, op1=ALU.add),
                     reads=[btb, btc, b_carry[ch]], writes=[bhs])
                P.op("act", lambda e: e.activation(out=Y2, in_=Y2, func=AF.Exp, scale=-1.0), reads=[by2], writes=[by2])
                yield
                P.op("dve", lambda e: e.tensor_copy(out=carry[:, ch:ch + 1], in_=HS[:, TT - 1:TT]), reads=[bhs], writes=[b_carry[ch]])
                P.op("dve", lambda e: e.tensor_tensor(out=YT, in0=YT, in1=HS, op=ALU.mult), reads=[byt, bhs], writes=[byt])
                yield
                sl = ocnt[0] % 4
                ocnt[0] += 1
                P.op("dve", lambda e: e.tensor_tensor(out=ost[:, sl, :], in0=YT, in1=Y2, op=ALU.mult), reads=[byt, by2], writes=[b_ost[sl]])
                j, off = t // 2, (t % 2) * TT
                P.op("sp", lambda e: e.dma_start(out=ag_lru_in[j, ch * 128:(ch + 1) * 128, off:off + TT], in_=ost[:, sl, :]),
                     reads=[b_ost[sl]], dma_sem=s_ost[sl])

            x_tile(0)
            for t in range(NTILE):
                nxt = (lambda t=t: x_tile(t + 1)) if t + 1 < NTILE else None
                run_interleaved([chain(t, 0), chain(t, 1)], hook_after=1, hook=nxt)

        def qkv_body(ph, st, x_tile, proj, rstd, b_rstd, A):
            P = ph.P
            T = lambda name, shape, dt: st.enter_context(nc.sbuf_tensor(name, shape, dt))
            kt = T("kt", [128, 2, TT], F32)
            sqk = T("sqk", [128, 2, TT], BF16)
            lk = T("lk", [128, 2, TT], F32)
            qf = T("qf", [128, 2, TT], F32)
            vt = T("vt", [128, 2, TT], BF16)
            gsb = T("gsb", [128, 2, 32], F32)
            m8 = T("m8", [128, 2, 8], F32)
            mk = lambda n: [Buf(n + "0"), Buf(n + "1")]
            b_kt, b_sqk, b_lk, b_qf, b_vt, b_m8, b_gsb = [mk(n) for n in "kt sqk lk qf vt m8 gsb".split()]
            psv = ps[:, 6, :].bitcast(BF16)
            for hl in range(2):
                P.op("act", lambda e, hl=hl: e.activation(out=A.Ttab[:, hl, :], in_=ctab[:, C_D:C_D + 64], func=AF.Exp, scale=sc(SM_NSL + hl)), writes=[A.b_tab])
                P.op("dve", lambda e, hl=hl: e.tensor_scalar(out=A.Bdiag[:, hl, :], in0=ctab[:, C_DD:C_DD + 128], scalar1=sc(SM_SL + hl), scalar2=None, op0=ALU.mult), writes=[A.b_tab])
                P.op("dve", lambda e, hl=hl: e.tensor_scalar(out=A.Bfull[:, hl, :], in0=ctab[:, C_DF:C_DF + 128], scalar1=sc(SM_SL + hl), scalar2=None, op0=ALU.mult), writes=[A.b_tab])
                P.op("dve", lambda e, hl=hl: e.memset(A.kmean[:, hl, :], 0.0), writes=[A.b_kmean[hl]])
                P.op("dve", lambda e, hl=hl: e.memset(gsb[:, hl, :], NEG), writes=[b_gsb[hl]])
                P.op("dve", lambda e, hl=hl: e.memset(A.Vext[hl][:, :, 128:130], 1.0), writes=[A.b_V[hl]])
                P.op("dve", lambda e, hl=hl: e.memset(A.selb[hl][:, 0:2, :], 0.0), writes=[A.b_sel[hl]])

            def headnorm(src_bank, par, hl):
                nb_ = 4 if hl == 0 else 7
                P.op("dve", lambda e: e.tensor_tensor(out=kt[:, hl, :], in0=ps[:, src_bank, :], in1=rstd[:, par, :], op=ALU.mult),
                     reads=[psb[src_bank], b_rstd[par]], writes=[b_kt[hl]])
                P.op("act", lambda e: e.activation(out=sqk[:, hl, :], in_=kt[:, hl, :], func=AF.Square), reads=[b_kt[hl]], writes=[b_sqk[hl]])
                P.op("pe", lambda e: e.matmul(ps[:, nb_, :], lhsT=ones[:], rhs=sqk[:, hl, :], start=True, stop=True), reads=[b_sqk[hl]], writes=[psb[nb_]])
                P.op("act", lambda e: e.activation(out=lk[:, hl, :], in_=ps[:, nb_, :], func=AF.Ln, scale=1.0 / DH, bias=EPS), reads=[psb[nb_]], writes=[b_lk[hl]])
                P.op("act", lambda e: e.activation(out=lk[:, hl, :], in_=lk[:, hl, :], func=AF.Exp, scale=-0.5), reads=[b_lk[hl]], writes=[b_lk[hl]])

            def k_chain(t, hl):
                par = t % 2
                cols = slice(t * TT, (t + 1) * TT)
                bank = proj(t, 0 + hl)
                yield
                headnorm(bank, par, hl)
                yield
                P.op("dve", lambda e: e.scalar_tensor_tensor(out=A.KT[hl][:, cols], in0=kt[:, hl, :], scalar=vc(V_KW), in1=lk[:, hl, :], op0=ALU.mult, op1=ALU.mult),
                     reads=[b_kt[hl], b_lk[hl]], writes=[A.b_KT[hl]])
                for bb in range(2):
                    n = 2 * t + bb
                    P.op("dve", lambda e, n=n: e.reduce_sum(out=A.kmean[:, hl, n:n + 1], in_=A.KT[hl][:, n * BLK:(n + 1) * BLK], axis=AX.X),
                         reads=[A.b_KT[hl]], writes=[A.b_kmean[hl]])

            def v_chain(t, hl):
                par = t % 2
                bank = proj(t, 2 + hl)
                yield
                P.op("dve", lambda e: e.tensor_tensor(out=vt[:, hl, :], in0=ps[:, bank, :], in1=rstd[:, par, :], op=ALU.mult),
                     reads=[psb[bank], b_rstd[par]], writes=[b_vt[hl]])
                yield
                for c in range(4):
                    P.op("pe", lambda e, c=c: e.transpose(out=psv[:, c * 128:(c + 1) * 128], in_=vt[:, hl, c * 128:(c + 1) * 128], identity=ident[:]),
                         reads=[b_vt[hl]], writes=[psb[6]])
                P.op("act", lambda e: e.activation(out=A.Vext[hl][:, 4 * t:4 * t + 4, 0:128], in_=psv[:, 0:512].rearrange("p (c d) -> p c d", c=4), func=AF.Copy),
                     reads=[psb[6]], writes=[A.b_V[hl]])

            def q_chain(t, hl):
                par = t % 2
                cols = slice(t * TT, (t + 1) * TT)
                bank = proj(t, 4 + hl)
                yield
                headnorm(bank, par, hl)
                yield
                P.op("dve", lambda e: e.scalar_tensor_tensor(out=qf[:, hl, :], in0=kt[:, hl, :], scalar=vc(V_QW), in1=lk[:, hl, :], op0=ALU.mult, op1=ALU.mult),
                     reads=[b_kt[hl], b_lk[hl]], writes=[b_qf[hl]])
                P.op("act", lambda e: e.activation(out=A.QT[hl][:, cols], in_=qf[:, hl, :], func=AF.Copy), reads=[b_qf[hl]], writes=[A.b_QT[hl]])
                yield
                for c in range(4):
                    P.op("pe", lambda e, c=c: e.matmul(ps[:, 5, hl * 128 + c * 32:hl * 128 + (c + 1) * 32], lhsT=qf[:, hl, c * 128:(c + 1) * 128], rhs=A.kmean[:, hl, :], start=True, stop=True),
                         reads=[b_qf[hl], A.b_kmean[hl]], writes=[psb[5]])
                yield
                for c in range(4):
                    b = 2 * t + c // 2
                    cg = 4 * t + c
                    if b == 0:
                        continue
                    P.op("dve", lambda e, c=c, b=b: e.tensor_copy(out=gsb[:, hl, 0:b], in_=ps[:, 5, hl * 128 + c * 32:hl * 128 + c * 32 + b]),
                         reads=[psb[5], b_gsb[hl]], writes=[b_gsb[hl]])
                    P.op("dve", lambda e: e.max(out=m8[:, hl, :], in_=gsb[:, hl, :]), reads=[b_gsb[hl]], writes=[b_m8[hl]])
                    P.op("dve", lambda e, cg=cg: e.tensor_scalar(out=A.selb[hl][:, cg, :], in0=gsb[:, hl, :], scalar1=m8[:, hl, 2:3], scalar2=None, op0=ALU.is_ge),
                         reads=[b_gsb[hl], b_m8[hl]], writes=[A.b_sel[hl]])
                    yield

            x_tile(0)
            for t in range(NTILE):
                nxt = (lambda t=t: x_tile(t + 1)) if t + 1 < NTILE else None
                run_interleaved([k_chain(t, 0), k_chain(t, 1)], hook_after=1, hook=nxt)
                run_interleaved([v_chain(t, 0), v_chain(t, 1)])
                run_interleaved([q_chain(t, 0), q_chain(t, 1)])
            if debug:
                sdb = ph.dsem()
                P.op("sp", lambda e: e.dma_start(out=dbg["kt"], in_=A.KT[0][:]), reads=[A.b_KT[0]], dma_sem=sdb)
                P.op("sp", lambda e: e.dma_start(out=dbg["qt"], in_=A.QT[0][:]), reads=[A.b_QT[0]], dma_sem=sdb)
                P.op("sp", lambda e: e.dma_start(out=dbg["v"], in_=A.Vext[0][:]), reads=[A.b_V[0]], dma_sem=sdb)
                P.op("sp", lambda e: e.dma_start(out=dbg["sel"], in_=A.selb[0][:]), reads=[A.b_sel[0]], dma_sem=sdb)

        def phase_att(A):
            ph = Phase("att")
            P = ph.P
            NA = 4
            LA = 2
            with contextlib.ExitStack() as st:
                T = lambda name, shape, dt: st.enter_context(nc.sbuf_tensor(name, shape, dt))
                pt = T("pt", [128, 3, 512], BF16)
                tmpd = T("tmpd", [128, 2, 384], F32)
                acc = T("acc", [128, 2, NA, 2, 130], F32)
                mfac = T("mfac", [128, 2, 2, 32], F32)
                rden = T("rden", [128, 2, 2], F32)
                ob = T("ob", [128, 2, 2, 128], BF16)
                ast_ = T("attst", [128, 4, BLK], BF16)
                b_pt = [[Buf(f"pt{i}a"), Buf(f"pt{i}b")] for i in range(3)]
                b_tmpd = _ring(2)
                b_acc = [[[Buf(f"acc{p}{a}{j}") for j in range(2)] for a in range(NA)] for p in range(2)]
                b_mfac = _ring(2)
                b_rden = _ring(2)
                b_ob = _ring(2)
                b_ast = _ring(4)
                s_ast = [ph.dsem() for _ in range(4)]
                pst = ps[:, 6, :].bitcast(BF16)

                items = []
                for hl in range(2):
                    for b in range(NBLK):
                        items.append((hl, b, -1))
                        for n in range(b):
                            items.append((hl, b, n))
                N = len(items)
                blk_index = {}
                for hl in range(2):
                    for b in range(NBLK):
                        blk_index[(hl, b)] = hl * NBLK + b

                def stage_qk(i):
                    hl, b, n = items[i]
                    KT, QT = A.KT[hl], A.QT[hl]
                    bK, bQ = A.b_KT[hl], A.b_QT[hl]
                    kb0, kb1 = sc(SM_KB + 2 * hl), sc(SM_KB + 2 * hl + 1)
                    q0 = b * BLK
                    sb = i % 3
                    pi = i % 3
                    ip = blk_index[(hl, b)] % 2
                    if n < 0:
                        P.op("pe", lambda e: e.matmul(ps[:, sb, 0:128], lhsT=KT[:, q0:q0 + 128], rhs=QT[:, q0:q0 + 128], start=True, stop=True), reads=[bK, bQ], writes=[psb[sb]])
                        P.op("pe", lambda e: e.matmul(ps[:, sb, 128:256], lhsT=KT[:, q0 + 128:q0 + 256], rhs=QT[:, q0 + 128:q0 + 256], start=True, stop=True), reads=[bK, bQ], writes=[psb[sb]])
                        P.op("pe", lambda e: e.matmul(ps[:, sb, 256:384], lhsT=KT[:, q0:q0 + 128], rhs=QT[:, q0 + 128:q0 + 256], start=True, stop=True), reads=[bK, bQ], writes=[psb[sb]])
                        for r, tabl in ((0, A.Bdiag), (1, A.Bdiag), (2, A.Bfull)):
                            P.op("dve", lambda e, r=r, tabl=tabl: e.scalar_tensor_tensor(
                                out=tmpd[:, ip, r * 128:(r + 1) * 128], in0=ps[:, sb, r * 128:(r + 1) * 128], scalar=SCALE, in1=tabl[:, hl, :], op0=ALU.mult, op1=ALU.add),
                                reads=[psb[sb], A.b_tab], writes=[b_tmpd[ip]])
                        P.op("act", lambda e: e.activation(out=pt[:, pi, 0:384], in_=tmpd[:, ip, :], func=AF.Exp), reads=[b_tmpd[ip]], writes=b_pt[pi])
                    else:
                        k0 = n * BLK
                        P.op("pe", lambda e: e.matmul(ps[:, sb, 0:256], lhsT=KT[:, k0:k0 + 128], rhs=QT[:, q0:q0 + 256], start=True, stop=True), reads=[bK, bQ], writes=[psb[sb]])
                        P.op("pe", lambda e: e.matmul(ps[:, sb, 256:512], lhsT=KT[:, k0 + 128:k0 + 256], rhs=QT[:, q0:q0 + 256], start=True, stop=True), reads=[bK, bQ], writes=[psb[sb]])
                        P.op("act", lambda e: e.activation(out=pt[:, pi, 0:256], in_=ps[:, sb, 0:256], func=AF.Exp, scale=SCALE, bias=kb0), reads=[psb[sb]], writes=[b_pt[pi][0]])
                        P.op("act", lambda e: e.activation(out=pt[:, pi, 256:512], in_=ps[:, sb, 256:512], func=AF.Exp, scale=SCALE, bias=kb1), reads=[psb[sb]], writes=[b_pt[pi][1]])

                def stage_pv(i):
                    hl, b, n = items[i]
                    V, bV = A.Vext[hl], A.b_V[hl]
                    pi = i % 3
                    ob_ = 3 + i % 3
                    bi = blk_index[(hl, b)]
                    ip = bi % 2
                    if n < 0:
                        if b > 0:
                            for j in range(2):
                                P.op("dve", lambda e, j=j: e.tensor_tensor(
                                    out=mfac[:, ip, j, 0:b], in0=A.selb[hl][:, 2 * b + j, 0:b], in1=A.Ttab[:, hl, j * 32 + 31 - b:j * 32 + 31], op=ALU.mult),
                                    reads=[A.b_sel[hl], A.b_tab], writes=[b_mfac[ip]])
                        P.op("pe", lambda e: e.matmul(ps[:, ob_, 0:129], lhsT=pt[:, pi, 0:128], rhs=V[:, 2 * b, 0:129], start=True, stop=True), reads=b_pt[pi] + [bV], writes=[psb[ob_]])
                        P.op("pe", lambda e: e.matmul(ps[:, ob_, 256:385], lhsT=pt[:, pi, 256:384], rhs=V[:, 2 * b, 0:129], start=True, stop=False), reads=b_pt[pi] + [bV], writes=[psb[ob_]])
                        P.op("pe", lambda e: e.matmul(ps[:, ob_, 256:385], lhsT=pt[:, pi, 128:256], rhs=V[:, 2 * b + 1, 0:129], start=False, stop=True), reads=b_pt[pi] + [bV], writes=[psb[ob_]])
                        for j in range(2):
                            P.op("dve", lambda e, j=j: e.tensor_copy(out=acc[:, ip, 0, j, 0:129], in_=ps[:, ob_, j * 256:j * 256 + 129]),
                                 reads=[psb[ob_]], writes=[b_acc[ip][0][j]])
                    else:
                        for j in range(2):
                            P.op("pe", lambda e, j=j: e.matmul(ps[:, ob_, j * 256:j * 256 + 129], lhsT=pt[:, pi, j * 128:(j + 1) * 128], rhs=V[:, 2 * n, 0:129], start=True, stop=False),
                                 reads=[b_pt[pi][0], bV], writes=[psb[ob_]])
                            P.op("pe", lambda e, j=j: e.matmul(ps[:, ob_, j * 256:j * 256 + 129], lhsT=pt[:, pi, 256 + j * 128:256 + (j + 1) * 128], rhs=V[:, 2 * n + 1, 0:129], start=False, stop=True),
                                 reads=[b_pt[pi][1], bV], writes=[psb[ob_]])
                        a = (n + 1) % NA
                        first = (n + 1) < NA
                        for j in range(2):
                            if first:
                                P.op("dve", lambda e, j=j: e.tensor_scalar(out=acc[:, ip, a, j, 0:129], in0=ps[:, ob_, j * 256:j * 256 + 129], scalar1=mfac[:, ip, j, n:n + 1], scalar2=None, op0=ALU.mult),
                                     reads=[psb[ob_], b_mfac[ip]], writes=[b_acc[ip][a][j]])
                            else:
                                P.op("dve", lambda e, j=j: e.scalar_tensor_tensor(
                                    out=acc[:, ip, a, j, 0:129], in0=ps[:, ob_, j * 256:j * 256 + 129], scalar=mfac[:, ip, j, n:n + 1], in1=acc[:, ip, a, j, 0:129], op0=ALU.mult, op1=ALU.add),
                                    reads=[psb[ob_], b_mfac[ip], b_acc[ip][a][j]], writes=[b_acc[ip][a][j]])
                    if n == b - 1:
                        nacc = min(NA, b + 1)
                        for j in range(2):
                            for a in range(1, nacc):
                                P.op("dve", lambda e, j=j, a=a: e.tensor_tensor(out=acc[:, ip, 0, j, 0:129], in0=acc[:, ip, 0, j, 0:129], in1=acc[:, ip, a, j, 0:129], op=ALU.add),
                                     reads=[b_acc[ip][0][j], b_acc[ip][a][j]], writes=[b_acc[ip][0][j]])
                            P.op("dve", lambda e, j=j: e.reciprocal(out=rden[:, ip, j:j + 1], in_=acc[:, ip, 0, j, 128:129]), reads=[b_acc[ip][0][j], b_rden[ip]], writes=[b_rden[ip]])
                            P.op("dve", lambda e, j=j: e.tensor_scalar(out=ob[:, ip, j, :], in0=acc[:, ip, 0, j, 0:128], scalar1=rden[:, ip, j:j + 1], scalar2=None, op0=ALU.mult),
                                 reads=[b_acc[ip][0][j], b_rden[ip], b_ob[ip]], writes=[b_ob[ip]])
                        for j in range(2):
                            P.op("pe", lambda e, j=j: e.transpose(out=pst[:, j * 128:(j + 1) * 128], in_=ob[:, ip, j, :], identity=ident[:]), reads=[b_ob[ip]], writes=[psb[6]])
                        ai = bi % 4
                        P.op("act", lambda e: e.activation(out=ast_[:, ai, :], in_=pst[:, 0:BLK], func=AF.Copy), reads=[psb[6]], writes=[b_ast[ai]])
                        jd, off = b // 4, (b % 4) * BLK
                        P.op("sp", lambda e: e.dma_start(out=ag_att_in[jd, hl * 128:(hl + 1) * 128, off:off + BLK], in_=ast_[:, ai, :]),
                             reads=[b_ast[ai]], dma_sem=s_ast[ai])

                for i in range(-LA, N):
                    if i + LA < N:
                        stage_qk(i + LA)
                    if i >= 0:
                        stage_pv(i)
                ph.finish()

        cc_vals = {}

        def issue_ag(ph, name, src, dst):
            o = ph.P.op("pool", lambda e: e.collective_compute("AllGather", ALU.bypass, replica_groups=[list(range(NC))],
                                                                ins=[src.rearrange("j f t -> (j f) t")], outs=[dst.rearrange("a f t -> (a f) t")]))
            v = sem_state.get(id(cc_sem), 0) + 1
            sem_state[id(cc_sem)] = v
            o.sem, o.inc, o.val, o.signal = cc_sem, 1, v, True
            cc_vals[name] = v

        def phase_B():
            with contextlib.ExitStack() as so:
                TO = lambda name, shape, dt: so.enter_context(nc.sbuf_tensor(name, shape, dt))
                NW = 7
                wr = TO("wr", [128, NW, KD * 128], BF16)
                merged = TO("merged", [128, KD, TB], BF16)
                rstd1 = TO("rstd1", [128, TB], F32)
                lnb = TO("lnb", [128, TB], F32)

                class WStream:
                    def __init__(self, ph, jobs, pf=6):
                        self.ph, self.jobs, self.pf = ph, jobs, pf
                        self.b = _ring(NW)
                        self.s = [ph.dsem() for _ in range(NW)]
                        self.issued = 0

                    def get(self, i):
                        while self.issued < min(len(self.jobs), i + 1 + self.pf):
                            src, L = self.jobs[self.issued]
                            sl = self.issued % NW
                            self.ph.P.op("pool", lambda e, src=src, L=L, sl=sl: e.dma_start(out=wr[:, sl, 0:L], in_=src),
                                         writes=[self.b[sl]], dma_sem=self.s[sl])
                            self.issued += 1
                        sl = i % NW
                        return sl, self.b[sl]

                bankc = [0]

                def nb():
                    b = bankc[0] % 8
                    bankc[0] += 1
                    return b

                ph = Phase("B12")
                P = ph.P
                issue_ag(ph, "att", ag_att_in, ag_att_out)
                with contextlib.ExitStack() as st:
                    T = lambda name, shape, dt: st.enter_context(nc.sbuf_tensor(name, shape, dt))
                    attlru = T("attlru", [128, 32, TB], BF16)
                    hb = T("hb", [128, KD, TB], BF16)
                    xring = T("xring", [128, 2, TB], F32)
                    sq1 = T("sq1", [128, 2, TB], BF16)
                    tmp = T("tmpB", [128, 2, 4, TT], F32)
                    b_al = [Buf(f"al{i}") for i in range(16)]
                    b_hb = [Buf(f"hb{k}") for k in range(KD)]
                    b_xr = _ring(2)
                    s_xr = [ph.dsem() for _ in range(2)]
                    b_sq1 = _ring(2)
                    b_tmp = [[Buf(f"t{p}{i}") for i in range(4)] for p in range(2)]
                    b_rstd1, b_lnb = Buf("rstd1"), Buf("lnb")
                    b_mg = [Buf(f"mg{k}") for k in range(KD)]
                    jobs = []
                    for m in range(KD):
                        jobs += [(w_g[m], KD * 128), (w_g[16 + m], KD * 128), (w_pl[m], KD * 128), (w_pa[m], KD * 128)]
                    W = WStream(ph, jobs, pf=3)
                    W.get(0)
                    s_al = ph.dsem()
                    vcache = {}
                    for name, base, src in (("lru", 16, ag_lru_out), ("att", 0, ag_att_out)):
                        src2 = src.rearrange("(c j) (h p) t -> j c h p t", j=NC, p=128)
                        for c in range(NC):
                            for h in range(2):
                                def ld(e, c=c, h=h, base=base, src2=src2):
                                    if "v" not in vcache:
                                        vcache["v"] = e.snap(creg, min_val=0, max_val=NC - 1)
                                    return e.dma_start(out=attlru[:, base + 2 * c + h, :], in_=src2[bass.ds(vcache["v"], 1), c, h].squeeze(0))
                                P.op("sp", ld, dma_sem=s_al, extra=[(cc_sem, cc_vals[name])])
                        P.op("sp", lambda e: e.nop(), writes=b_al[(base // 16) * 8:(base // 16) * 8 + 8], extra=[(s_al, sem_state[id(s_al)])])
                    for k in range(KD):
                        sl = k % 2
                        P.op("sp", lambda e, k=k, sl=sl: e.dma_start(out=xring[:, sl, :], in_=xTs[k * 128:(k + 1) * 128, :]), writes=[b_xr[sl]], dma_sem=s_xr[sl])
                        P.op("act", lambda e, sl=sl: e.activation(out=sq1[:, sl, :], in_=xring[:, sl, :], func=AF.Square), reads=[b_xr[sl]], writes=[b_sq1[sl]])
                        P.op("dve", lambda e, k=k, sl=sl: e.tensor_scalar(out=hb[:, k, :], in0=xring[:, sl, :], scalar1=vc(V_N1 + k), scalar2=None, op0=ALU.mult),
                             reads=[b_xr[sl]], writes=[b_hb[k]])
                        for hf in range(2):
                            P.op("pe", lambda e, k=k, sl=sl, hf=hf: e.matmul(ps[:, hf, :], lhsT=ones[:], rhs=sq1[:, sl, hf * TT:(hf + 1) * TT], start=(k == 0), stop=(k == KD - 1)),
                                 reads=[b_sq1[sl]], writes=[psb[hf]])
                    for hf in range(2):
                        P.op("act", lambda e, hf=hf: e.activation(out=lnb[:, hf * TT:(hf + 1) * TT], in_=ps[:, hf, :], func=AF.Ln, scale=1.0 / D, bias=EPS), reads=[psb[hf]], writes=[b_lnb])
                    P.op("act", lambda e: e.activation(out=rstd1[:], in_=lnb[:], func=AF.Exp, scale=-0.5), reads=[b_lnb], writes=[b_rstd1])
                    bankc[0] = 2
                    def b2_step(m, hf, tp, slots):
                        if True:
                            hs_ = slice(hf * TT, (hf + 1) * TT)
                            banks = []
                            for i, (sl, bw) in enumerate(slots):
                                bk = nb()
                                banks.append(bk)
                                for k in range(KD):
                                    if i < 2:
                                        rhs, rb = hb[:, k, hs_], b_hb[k]
                                    elif i == 2:
                                        rhs, rb = attlru[:, 16 + k, hs_], b_al[8 + k // 2]
                                    else:
                                        rhs, rb = attlru[:, k, hs_], b_al[k // 2]
                                    P.op("pe", lambda e, sl=sl, k=k, bk=bk, rhs=rhs: e.matmul(ps[:, bk, :], lhsT=wr[:, sl, k * 128:(k + 1) * 128], rhs=rhs, start=(k == 0), stop=(k == KD - 1)),
                                         reads=[bw, rb], writes=[psb[bk]])
                            for i in range(2):
                                gb, pb = banks[i], banks[3 - i]
                                P.op("dve", lambda e, gb=gb, tp=tp, i=i: e.tensor_tensor(out=tmp[:, tp, i, :], in0=ps[:, gb, :], in1=rstd1[:, hs_], op=ALU.mult),
                                     reads=[psb[gb], b_rstd1], writes=[b_tmp[tp][i]])
                                P.op("act", lambda e, tp=tp, i=i: e.activation(out=tmp[:, tp, i, :], in_=tmp[:, tp, i, :], func=AF.Tanh, scale=0.5), reads=[b_tmp[tp][i]], writes=[b_tmp[tp][i]])
                                P.op("dve", lambda e, pb=pb, tp=tp, i=i: e.scalar_tensor_tensor(out=tmp[:, tp, 2 + i, :], in0=tmp[:, tp, i, :], scalar=1.0, in1=ps[:, pb, :], op0=ALU.add, op1=ALU.mult),
                                     reads=[b_tmp[tp][i], psb[pb]], writes=[b_tmp[tp][2 + i]])
                            P.op("dve", lambda e, tp=tp: e.tensor_tensor(out=tmp[:, tp, 2, :], in0=tmp[:, tp, 2, :], in1=tmp[:, tp, 3, :], op=ALU.add),
                                 reads=[b_tmp[tp][2], b_tmp[tp][3]], writes=[b_tmp[tp][2]])
                            P.op("act", lambda e, tp=tp, m=m: e.activation(out=merged[:, m, hs_], in_=tmp[:, tp, 2, :], func=AF.Copy, scale=0.5), reads=[b_tmp[tp][2]], writes=[b_mg[m]])

                    it = 0
                    for m in range(KD):
                        slots = [W.get(4 * m + i) for i in range(4)]
                        for hf in range(2):
                            b2_step(m, hf, it % 2, slots)
                            it += 1
                    if debug:
                        P.op("sp", lambda e: e.dma_start(out=dbg["merged"], in_=merged[:]), reads=b_mg, dma_sem=ph.dsem())
                        sdb = ph.dsem()
                        P.op("sp", lambda e: e.dma_start(out=dbg["lru"], in_=ag_lru_in), dma_sem=sdb)
                        P.op("sp", lambda e: e.dma_start(out=dbg["att"], in_=ag_att_in), dma_sem=sdb)
                    ph.finish()

                ph = Phase("B36")
                P = ph.P
                with contextlib.ExitStack() as st:
                    T = lambda name, shape, dt: st.enter_context(nc.sbuf_tensor(name, shape, dt))
                    x1 = T("x1", [128, KD, TB], F32)
                    h2b = T("h2b", [128, KD, TB], BF16)
                    actT = T("actT", [128, FGC, TB], BF16)
                    sq2 = T("sq2", [128, 2, TB], BF16)
                    tmp = T("tmpF", [128, 2, 2, TT], F32)
                    rstd2 = T("rstd2", [128, TB], F32)
                    b_x1 = [[Buf(f"x1_{m}_{h}") for h in range(2)] for m in range(KD)]
                    b_h2 = [Buf(f"h2{k}") for k in range(KD)]
                    b_act = [[Buf(f"a{f}_{h}") for h in range(2)] for f in range(FGC)]
                    b_sq2 = _ring(2)
                    b_tmp = [[Buf(f"tf{p}{i}") for i in range(2)] for p in range(2)]
                    b_rstd2, b_lnb = Buf("rstd2"), Buf("lnb")
                    b_mg = Buf("mg")
                    s_x1 = ph.dsem()
                    jobs = [(w_o[m], KD * 128) for m in range(KD)]
                    for g in range(FG):
                        for f in range(FGC):
                            jobs += [(w_fg[g * FGC + f], KD * 128), (w_fu[g * FGC + f], KD * 128)]
                        jobs += [(w_fd[g * 16 + m], FGC * 128) for m in range(KD)]
                    W = WStream(ph, jobs, pf=4)
                    W.get(0)
                    for m in range(KD):
                        P.op("sp", lambda e, m=m: e.dma_start(out=x1[:, m, :], in_=xTs[m * 128:(m + 1) * 128, :]), dma_sem=s_x1)
                    P.op("sp", lambda e: e.nop(), writes=[b for bb in b_x1 for b in bb], extra=[(s_x1, sem_state[id(s_x1)])])
                    ji = 0
                    for m in range(KD):
                        sl, bw = W.get(ji)
                        ji += 1
                        for hf in range(2):
                            hs_ = slice(hf * TT, (hf + 1) * TT)
                            bk = nb()
                            for k in range(KD):
                                P.op("pe", lambda e, sl=sl, k=k, bk=bk, hs_=hs_: e.matmul(ps[:, bk, :], lhsT=wr[:, sl, k * 128:(k + 1) * 128], rhs=merged[:, k, hs_], start=(k == 0), stop=(k == KD - 1)),
                                     reads=[bw, b_mg], writes=[psb[bk]])
                            P.op("dve", lambda e, m=m, bk=bk, hs_=hs_: e.tensor_tensor(out=x1[:, m, hs_], in0=x1[:, m, hs_], in1=ps[:, bk, :], op=ALU.add),
                                 reads=[psb[bk], b_x1[m][hf]], writes=[b_x1[m][hf]])
                    if debug:
                        P.op("sp", lambda e: e.dma_start(out=dbg["x1"], in_=x1[:]), reads=[b for bb in b_x1 for b in bb], dma_sem=ph.dsem())
                    sb0, sb1 = nb(), nb()
                    for k in range(KD):
                        sl = k % 2
                        P.op("act", lambda e, k=k, sl=sl: e.activation(out=sq2[:, sl, :], in_=x1[:, k, :], func=AF.Square), reads=b_x1[k], writes=[b_sq2[sl]])
                        for hf, bk in ((0, sb0), (1, sb1)):
                            P.op("pe", lambda e, k=k, sl=sl, hf=hf, bk=bk: e.matmul(ps[:, bk, :], lhsT=ones[:], rhs=sq2[:, sl, hf * TT:(hf + 1) * TT], start=(k == 0), stop=(k == KD - 1)),
                                 reads=[b_sq2[sl]], writes=[psb[bk]])
                    for hf, bk in ((0, sb0), (1, sb1)):
                        P.op("act", lambda e, hf=hf, bk=bk: e.activation(out=lnb[:, hf * TT:(hf + 1) * TT], in_=ps[:, bk, :], func=AF.Ln, scale=1.0 / D, bias=EPS), reads=[psb[bk]], writes=[b_lnb])
                    P.op("act", lambda e: e.activation(out=rstd2[:], in_=lnb[:], func=AF.Exp, scale=-0.5), reads=[b_lnb], writes=[b_rstd2])
                    for k in range(KD):
                        P.op("dve", lambda e, k=k: e.scalar_tensor_tensor(out=h2b[:, k, :], in0=x1[:, k, :], scalar=vc(V_N2 + k), in1=rstd2[:], op0=ALU.mult, op1=ALU.mult),
                             reads=b_x1[k] + [b_rstd2], writes=[b_h2[k]])
                    it = 0
                    for g in range(FG):
                        for f in range(FGC):
                            (slg, bwg), (slu, bwu) = W.get(ji), W.get(ji + 1)
                            ji += 2
                            for hf in range(2):
                                tp = it % 2
                                it += 1
                                hs_ = slice(hf * TT, (hf + 1) * TT)
                                bg, bu = nb(), nb()
                                for sl, bw, bk in ((slg, bwg, bg), (slu, bwu, bu)):
                                    for k in range(KD):
                                        P.op("pe", lambda e, sl=sl, k=k, bk=bk, hs_=hs_: e.matmul(ps[:, bk, :], lhsT=wr[:, sl, k * 128:(k + 1) * 128], rhs=h2b[:, k, hs_], start=(k == 0), stop=(k == KD - 1)),
                                             reads=[bw, b_h2[k]], writes=[psb[bk]])
                                P.op("act", lambda e, tp=tp, bg=bg: e.activation(out=tmp[:, tp, 0, :], in_=ps[:, bg, :], func=AF.Tanh, scale=0.5), reads=[psb[bg]], writes=[b_tmp[tp][0]])
                                P.op("dve", lambda e, tp=tp, bg=bg: e.scalar_tensor_tensor(out=tmp[:, tp, 1, :], in0=tmp[:, tp, 0, :], scalar=1.0, in1=ps[:, bg, :], op0=ALU.add, op1=ALU.mult),
                                     reads=[b_tmp[tp][0], psb[bg]], writes=[b_tmp[tp][1]])
                                P.op("dve", lambda e, tp=tp, bu=bu, f=f, hs_=hs_: e.scalar_tensor_tensor(out=actT[:, f, hs_], in0=tmp[:, tp, 1, :], scalar=0.5, in1=ps[:, bu, :], op0=ALU.mult, op1=ALU.mult),
                                     reads=[b_tmp[tp][1], psb[bu]], writes=[b_act[f][hf]])
                        for m in range(KD):
                            sl, bw = W.get(ji)
                            ji += 1
                            for hf in range(2):
                                hs_ = slice(hf * TT, (hf + 1) * TT)
                                bk = nb()
                                for f in range(FGC):
                                    P.op("pe", lambda e, sl=sl, f=f, bk=bk, hs_=hs_: e.matmul(ps[:, bk, :], lhsT=wr[:, sl, f * 128:(f + 1) * 128], rhs=actT[:, f, hs_], start=(f == 0), stop=(f == FGC - 1)),
                                         reads=[bw, b_act[f][hf]], writes=[psb[bk]])
                                P.op("dve", lambda e, m=m, bk=bk, hs_=hs_: e.tensor_tensor(out=x1[:, m, hs_], in0=x1[:, m, hs_], in1=ps[:, bk, :], op=ALU.add),
                                     reads=[psb[bk], b_x1[m][hf]], writes=[b_x1[m][hf]])
                    s_out = ph.dsem()
                    for m in range(KD):
                        P.op("sp", lambda e, m=m: e.dma_start(out=outT[m * 128:(m + 1) * 128, :], in_=x1[:, m, :]), reads=b_x1[m], dma_sem=s_out)
                    ph.finish()

        phase_A("lru")
        with contextlib.ExitStack() as sa:
            A = Ctx()
            TA = lambda name, shape, dt: sa.enter_context(nc.sbuf_tensor(name, shape, dt))
            A.QT = [TA(f"QT{h}", [128, S], BF16) for h in range(2)]
            A.KT = [TA(f"KT{h}", [128, S], BF16) for h in range(2)]
            A.Vext = [TA(f"V{h}", [128, 64, 130], BF16) for h in range(2)]
            A.selb = [TA(f"sel{h}", [128, 64, 32], BF16) for h in range(2)]
            A.kmean = TA("kmean", [128, 2, 32], F32)
            A.Ttab = TA("Ttab", [128, 2, 64], F32)
            A.Bdiag = TA("Bdiag", [128, 2, 128], F32)
            A.Bfull = TA("Bfull", [128, 2, 128], F32)
            A.b_QT = [Buf("QT0"), Buf("QT1")]
            A.b_KT = [Buf("KT0"), Buf("KT1")]
            A.b_V = [Buf("V0"), Buf("V1")]
            A.b_sel = [Buf("sel0"), Buf("sel1")]
            A.b_kmean = [Buf("km0"), Buf("km1")]
            A.b_tab = Buf("tab")
            phase_A("qkv", A)
            for lst in (A.b_QT, A.b_KT, A.b_V, A.b_sel, A.b_kmean, [A.b_tab]):
                for b_ in lst:
                    b_.last_w, b_.readers = None, []
            phase_att(A)
        phase_B()
    return nc


def _slabs(W):
    K, N = W.shape
    return np.ascontiguousarray(W.reshape(K // 128, 128, N // 128, 128).transpose(2, 1, 0, 3).reshape(N // 128, 128, (K // 128) * 128))


def _const_tables():
    ct = np.zeros((128, NCT), np.float32)
    q = np.arange(128, dtype=np.float32)[:, None]
    for j in range(2):
        i = np.arange(32, dtype=np.float32)[None, :]
        ct[:, C_D + j * 32:C_D + (j + 1) * 32] = 256.0 * (31 - i) + 128.0 * j + q - 255.0
    ct[:, C_D + 31] = 0.0
    ct[:, C_D + 63] = 0.0
    for kc in range(2):
        ct[:, C_KB + kc] = 128.0 * kc + q[:, 0] - 255.0
    p = np.arange(128, dtype=np.float32)[:, None]
    qq = np.arange(128, dtype=np.float32)[None, :]
    ct[:, C_DD:C_DD + 128] = np.where(p <= qq, p - qq, -1.0e9)
    ct[:, C_DF:C_DF + 128] = p - qq - 128.0
    return ct


def make_in_maps(inp):
    f32 = lambda a: np.ascontiguousarray(np.asarray(a, dtype=np.float32))
    x = f32(inp["x"])[0]
    xT = np.ascontiguousarray(x.T)
    w_in = f32(inp["w_in"])[0]
    ctab = _const_tables()
    shared = dict(
        xT=xT, ctab=ctab,
        w_g=_slabs(w_in[:, 10240:14336]),
        w_pa=_slabs(f32(inp["w_proj_attn"])[0]),
        w_pl=_slabs(f32(inp["w_proj_lru"])[0]),
        w_o=_slabs(f32(inp["w_out"])[0]),
        w_fg=_slabs(f32(inp["w_ffn_gate"])[0]),
        w_fu=_slabs(f32(inp["w_ffn_up"])[0]),
    )
    wfd = f32(inp["w_ffn_down"])[0]
    shared["w_fd"] = np.ascontiguousarray(
        wfd.reshape(FG, FGC, 128, KD, 128).transpose(0, 3, 2, 1, 4).reshape(FG * KD, 128, FGC * 128))
    n1, n2 = f32(inp["norm1_w"])[0], f32(inp["norm2_w"])[0]
    cw, cb = f32(inp["conv_w"])[0], f32(inp["conv_b"])[0]
    ba, bx, lam = f32(inp["b_rg_a"])[0], f32(inp["b_rg_x"])[0], f32(inp["lru_lambda"])[0]
    qw, kw = f32(inp["q_norm_w"])[0], f32(inp["k_norm_w"])[0]
    wra, wrx = f32(inp["w_rg_a"])[0], f32(inp["w_rg_x"])[0]
    maps = []
    for c in range(NC):
        cols = np.concatenate([np.arange(256) + base + 256 * c for base in (6144, 8192, 2048, 4096, 0)])
        w_a = np.ascontiguousarray(w_in[:, cols].reshape(KD, 128, 1280).transpose(1, 0, 2))
        vecs = np.zeros((128, NV), np.float32)
        vecs[:, V_N1:V_N1 + KD] = n1.reshape(KD, 128).T
        vecs[:, V_N2:V_N2 + KD] = n2.reshape(KD, 128).T
        for ch in range(2):
            sl = slice(256 * c + 128 * ch, 256 * c + 128 * ch + 128)
            for tap in range(4):
                vecs[:, V_CW + ch * 4 + tap] = cw[tap, sl]
            vecs[:, V_CB + ch] = cb[sl]
            vecs[:, V_BA + ch] = ba[sl]
            vecs[:, V_BX + ch] = bx[sl]
            vecs[:, V_LAM + ch] = lam[sl]
            vecs[:, V_HI + ch] = float(2 * c + ch + 1)
        vecs[:, V_QW] = qw
        vecs[:, V_KW] = kw
        w_rg = np.zeros((128, 2, 2, 128), np.float32)
        for ch in range(2):
            w_rg[:, 0, ch, :] = wra[2 * c + ch]
            w_rg[:, 1, ch, :] = wrx[2 * c + ch]
        m = dict(shared)
        m.update(w_a=w_a, vecs=vecs, w_rg=w_rg, xTs=np.ascontiguousarray(xT[:, c * TB:(c + 1) * TB]),
                 cidx=np.array([[c]], np.int32))
        maps.append(m)
    return maps


_NC_CACHE = {}


def kernel(**inputs):
    if "nc" not in _NC_CACHE:
        _NC_CACHE["nc"] = build_program(debug=True)
    nc = _NC_CACHE["nc"]
    in_maps = make_in_maps(inputs)
    res = run_bass_kernel_spmd(nc, in_maps, core_ids=list(range(NC)))
    out = np.empty((1, S, D), np.float32)
    for c in range(NC):
        out[0, c * TB:(c + 1) * TB, :] = res.results[c]["outT"].T
    return out
```

```python
import numpy as np
import concourse.bass as bass
import concourse.mybir as mybir
from concourse.bass_utils import run_bass_kernel_spmd

F32 = mybir.dt.float32
BF16 = mybir.dt.bfloat16
AF = mybir.ActivationFunctionType
ALU = mybir.AluOpType
AX = mybir.AxisListType


class Buf:
    __slots__ = ("name", "last_w", "readers")

    def __init__(self, name):
        self.name = name
        self.last_w = None
        self.readers = []


class Op:
    __slots__ = ("eng", "fn", "deps", "signal", "sem", "val", "inc", "idx", "extra")

    def __init__(self, eng, fn, idx):
        self.eng = eng
        self.fn = fn
        self.deps = []
        self.signal = False
        self.sem = None
        self.val = None
        self.inc = 1
        self.idx = idx
        self.extra = ()


class Prog:
    ENGS = ("pe", "act", "dve", "pool", "sp")

    def __init__(self, nc, same_engine_sync=True):
        self.nc = nc
        self.ops = []
        self.same_engine_sync = same_engine_sync
        self.dma_sem_count = {}

    def op(self, eng, fn, reads=(), writes=(), dma_sem=None, extra=()):
        o = Op(eng, fn, len(self.ops))
        o.extra = tuple(extra)
        deps = {}
        for b in reads:
            if b.last_w is not None:
                deps[b.last_w.idx] = b.last_w
        for b in writes:
            if b.last_w is not None:
                deps[b.last_w.idx] = b.last_w
            for r in b.readers:
                deps[r.idx] = r
        is_dma = dma_sem is not None
        for d in deps.values():
            d_is_dma = d.sem is not None
            if d.eng == eng and not d_is_dma and not is_dma:
                if eng == "pe" or not self.same_engine_sync:
                    continue
            if d is o:
                continue
            o.deps.append(d)
            d.signal = True
        if is_dma:
            o.sem = dma_sem
            o.inc = 16
            o.signal = True
            c = self.dma_sem_count.get(id(dma_sem), 0) + 16
            self.dma_sem_count[id(dma_sem)] = c
            o.val = c
        for b in reads:
            b.readers.append(o)
        for b in writes:
            b.last_w = o
            b.readers = []
        self.ops.append(o)
        return o

    def emit(self, block, eng_sems, final_sems=(), counters=None):
        nc = self.nc
        if counters is None:
            counters = {e: 0 for e in self.ENGS}
        for o in self.ops:
            if o.sem is None:
                if o.signal:
                    counters[o.eng] += 1
                    o.sem = eng_sems[o.eng]
                    o.val = counters[o.eng]
        by_eng = {e: [o for o in self.ops if o.eng == e] for e in self.ENGS}

        def run(engine, ops, is_last_owner):
            seen = {}
            for o in ops:
                need = {}
                for d in o.deps:
                    k = id(d.sem)
                    if k not in need or need[k][1] < d.val:
                        need[k] = (d.sem, d.val)
                for (xs, xv) in o.extra:
                    k = id(xs)
                    if k not in need or need[k][1] < xv:
                        need[k] = (xs, xv)
                for k, (s, v) in need.items():
                    if seen.get(k, 0) >= v:
                        continue
                    engine.wait_ge(s, v)
                    seen[k] = v
                ins = o.fn(engine)
                if o.signal:
                    ins.then_inc(o.sem, o.inc)
            if is_last_owner:
                for s in final_sems:
                    engine.wait_ge(s, self.dma_sem_count[id(s)])

        if by_eng["pe"]:
            @block.tensor
            def _(e):
                run(e, by_eng["pe"], False)
        if by_eng["act"]:
            @block.scalar
            def _(e):
                run(e, by_eng["act"], False)
        if by_eng["dve"]:
            @block.vector
            def _(e):
                run(e, by_eng["dve"], False)
        if by_eng["pool"]:
            @block.gpsimd
            def _(e):
                run(e, by_eng["pool"], False)

        @block.sync
        def _(e):
            run(e, by_eng["sp"], True)


S = 8192
D = 2048
H = 16
DH = 128
BLK = 256
NBLK = S // BLK
DFF = 5632
NFF = DFF // 128
KD = D // 128
TT = 512
NTILE = S // TT
NC = 8
TB = S // NC
EPS = 1e-6
SCALE = DH ** -0.5
FG = 4
FGC = NFF // FG
NEG = -1.0e30

V_N1 = 0
V_CW = 16
V_CB = 24
V_BA = 26
V_BX = 28
V_LAM = 30
V_QW = 32
V_KW = 33
V_N2 = 34
V_HI = 50
NV = 52
C_D = 0
C_KB = 64
C_DD = 66
C_DF = 194
NCT = 322


class Ctx:
    pass


def _ring(n):
    return [Buf(f"r{i}") for i in range(n)]


def build_program(debug=False):
    nc = bass.Bass("TRN2", target_bir_lowering=False)
    dt_i32 = mybir.dt.int32

    def din(name, shape, dt=F32):
        return nc.dram_tensor(name, list(shape), dt, kind="ExternalInput").ap()

    def dout(name, shape, dt=F32):
        return nc.dram_tensor(name, list(shape), dt, kind="ExternalOutput").ap()

    xT = din("xT", [D, S])
    xTs = din("xTs", [D, TB])
    w_a = din("w_a", [128, KD, 1280])
    vecs_d = din("vecs", [128, NV])
    ctab_d = din("ctab", [128, NCT])
    wrg_d = din("w_rg", [128, 2, 2, 128])
    cidx = din("cidx", [1, 1], dt_i32)
    w_g = din("w_g", [32, 128, KD * 128])
    w_pa = din("w_pa", [16, 128, KD * 128])
    w_pl = din("w_pl", [16, 128, KD * 128])
    w_o = din("w_o", [16, 128, KD * 128])
    w_fg = din("w_fg", [NFF, 128, KD * 128])
    w_fu = din("w_fu", [NFF, 128, KD * 128])
    w_fd = din("w_fd", [FG * 16, 128, FGC * 128])
    outT = dout("outT", [D, TB])

    ag_lru_in = nc.dram_tensor("ag_lru_in", [NC, 256, TB], BF16).ap()
    ag_att_in = nc.dram_tensor("ag_att_in", [NC, 256, TB], BF16).ap()
    ag_lru_out = nc.dram_tensor("ag_lru_out", [NC * NC, 256, TB], BF16).ap()
    ag_att_out = nc.dram_tensor("ag_att_out", [NC * NC, 256, TB], BF16).ap()

    dbg = {}
    if debug:
        dbg["lru"] = dout("dbg_lru", [NC, 256, TB], BF16)
        dbg["att"] = dout("dbg_att", [NC, 256, TB], BF16)
        dbg["kt"] = dout("dbg_kt", [128, S], BF16)
        dbg["qt"] = dout("dbg_qt", [128, S], BF16)
        dbg["v"] = dout("dbg_v", [128, 64, 130], BF16)
        dbg["sel"] = dout("dbg_sel", [128, 64, 32], BF16)
        dbg["merged"] = dout("dbg_merged", [128, KD, TB], BF16)
        dbg["x1"] = dout("dbg_x1", [128, KD, TB], F32)

    sem_state = {}
    eng_counts = {e: 0 for e in Prog.ENGS}

    import contextlib
    es = contextlib.ExitStack()
    with es:
        eng_sems = {e: es.enter_context(nc.semaphore(f"s_{e}")) for e in Prog.ENGS}
        NDS = 40
        dsems = [es.enter_context(nc.semaphore(f"d{i}")) for i in range(NDS)]
        cc_sem = es.enter_context(nc.semaphore("cc"))
        creg = es.enter_context(nc.sync.register("creg"))
        ps = es.enter_context(nc.psum_tensor("ps", [128, 8, 512], F32))
        psb = [Buf(f"ps{i}") for i in range(8)]

        vecs = es.enter_context(nc.sbuf_tensor("vecs_sb", [128, NV], F32))
        ctab = es.enter_context(nc.sbuf_tensor("ctab_sb", [128, NCT], F32))
        ident = es.enter_context(nc.sbuf_tensor("ident", [128, 128], BF16))
        ones = es.enter_context(nc.sbuf_tensor("ones", [128, 128], BF16))
        sm = es.enter_context(nc.sbuf_tensor("sm", [128, 32], F32))
        SM_SL, SM_NSL, SM_SP, SM_SP2, SM_NBA, SM_NBX, SM_KB = 0, 2, 4, 6, 8, 10, 12

        class Phase:
            def __init__(self, name):
                self.name = name
                self.P = Prog(nc)
                self.P.dma_sem_count = sem_state
                self.used = []
                self.next_ds = 0
                for b_ in psb:
                    b_.last_w, b_.readers = None, []

            def dsem(self):
                s = dsems[self.next_ds]
                self.next_ds += 1
                self.used.append(s)
                return s

            def finish(self):
                with nc.Block() as block:
                    self.P.emit(block, eng_sems, final_sems=[s for s in self.used if id(s) in sem_state],
                                counters=eng_counts)

        def sc(col, n=1):
            return sm[:, col:col + n]

        def vc(col, n=1):
            return vecs[:, col:col + n]

        ph = Phase("setup")
        P = ph.P
        b_vecs, b_ctab, b_sm, b_id, b_ones = Buf("vecs"), Buf("ctab"), Buf("sm"), Buf("id"), Buf("ones")
        P.op("sp", lambda e: e.dma_start(out=vecs[:], in_=vecs_d), writes=[b_vecs], dma_sem=ph.dsem())
        P.op("sp", lambda e: e.dma_start(out=ctab[:], in_=ctab_d), writes=[b_ctab], dma_sem=ph.dsem())
        P.op("sp", lambda e: e.reg_load(creg, cidx[0:1, 0:1]))
        P.op("dve", lambda e: e.memset(ident[:], 0.0), writes=[b_id])
        P.op("pool", lambda e: e.affine_select(out=ident[:], in_=ident[:], pattern=[[-1, 128]], compare_op=ALU.not_equal,
                                                fill=1.0, base=0, channel_multiplier=1), reads=[b_id], writes=[b_id])
        P.op("dve", lambda e: e.memset(ones[:], 1.0), writes=[b_ones])
        P.op("act", lambda e: e.activation(out=sc(SM_SL, 2), in_=vc(V_HI, 2), func=AF.Exp, scale=-0.5 * float(np.log(2.0))),
             reads=[b_vecs], writes=[b_sm])
        P.op("dve", lambda e: e.tensor_scalar(out=sc(SM_NSL, 2), in0=sc(SM_SL, 2), scalar1=-1.0, scalar2=None, op0=ALU.mult),
             reads=[b_sm], writes=[b_sm])
        P.op("act", lambda e: e.activation(out=sc(SM_SP, 2), in_=vc(V_LAM, 2), func=AF.Exp, scale=-1.0), reads=[b_vecs, b_sm], writes=[b_sm])
        P.op("act", lambda e: e.activation(out=sc(SM_SP, 2), in_=sc(SM_SP, 2), func=AF.Ln, bias=1.0, scale=1.0), reads=[b_sm], writes=[b_sm])
        P.op("dve", lambda e: e.tensor_scalar(out=sc(SM_SP2, 2), in0=sc(SM_SP, 2), scalar1=-16.0, scalar2=None, op0=ALU.mult), reads=[b_sm], writes=[b_sm])
        P.op("dve", lambda e: e.tensor_scalar(out=sc(SM_SP, 2), in0=sc(SM_SP, 2), scalar1=-8.0, scalar2=None, op0=ALU.mult), reads=[b_sm], writes=[b_sm])
        P.op("dve", lambda e: e.tensor_scalar(out=sc(SM_NBA, 2), in0=vc(V_BA, 2), scalar1=-1.0, scalar2=None, op0=ALU.mult), reads=[b_vecs, b_sm], writes=[b_sm])
        P.op("dve", lambda e: e.tensor_scalar(out=sc(SM_NBX, 2), in0=vc(V_BX, 2), scalar1=-1.0, scalar2=None, op0=ALU.mult), reads=[b_vecs, b_sm], writes=[b_sm])
        for hl in range(2):
            P.op("dve", lambda e, hl=hl: e.tensor_scalar(out=sc(SM_KB + 2 * hl, 2), in0=ctab[:, C_KB:C_KB + 2], scalar1=sc(SM_SL + hl),
                                                          scalar2=None, op0=ALU.mult), reads=[b_ctab, b_sm], writes=[b_sm])
        ph.finish()

        def phase_A(which, A=None):
            ph = Phase("A_" + which)
            P = ph.P
            c0, ncol = (0, 512) if which == "lru" else (512, 768)
            nm = ncol // 128
            with contextlib.ExitStack() as st:
                T = lambda name, shape, dt: st.enter_context(nc.sbuf_tensor(which + "_" + name, shape, dt))
                wa = T("wa", [128, KD, ncol], BF16)
                NXF = 6 if which == "lru" else 4
                NWS = 2 if which == "lru" else 1
                wst = T("wst", [128, NWS, ncol], F32)
                xf = T("xf", [128, NXF, TT], F32)
                sqb = T("sqb", [128, 3, TT], BF16)
                xb = T("xb", [128, 2, KD, TT], BF16)
                rstd = T("rstd", [128, 2, TT], F32)
                lnt = T("lnt", [128, TT], F32)
                b_wa = [Buf(f"wa{k}") for k in range(KD)]
                b_wst = _ring(NWS)
                s_wst = [ph.dsem() for _ in range(NWS)]
                b_xf = _ring(NXF)
                s_xf = [ph.dsem() for _ in range(NXF)]
                b_sqb = _ring(3)
                b_xb = [[Buf(f"xb{p}_{k}") for k in range(KD)] for p in range(2)]
                b_rstd = _ring(2)
                b_lnt = Buf("lnt")
                BV = Buf("vecs_ro")

                for k in range(KD):
                    sl = k % NWS
                    P.op("sp", lambda e, k=k, sl=sl: e.dma_start(out=wst[:, sl, :], in_=w_a[:, k, c0:c0 + ncol]),
                         writes=[b_wst[sl]], dma_sem=s_wst[sl])
                    P.op("dve", lambda e, k=k, sl=sl: e.tensor_scalar(out=wa[:, k, :], in0=wst[:, sl, :], scalar1=vc(V_N1 + k),
                                                                     scalar2=None, op0=ALU.mult),
                         reads=[b_wst[sl]], writes=[b_wa[k]])

                xcnt = [0]
                sqcnt = [0]

                def x_tile(t):
                    par = t % 2
                    for k in range(KD):
                        sl = xcnt[0] % NXF
                        xcnt[0] += 1
                        sq = sqcnt[0] % 3
                        sqcnt[0] += 1
                        P.op("sp", lambda e, k=k, sl=sl: e.dma_start(out=xf[:, sl, :], in_=xT[k * 128:(k + 1) * 128, t * TT:(t + 1) * TT]),
                             writes=[b_xf[sl]], dma_sem=s_xf[sl])
                        P.op("act", lambda e, sl=sl, sq=sq: e.activation(out=sqb[:, sq, :], in_=xf[:, sl, :], func=AF.Square),
                             reads=[b_xf[sl]], writes=[b_sqb[sq]])
                        P.op("dve", lambda e, k=k, sl=sl: e.tensor_copy(out=xb[:, par, k, :], in_=xf[:, sl, :]),
                             reads=[b_xf[sl]], writes=[b_xb[par][k]])
                        P.op("pe", lambda e, k=k, sq=sq: e.matmul(ps[:, 0, :], lhsT=ones[:], rhs=sqb[:, sq, :], start=(k == 0), stop=(k == KD - 1)),
                             reads=[b_sqb[sq]], writes=[psb[0]])
                    P.op("act", lambda e: e.activation(out=lnt[:], in_=ps[:, 0, :], func=AF.Ln, scale=1.0 / D, bias=EPS),
                         reads=[psb[0]], writes=[b_lnt])
                    P.op("act", lambda e: e.activation(out=rstd[:, par, :], in_=lnt[:], func=AF.Exp, scale=-0.5),
                         reads=[b_lnt], writes=[b_rstd[par]])

                pcnt = [0]

                def proj(t, m):
                    par = t % 2
                    bank = 1 + pcnt[0] % 3
                    pcnt[0] += 1
                    for k in range(KD):
                        P.op("pe", lambda e, k=k, bank=bank: e.matmul(ps[:, bank, :], lhsT=wa[:, k, m * 128:(m + 1) * 128], rhs=xb[:, par, k, :],
                                                                       start=(k == 0), stop=(k == KD - 1)),
                             reads=[b_wa[k], b_xb[par][k]], writes=[psb[bank]])
                    return bank

                if which == "lru":
                    lru_body(ph, st, x_tile, proj, rstd, b_rstd)
                else:
                    qkv_body(ph, st, x_tile, proj, rstd, b_rstd, A)
                ph.finish()

        def run_interleaved(gens, hook_after=None, hook=None):
            alive = list(gens)
            step = 0
            while alive:
                for g in list(alive):
                    try:
                        next(g)
                    except StopIteration:
                        alive.remove(g)
                step += 1
                if hook is not None and step == hook_after:
                    hook()
                    hook = None
            if hook is not None:
                hook()

        def lru_body(ph, st, x_tile, proj, rstd, b_rstd):
            P = ph.P
            T = lambda name, shape, dt: st.enter_context(nc.sbuf_tensor(name, shape, dt))
            wrgf = T("wrgf", [128, 2, 2, 128], F32)
            wrg = T("wrg", [128, 2, 2, 128], BF16)
            xrt = T("xrt", [128, 2, TT + 3], F32)
            u = T("u", [128, 2, TT], F32)
            ub = T("ub", [128, 2, TT], BF16)
            ta = T("ta", [128, 2, TT], F32)
            tb = T("tb", [128, 2, TT], F32)
            tc_ = T("tc", [128, 2, TT], F32)
            hs = T("hs", [128, 2, TT], F32)
            yt = T("yt", [128, 2, TT], F32)
            y2 = T("y2", [128, 2, TT], F32)
            carry = T("carry", [128, 2], F32)
            ost = T("ost", [128, 4, TT], BF16)
            b_wrgf, b_wrg = Buf("wrgf"), Buf("wrg")
            b_xrt = [Buf("xrt0"), Buf("xrt1")]
            mk = lambda n: [Buf(n + "0"), Buf(n + "1")]
            b_u, b_ub, b_ta, b_tb, b_tc, b_hs, b_yt, b_y2 = [mk(n) for n in "u ub ta tb tc hs yt y2".split()]
            b_carry = [Buf("c0"), Buf("c1")]
            b_ost = _ring(4)
            s_ost = [ph.dsem() for _ in range(4)]
            P.op("sp", lambda e: e.dma_start(out=wrgf[:], in_=wrg_d), writes=[b_wrgf], dma_sem=ph.dsem())
            P.op("dve", lambda e: e.tensor_copy(out=wrg[:], in_=wrgf[:]), reads=[b_wrgf], writes=[b_wrg])
            for ch in range(2):
                P.op("dve", lambda e, ch=ch: e.memset(xrt[:, ch, :], 0.0), writes=[b_xrt[ch]])
            ocnt = [0]

            def chain(t, ch):
                par = t % 2
                gb0, gb1 = (4, 5) if ch == 0 else (6, 7)
                cw = lambda tap: vc(V_CW + ch * 4 + tap)
                U, UB, TA, TB_, TC, HS, YT, Y2 = u[:, ch, :], ub[:, ch, :], ta[:, ch, :], tb[:, ch, :], tc_[:, ch, :], hs[:, ch, :], yt[:, ch, :], y2[:, ch, :]
                bu, bub, bta, btb, btc, bhs, byt, by2 = b_u[ch], b_ub[ch], b_ta[ch], b_tb[ch], b_tc[ch], b_hs[ch], b_yt[ch], b_y2[ch]
                if t > 0:
                    P.op("dve", lambda e: e.tensor_copy(out=xrt[:, ch, 0:3], in_=xrt[:, ch, TT:TT + 3]), reads=[b_xrt[ch]], writes=[b_xrt[ch]])
                bank = proj(t, ch)
                P.op("dve", lambda e: e.tensor_tensor(out=xrt[:, ch, 3:TT + 3], in0=ps[:, bank, :], in1=rstd[:, par, :], op=ALU.mult),
                     reads=[psb[bank], b_rstd[par], b_xrt[ch]], writes=[b_xrt[ch]])
                yield
                P.op("dve", lambda e: e.tensor_scalar(out=U, in0=xrt[:, ch, 3:TT + 3], scalar1=cw(3), scalar2=vc(V_CB + ch), op0=ALU.mult, op1=ALU.add),
                     reads=[b_xrt[ch]], writes=[bu])
                for tap in (2, 1, 0):
                    P.op("dve", lambda e, tap=tap: e.scalar_tensor_tensor(out=U, in0=xrt[:, ch, tap:tap + TT], scalar=cw(tap), in1=U, op0=ALU.mult, op1=ALU.add),
                         reads=[b_xrt[ch], bu], writes=[bu])
                P.op("act", lambda e: e.activation(out=UB, in_=U, func=AF.Copy), reads=[bu], writes=[bub])
                yield
                P.op("pe", lambda e: e.matmul(ps[:, gb0, :], lhsT=wrg[:, 0, ch, :], rhs=UB, start=True, stop=True), reads=[b_wrg, bub], writes=[psb[gb0]])
                P.op("pe", lambda e: e.matmul(ps[:, gb1, :], lhsT=wrg[:, 1, ch, :], rhs=UB, start=True, stop=True), reads=[b_wrg, bub], writes=[psb[gb1]])
                ybank = proj(t, 2 + ch)
                yield
                P.op("act", lambda e: e.activation(out=TA, in_=ps[:, gb0, :], func=AF.Exp, scale=-1.0, bias=sc(SM_NBA + ch)), reads=[psb[gb0]], writes=[bta])
                P.op("act", lambda e: e.activation(out=TC, in_=ps[:, gb1, :], func=AF.Exp, scale=-1.0, bias=sc(SM_NBX + ch)), reads=[psb[gb1]], writes=[btc])
                P.op("dve", lambda e: e.tensor_tensor(out=YT, in0=ps[:, ybank, :], in1=rstd[:, par, :], op=ALU.mult), reads=[psb[ybank], b_rstd[par]], writes=[byt])
                yield
                P.op("act", lambda e: e.activation(out=TA, in_=TA, func=AF.Ln, scale=1.0, bias=1.0), reads=[bta], writes=[bta])
                P.op("act", lambda e: e.activation(out=TC, in_=TC, func=AF.Ln, scale=1.0, bias=1.0), reads=[btc], writes=[btc])
                yield
                P.op("act", lambda e: e.activation(out=TA, in_=TA, func=AF.Exp, scale=-1.0), reads=[bta], writes=[bta])
                P.op("act", lambda e: e.activation(out=Y2, in_=YT, func=AF.Square), reads=[byt], writes=[by2])
                yield
                P.op("act", lambda e: e.activation(out=TB_, in_=TA, func=AF.Exp, scale=sc(SM_SP + ch)), reads=[bta], writes=[btb])
                P.op("dve", lambda e: e.tensor_scalar(out=Y2, in0=Y2, scalar1=0.044715, scalar2=1.0, op0=ALU.mult, op1=ALU.add), reads=[by2], writes=[by2])
                yield
                P.op("act", lambda e: e.activation(out=TA, in_=TA, func=AF.Exp, scale=sc(SM_SP2 + ch)), reads=[bta], writes=[bta])
                P.op("dve", lambda e: e.tensor_tensor(out=Y2, in0=Y2, in1=YT, op=ALU.mult), reads=[by2, byt], writes=[by2])
                yield
                P.op("act", lambda e: e.activation(out=TA, in_=TA, func=AF.Ln, scale=-1.0, bias=1.0), reads=[bta], writes=[bta])
                yield
                P.op("dve", lambda e: e.scalar_tensor_tensor(out=TC, in0=TA, scalar=0.5, in1=TC, op0=ALU.mult, op1=ALU.subtract), reads=[bta, btc], writes=[btc])
                P.op("act", lambda e: e.activation(out=Y2, in_=Y2, func=AF.Exp, scale=-1.5957691216), reads=[by2], writes=[by2])
                yield
                P.op("act", lambda e: e.activation(out=TC, in_=TC, func=AF.Exp), reads=[btc], writes=[btc])
                yield
                P.op("dve", lambda e: e.tensor_tensor(out=TC, in0=TC, in1=U, op=ALU.mult), reads=[btc, bu], writes=[btc])
                P.op("act", lambda e: e.activation(out=Y2, in_=Y2, func=AF.Ln, scale=1.0, bias=1.0), reads=[by2], writes=[by2])
                yield
                init = 0.0 if t == 0 else carry[:, ch:ch + 1]
                P.op("dve", lambda e: e.tensor_tensor_scan(out=HS, data0=TB_, data1=TC, initial=init, op0=ALU.mult, op1=ALU.add),
                     reads=[btb, btc, b_carry[ch]], writes=[bhs])
                P.op("act", lambda e: e.activation(out=Y2, in_=Y2, func=AF.Exp, scale=-1.0), reads=[by2], writes=[by2])
                yield
                P.op("dve", lambda e: e.tensor_copy(out=carry[:, ch:ch + 1], in_=HS[:, TT - 1:TT]), reads=[bhs], writes=[b_carry[ch]])
                P.op("dve", lambda e: e.tensor_tensor(out=YT, in0=YT, in1=HS, op=ALU.mult), reads=[byt, bhs], writes=[byt])
                yield
                sl = ocnt[0] % 4
                ocnt[0] += 1
                P.op("dve", lambda e: e.tensor_tensor(out=ost[:, sl, :], in0=YT, in1=Y2, op=ALU.mult), reads=[byt, by2], writes=[b_ost[sl]])
                j, off = t // 2, (t % 2) * TT
                P.op("sp", lambda e: e.dma_start(out=ag_lru_in[j, ch * 128:(ch + 1) * 128, off:off + TT], in_=ost[:, sl, :]),
                     reads=[b_ost[sl]], dma_sem=s_ost[sl])

            x_tile(0)
            for t in range(NTILE):
                nxt = (lambda t=t: x_tile(t + 1)) if t + 1 < NTILE else None
                run_interleaved([chain(t, 0), chain(t, 1)], hook_after=1, hook=nxt)

        def qkv_body(ph, st, x_tile, proj, rstd, b_rstd, A):
            P = ph.P
            T = lambda name, shape, dt: st.enter_context(nc.sbuf_tensor(name, shape, dt))
            kt = T("kt", [128, 2, TT], F32)
            sqk = T("sqk", [128, 2, TT], BF16)
            lk = T("lk", [128, 2, TT], F32)
            qf = T("qf", [128, 2, TT], F32)
            vt = T("vt", [128, 2, TT], BF16)
            gsb = T("gsb", [128, 2, 32], F32)
            m8 = T("m8", [128, 2, 8], F32)
            mk = lambda n: [Buf(n + "0"), Buf(n + "1")]
            b_kt, b_sqk, b_lk, b_qf, b_vt, b_m8, b_gsb = [mk(n) for n in "kt sqk lk qf vt m8 gsb".split()]
            psv = ps[:, 6, :].bitcast(BF16)
            for hl in range(2):
                P.op("act", lambda e, hl=hl: e.activation(out=A.Ttab[:, hl, :], in_=ctab[:, C_D:C_D + 64], func=AF.Exp, scale=sc(SM_NSL + hl)), writes=[A.b_tab])
                P.op("dve", lambda e, hl=hl: e.tensor_scalar(out=A.Bdiag[:, hl, :], in0=ctab[:, C_DD:C_DD + 128], scalar1=sc(SM_SL + hl), scalar2=None, op0=ALU.mult), writes=[A.b_tab])
                P.op("dve", lambda e, hl=hl: e.tensor_scalar(out=A.Bfull[:, hl, :], in0=ctab[:, C_DF:C_DF + 128], scalar1=sc(SM_SL + hl), scalar2=None, op0=ALU.mult), writes=[A.b_tab])
                P.op("dve", lambda e, hl=hl: e.memset(A.kmean[:, hl, :], 0.0), writes=[A.b_kmean[hl]])
                P.op("dve", lambda e, hl=hl: e.memset(gsb[:, hl, :], NEG), writes=[b_gsb[hl]])
                P.op("dve", lambda e, hl=hl: e.memset(A.Vext[hl][:, :, 128:130], 1.0), writes=[A.b_V[hl]])
                P.op("dve", lambda e, hl=hl: e.memset(A.selb[hl][:, 0:2, :], 0.0), writes=[A.b_sel[hl]])

            def headnorm(src_bank, par, hl):
                nb_ = 4 if hl == 0 else 7
                P.op("dve", lambda e: e.tensor_tensor(out=kt[:, hl, :], in0=ps[:, src_bank, :], in1=rstd[:, par, :], op=ALU.mult),
                     reads=[psb[src_bank], b_rstd[par]], writes=[b_kt[hl]])
                P.op("act", lambda e: e.activation(out=sqk[:, hl, :], in_=kt[:, hl, :], func=AF.Square), reads=[b_kt[hl]], writes=[b_sqk[hl]])
                P.op("pe", lambda e: e.matmul(ps[:, nb_, :], lhsT=ones[:], rhs=sqk[:, hl, :], start=True, stop=True), reads=[b_sqk[hl]], writes=[psb[nb_]])
                P.op("act", lambda e: e.activation(out=lk[:, hl, :], in_=ps[:, nb_, :], func=AF.Ln, scale=1.0 / DH, bias=EPS), reads=[psb[nb_]], writes=[b_lk[hl]])
                P.op("act", lambda e: e.activation(out=lk[:, hl, :], in_=lk[:, hl, :], func=AF.Exp, scale=-0.5), reads=[b_lk[hl]], writes=[b_lk[hl]])

            def k_chain(t, hl):
                par = t % 2
                cols = slice(t * TT, (t + 1) * TT)
                bank = proj(t, 0 + hl)
                yield
                headnorm(bank, par, hl)
                yield
                P.op("dve", lambda e: e.scalar_tensor_tensor(out=A.KT[hl][:, cols], in0=kt[:, hl, :], scalar=vc(V_KW), in1=lk[:, hl, :], op0=ALU.mult, op1=ALU.mult),
                     reads=[b_kt[hl], b_lk[hl]], writes=[A.b_KT[hl]])
                for bb in range(2):
                    n = 2 * t + bb
                    P.op("dve", lambda e, n=n: e.reduce_sum(out=A.kmean[:, hl, n:n + 1], in_=A.KT[hl][:, n * BLK:(n + 1) * BLK], axis=AX.X),
                         reads=[A.b_KT[hl]], writes=[A.b_kmean[hl]])

            def v_chain(t, hl):
                par = t % 2
                bank = proj(t, 2 + hl)
                yield
                P.op("dve", lambda e: e.tensor_tensor(out=vt[:, hl, :], in0=ps[:, bank, :], in1=rstd[:, par, :], op=ALU.mult),
                     reads=[psb[bank], b_rstd[par]], writes=[b_vt[hl]])
                yield
                for c in range(4):
                    P.op("pe", lambda e, c=c: e.transpose(out=psv[:, c * 128:(c + 1) * 128], in_=vt[:, hl, c * 128:(c + 1) * 128], identity=ident[:]),
                         reads=[b_vt[hl]], writes=[psb[6]])
                P.op("act", lambda e: e.activation(out=A.Vext[hl][:, 4 * t:4 * t + 4, 0:128], in_=psv[:, 0:512].rearrange("p (c d) -> p c d", c=4), func=AF.Copy),
                     reads=[psb[6]], writes=[A.b_V[hl]])

            def q_chain(t, hl):
                par = t % 2
                cols = slice(t * TT, (t + 1) * TT)
                bank = proj(t, 4 + hl)
                yield
                headnorm(bank, par, hl)
                yield
                P.op("dve", lambda e: e.scalar_tensor_tensor(out=qf[:, hl, :], in0=kt[:, hl, :], scalar=vc(V_QW), in1=lk[:, hl, :], op0=ALU.mult, op1=ALU.mult),
                     reads=[b_kt[hl], b_lk[hl]], writes=[b_qf[hl]])
                P.op("act", lambda e: e.activation(out=A.QT[hl][:, cols], in_=qf[:, hl, :], func=AF.Copy), reads=[b_qf[hl]], writes=[A.b_QT[hl]])
                yield
                for c in range(4):
                    P.op("pe", lambda e, c=c: e.matmul(ps[:, 5, hl * 128 + c * 32:hl * 128 + (c + 1) * 32], lhsT=qf[:, hl, c * 128:(c + 1) * 128], rhs=A.kmean[:, hl, :], start=True, stop=True),
                         reads=[b_qf[hl], A.b_kmean[hl]], writes=[psb[5]])
                yield
                for c in range(4):
                    b = 2 * t + c // 2
                    cg = 4 * t + c
                    if b == 0:
                        continue
                    P.op("dve", lambda e, c=c, b=b: e.tensor_copy(out=gsb[:, hl, 0:b], in_=ps[:, 5, hl * 128 + c * 32:hl * 128 + c * 32 + b]),
                         reads=[psb[5], b_gsb[hl]], writes=[b_gsb[hl]])
                    P.op("dve", lambda e: e.max(out=m8[:, hl, :], in_=gsb[:, hl, :]), reads=[b_gsb[hl]], writes=[b_m8[hl]])
                    P.op("dve", lambda e, cg=cg: e.tensor_scalar(out=A.selb[hl][:, cg, :], in0=gsb[:, hl, :], scalar1=m8[:, hl, 2:3], scalar2=None, op0=ALU.is_ge),
                         reads=[b_gsb[hl], b_m8[hl]], writes=[A.b_sel[hl]])
                    yield

            x_tile(0)
            for t in range(NTILE):
                nxt = (lambda t=t: x_tile(t + 1)) if t + 1 < NTILE else None
                run_interleaved([k_chain(t, 0), k_chain(t, 1)], hook_after=1, hook=nxt)
                run_interleaved([v_chain(t, 0), v_chain(t, 1)])
                run_interleaved([q_chain(t, 0), q_chain(t, 1)])
            if debug:
                sdb = ph.dsem()
                P.op("sp", lambda e: e.dma_start(out=dbg["kt"], in_=A.KT[0][:]), reads=[A.b_KT[0]], dma_sem=sdb)
                P.op("sp", lambda e: e.dma_start(out=dbg["qt"], in_=A.QT[0][:]), reads=[A.b_QT[0]], dma_sem=sdb)
                P.op("sp", lambda e: e.dma_start(out=dbg["v"], in_=A.Vext[0][:]), reads=[A.b_V[0]], dma_sem=sdb)
                P.op("sp", lambda e: e.dma_start(out=dbg["sel"], in_=A.selb[0][:]), reads=[A.b_sel[0]], dma_sem=sdb)

        def phase_att(A):
            ph = Phase("att")
            P = ph.P
            issue_ag(ph, "lru", ag_lru_in, ag_lru_out)
            NA = 4
            LA = 2
            with contextlib.ExitStack() as st:
                T = lambda name, shape, dt: st.enter_context(nc.sbuf_tensor(name, shape, dt))
                pt = T("pt", [128, 3, 512], BF16)
                tmpd = T("tmpd", [128, 2, 384], F32)
                acc = T("acc", [128, 2, NA, 2, 130], F32)
                mfac = T("mfac", [128, 2, 2, 32], F32)
                rden = T("rden", [128, 2, 2], F32)
                ob = T("ob", [128, 2, 2, 128], BF16)
                ast_ = T("attst", [128, 4, BLK], BF16)
                b_pt = [[Buf(f"pt{i}a"), Buf(f"pt{i}b")] for i in range(3)]
                b_tmpd = _ring(2)
                b_acc = [[[Buf(f"acc{p}{a}{j}") for j in range(2)] for a in range(NA)] for p in range(2)]
                b_mfac = _ring(2)
                b_rden = _ring(2)
                b_ob = _ring(2)
                b_ast = _ring(4)
                s_ast = [ph.dsem() for _ in range(4)]
                pst = ps[:, 6, :].bitcast(BF16)

                items = []
                for hl in range(2):
                    for b in range(NBLK):
                        items.append((hl, b, -1))
                        for n in range(b):
                            items.append((hl, b, n))
                N = len(items)
                blk_index = {}
                for hl in range(2):
                    for b in range(NBLK):
                        blk_index[(hl, b)] = hl * NBLK + b

                def stage_qk(i):
                    hl, b, n = items[i]
                    KT, QT = A.KT[hl], A.QT[hl]
                    bK, bQ = A.b_KT[hl], A.b_QT[hl]
                    kb0, kb1 = sc(SM_KB + 2 * hl), sc(SM_KB + 2 * hl + 1)
                    q0 = b * BLK
                    sb = i % 3
                    pi = i % 3
                    ip = blk_index[(hl, b)] % 2
                    if n < 0:
                        P.op("pe", lambda e: e.matmul(ps[:, sb, 0:128], lhsT=KT[:, q0:q0 + 128], rhs=QT[:, q0:q0 + 128], start=True, stop=True), reads=[bK, bQ], writes=[psb[sb]])
                        P.op("pe", lambda e: e.matmul(ps[:, sb, 128:256], lhsT=KT[:, q0 + 128:q0 + 256], rhs=QT[:, q0 + 128:q0 + 256], start=True, stop=True), reads=[bK, bQ], writes=[psb[sb]])
                        P.op("pe", lambda e: e.matmul(ps[:, sb, 256:384], lhsT=KT[:, q0:q0 + 128], rhs=QT[:, q0 + 128:q0 + 256], start=True, stop=True), reads=[bK, bQ], writes=[psb[sb]])
                        for r, tabl in ((0, A.Bdiag), (1, A.Bdiag), (2, A.Bfull)):
                            P.op("dve", lambda e, r=r, tabl=tabl: e.scalar_tensor_tensor(
                                out=tmpd[:, ip, r * 128:(r + 1) * 128], in0=ps[:, sb, r * 128:(r + 1) * 128], scalar=SCALE, in1=tabl[:, hl, :], op0=ALU.mult, op1=ALU.add),
                                reads=[psb[sb], A.b_tab], writes=[b_tmpd[ip]])
                        P.op("act", lambda e: e.activation(out=pt[:, pi, 0:384], in_=tmpd[:, ip, :], func=AF.Exp), reads=[b_tmpd[ip]], writes=b_pt[pi])
                    else:
                        k0 = n * BLK
                        P.op("pe", lambda e: e.matmul(ps[:, sb, 0:256], lhsT=KT[:, k0:k0 + 128], rhs=QT[:, q0:q0 + 256], start=True, stop=True), reads=[bK, bQ], writes=[psb[sb]])
                        P.op("pe", lambda e: e.matmul(ps[:, sb, 256:512], lhsT=KT[:, k0 + 128:k0 + 256], rhs=QT[:, q0:q0 + 256], start=True, stop=True), reads=[bK, bQ], writes=[psb[sb]])
                        P.op("act", lambda e: e.activation(out=pt[:, pi, 0:256], in_=ps[:, sb, 0:256], func=AF.Exp, scale=SCALE, bias=kb0), reads=[psb[sb]], writes=[b_pt[pi][0]])
                        P.op("act", lambda e: e.activation(out=pt[:, pi, 256:512], in_=ps[:, sb, 256:512], func=AF.Exp, scale=SCALE, bias=kb1), reads=[psb[sb]], writes=[b_pt[pi][1]])

                def stage_pv(i):
                    hl, b, n = items[i]
                    V, bV = A.Vext[hl], A.b_V[hl]
                    pi = i % 3
                    ob_ = 3 + i % 3
                    bi = blk_index[(hl, b)]
                    ip = bi % 2
                    if n < 0:
                        if b > 0:
                            for j in range(2):
                                P.op("dve", lambda e, j=j: e.tensor_tensor(
                                    out=mfac[:, ip, j, 0:b], in0=A.selb[hl][:, 2 * b + j, 0:b], in1=A.Ttab[:, hl, j * 32 + 31 - b:j * 32 + 31], op=ALU.mult),
                                    reads=[A.b_sel[hl], A.b_tab], writes=[b_mfac[ip]])
                        P.op("pe", lambda e: e.matmul(ps[:, ob_, 0:129], lhsT=pt[:, pi, 0:128], rhs=V[:, 2 * b, 0:129], start=True, stop=True), reads=b_pt[pi] + [bV], writes=[psb[ob_]])
                        P.op("pe", lambda e: e.matmul(ps[:, ob_, 256:385], lhsT=pt[:, pi, 256:384], rhs=V[:, 2 * b, 0:129], start=True, stop=False), reads=b_pt[pi] + [bV], writes=[psb[ob_]])
                        P.op("pe", lambda e: e.matmul(ps[:, ob_, 256:385], lhsT=pt[:, pi, 128:256], rhs=V[:, 2 * b + 1, 0:129], start=False, stop=True), reads=b_pt[pi] + [bV], writes=[psb[ob_]])
                        for j in range(2):
                            P.op("dve", lambda e, j=j: e.tensor_copy(out=acc[:, ip, 0, j, 0:129], in_=ps[:, ob_, j * 256:j * 256 + 129]),
                                 reads=[psb[ob_]], writes=[b_acc[ip][0][j]])
                    else:
                        for j in range(2):
                            P.op("pe", lambda e, j=j: e.matmul(ps[:, ob_, j * 256:j * 256 + 129], lhsT=pt[:, pi, j * 128:(j + 1) * 128], rhs=V[:, 2 * n, 0:129], start=True, stop=False),
                                 reads=[b_pt[pi][0], bV], writes=[psb[ob_]])
                            P.op("pe", lambda e, j=j: e.matmul(ps[:, ob_, j * 256:j * 256 + 129], lhsT=pt[:, pi, 256 + j * 128:256 + (j + 1) * 128], rhs=V[:, 2 * n + 1, 0:129], start=False, stop=True),
                                 reads=[b_pt[pi][1], bV], writes=[psb[ob_]])
                        a = (n + 1) % NA
                        first = (n + 1) < NA
                        for j in range(2):
                            if first:
                                P.op("dve", lambda e, j=j: e.tensor_scalar(out=acc[:, ip, a, j, 0:129], in0=ps[:, ob_, j * 256:j * 256 + 129], scalar1=mfac[:, ip, j, n:n + 1], scalar2=None, op0=ALU.mult),
                                     reads=[psb[ob_], b_mfac[ip]], writes=[b_acc[ip][a][j]])
                            else:
                                P.op("dve", lambda e, j=j: e.scalar_tensor_tensor(
                                    out=acc[:, ip, a, j, 0:129], in0=ps[:, ob_, j * 256:j * 256 + 129], scalar=mfac[:, ip, j, n:n + 1], in1=acc[:, ip, a, j, 0:129], op0=ALU.mult, op1=ALU.add),
                                    reads=[psb[ob_], b_mfac[ip], b_acc[ip][a][j]], writes=[b_acc[ip][a][j]])
                    if n == b - 1:
                        nacc = min(NA, b + 1)
                        for j in range(2):
                            for a in range(1, nacc):
                                P.op("dve", lambda e, j=j, a=a: e.tensor_tensor(out=acc[:, ip, 0, j, 0:129], in0=acc[:, ip, 0, j, 0:129], in1=acc[:, ip, a, j, 0:129], op=ALU.add),
                                     reads=[b_acc[ip][0][j], b_acc[ip][a][j]], writes=[b_acc[ip][0][j]])
                            P.op("dve", lambda e, j=j: e.reciprocal(out=rden[:, ip, j:j + 1], in_=acc[:, ip, 0, j, 128:129]), reads=[b_acc[ip][0][j], b_rden[ip]], writes=[b_rden[ip]])
                            P.op("dve", lambda e, j=j: e.tensor_scalar(out=ob[:, ip, j, :], in0=acc[:, ip, 0, j, 0:128], scalar1=rden[:, ip, j:j + 1], scalar2=None, op0=ALU.mult),
                                 reads=[b_acc[ip][0][j], b_rden[ip], b_ob[ip]], writes=[b_ob[ip]])
                        for j in range(2):
                            P.op("pe", lambda e, j=j: e.transpose(out=pst[:, j * 128:(j + 1) * 128], in_=ob[:, ip, j, :], identity=ident[:]), reads=[b_ob[ip]], writes=[psb[6]])
                        ai = bi % 4
                        P.op("act", lambda e: e.activation(out=ast_[:, ai, :], in_=pst[:, 0:BLK], func=AF.Copy), reads=[psb[6]], writes=[b_ast[ai]])
                        jd, off = b // 4, (b % 4) * BLK
                        P.op("sp", lambda e: e.dma_start(out=ag_att_in[jd, hl * 128:(hl + 1) * 128, off:off + BLK], in_=ast_[:, ai, :]),
                             reads=[b_ast[ai]], dma_sem=s_ast[ai])

                for i in range(-LA, N):
                    if i + LA < N:
                        stage_qk(i + LA)
                    if i >= 0:
                        stage_pv(i)
                ph.finish()

        cc_vals = {}

        def issue_ag(ph, name, src, dst):
            o = ph.P.op("pool", lambda e: e.collective_compute("AllGather", ALU.bypass, replica_groups=[list(range(NC))],
                                                                ins=[src.rearrange("j f t -> (j f) t")], outs=[dst.rearrange("a f t -> (a f) t")]))
            v = sem_state.get(id(cc_sem), 0) + 1
            sem_state[id(cc_sem)] = v
            o.sem, o.inc, o.val, o.signal = cc_sem, 1, v, True
            cc_vals[name] = v

        def phase_B():
            with contextlib.ExitStack() as so:
                TO = lambda name, shape, dt: so.enter_context(nc.sbuf_tensor(name, shape, dt))
                NW = 7
                wr = TO("wr", [128, NW, KD * 128], BF16)
                merged = TO("merged", [128, KD, TB], BF16)
                rstd1 = TO("rstd1", [128, TB], F32)
                lnb = TO("lnb", [128, TB], F32)

                class WStream:
                    def __init__(self, ph, jobs, pf=6):
                        self.ph, self.jobs, self.pf = ph, jobs, pf
                        self.b = _ring(NW)
                        self.s = [ph.dsem() for _ in range(NW)]
                        self.issued = 0

                    def get(self, i):
                        while self.issued < min(len(self.jobs), i + 1 + self.pf):
                            src, L = self.jobs[self.issued]
                            sl = self.issued % NW
                            self.ph.P.op("pool", lambda e, src=src, L=L, sl=sl: e.dma_start(out=wr[:, sl, 0:L], in_=src),
                                         writes=[self.b[sl]], dma_sem=self.s[sl])
                            self.issued += 1
                        sl = i % NW
                        return sl, self.b[sl]

                bankc = [0]

                def nb():
                    b = bankc[0] % 8
                    bankc[0] += 1
                    return b

                ph = Phase("B12")
                P = ph.P
                issue_ag(ph, "att", ag_att_in, ag_att_out)
                with contextlib.ExitStack() as st:
                    T = lambda name, shape, dt: st.enter_context(nc.sbuf_tensor(name, shape, dt))
                    attlru = T("attlru", [128, 32, TB], BF16)
                    hb = T("hb", [128, KD, TB], BF16)
                    xring = T("xring", [128, 2, TB], F32)
                    sq1 = T("sq1", [128, 2, TB], BF16)
                    tmp = T("tmpB", [128, 2, 4, TT], F32)
                    b_al = [Buf(f"al{i}") for i in range(16)]
                    b_hb = [Buf(f"hb{k}") for k in range(KD)]
                    b_xr = _ring(2)
                    s_xr = [ph.dsem() for _ in range(2)]
                    b_sq1 = _ring(2)
                    b_tmp = [[Buf(f"t{p}{i}") for i in range(4)] for p in range(2)]
                    b_rstd1, b_lnb = Buf("rstd1"), Buf("lnb")
                    b_mg = [Buf(f"mg{k}") for k in range(KD)]
                    jobs = []
                    for m in range(KD):
                        jobs += [(w_g[m], KD * 128), (w_g[16 + m], KD * 128), (w_pl[m], KD * 128), (w_pa[m], KD * 128)]
                    W = WStream(ph, jobs, pf=3)
                    W.get(0)
                    s_al = ph.dsem()
                    vcache = {}
                    for name, base, src in (("lru", 16, ag_lru_out), ("att", 0, ag_att_out)):
                        src2 = src.rearrange("(c j) (h p) t -> j c h p t", j=NC, p=128)
                        for c in range(NC):
                            for h in range(2):
                                def ld(e, c=c, h=h, base=base, src2=src2):
                                    if "v" not in vcache:
                                        vcache["v"] = e.snap(creg, min_val=0, max_val=NC - 1)
                                    return e.dma_start(out=attlru[:, base + 2 * c + h, :], in_=src2[bass.ds(vcache["v"], 1), c, h].squeeze(0))
                                P.op("sp", ld, dma_sem=s_al, extra=[(cc_sem, cc_vals[name])])
                        P.op("sp", lambda e: e.nop(), writes=b_al[(base // 16) * 8:(base // 16) * 8 + 8], extra=[(s_al, sem_state[id(s_al)])])
                    for k in range(KD):
                        sl = k % 2
                        P.op("sp", lambda e, k=k, sl=sl: e.dma_start(out=xring[:, sl, :], in_=xTs[k * 128:(k + 1) * 128, :]), writes=[b_xr[sl]], dma_sem=s_xr[sl])
                        P.op("act", lambda e, sl=sl: e.activation(out=sq1[:, sl, :], in_=xring[:, sl, :], func=AF.Square), reads=[b_xr[sl]], writes=[b_sq1[sl]])
                        P.op("dve", lambda e, k=k, sl=sl: e.tensor_scalar(out=hb[:, k, :], in0=xring[:, sl, :], scalar1=vc(V_N1 + k), scalar2=None, op0=ALU.mult),
                             reads=[b_xr[sl]], writes=[b_hb[k]])
                        for hf in range(2):
                            P.op("pe", lambda e, k=k, sl=sl, hf=hf: e.matmul(ps[:, hf, :], lhsT=ones[:], rhs=sq1[:, sl, hf * TT:(hf + 1) * TT], start=(k == 0), stop=(k == KD - 1)),
                                 reads=[b_sq1[sl]], writes=[psb[hf]])
                    for hf in range(2):
                        P.op("act", lambda e, hf=hf: e.activation(out=lnb[:, hf * TT:(hf + 1) * TT], in_=ps[:, hf, :], func=AF.Ln, scale=1.0 / D, bias=EPS), reads=[psb[hf]], writes=[b_lnb])
                    P.op("act", lambda e: e.activation(out=rstd1[:], in_=lnb[:], func=AF.Exp, scale=-0.5), reads=[b_lnb], writes=[b_rstd1])
                    bankc[0] = 2
                    def b2_step(m, hf, tp, slots):
                        if True:
                            hs_ = slice(hf * TT, (hf + 1) * TT)
                            banks = []
                            for i, (sl, bw) in enumerate(slots):
                                bk = nb()
                                banks.append(bk)
                                for k in range(KD):
                                    if i < 2:
                                        rhs, rb = hb[:, k, hs_], b_hb[k]
                                    elif i == 2:
                                        rhs, rb = attlru[:, 16 + k, hs_], b_al[8 + k // 2]
                                    else:
                                        rhs, rb = attlru[:, k, hs_], b_al[k // 2]
                                    P.op("pe", lambda e, sl=sl, k=k, bk=bk, rhs=rhs: e.matmul(ps[:, bk, :], lhsT=wr[:, sl, k * 128:(k + 1) * 128], rhs=rhs, start=(k == 0), stop=(k == KD - 1)),
                                         reads=[bw, rb], writes=[psb[bk]])
                            for i in range(2):
                                gb, pb = banks[i], banks[3 - i]
                                P.op("dve", lambda e, gb=gb, tp=tp, i=i: e.tensor_tensor(out=tmp[:, tp, i, :], in0=ps[:, gb, :], in1=rstd1[:, hs_], op=ALU.mult),
                                     reads=[psb[gb], b_rstd1], writes=[b_tmp[tp][i]])
                                P.op("act", lambda e, tp=tp, i=i: e.activation(out=tmp[:, tp, i, :], in_=tmp[:, tp, i, :], func=AF.Tanh, scale=0.5), reads=[b_tmp[tp][i]], writes=[b_tmp[tp][i]])
                                P.op("dve", lambda e, pb=pb, tp=tp, i=i: e.scalar_tensor_tensor(out=tmp[:, tp, 2 + i, :], in0=tmp[:, tp, i, :], scalar=1.0, in1=ps[:, pb, :], op0=ALU.add, op1=ALU.mult),
                                     reads=[b_tmp[tp][i], psb[pb]], writes=[b_tmp[tp][2 + i]])
                            P.op("dve", lambda e, tp=tp: e.tensor_tensor(out=tmp[:, tp, 2, :], in0=tmp[:, tp, 2, :], in1=tmp[:, tp, 3, :], op=ALU.add),
                                 reads=[b_tmp[tp][2], b_tmp[tp][3]], writes=[b_tmp[tp][2]])
                            P.op("act", lambda e, tp=tp, m=m: e.activation(out=merged[:, m, hs_], in_=tmp[:, tp, 2, :], func=AF.Copy, scale=0.5), reads=[b_tmp[tp][2]], writes=[b_mg[m]])

                    it = 0
                    for m in range(KD):
                        slots = [W.get(4 * m + i) for i in range(4)]
                        for hf in range(2):
                            b2_step(m, hf, it % 2, slots)
                            it += 1
                    if debug:
                        P.op("sp", lambda e: e.dma_start(out=dbg["merged"], in_=merged[:]), reads=b_mg, dma_sem=ph.dsem())
                        sdb = ph.dsem()
                        P.op("sp", lambda e: e.dma_start(out=dbg["lru"], in_=ag_lru_in), dma_sem=sdb)
                        P.op("sp", lambda e: e.dma_start(out=dbg["att"], in_=ag_att_in), dma_sem=sdb)
                    ph.finish()

                ph = Phase("B36")
                P = ph.P
                with contextlib.ExitStack() as st:
                    T = lambda name, shape, dt: st.enter_context(nc.sbuf_tensor(name, shape, dt))
                    x1 = T("x1", [128, KD, TB], F32)
                    h2b = T("h2b", [128, KD, TB], BF16)
                    actT = T("actT", [128, FGC, TB], BF16)
                    sq2 = T("sq2", [128, 2, TB], BF16)
                    tmp = T("tmpF", [128, 2, 2, TT], F32)
                    rstd2 = T("rstd2", [128, TB], F32)
                    b_x1 = [[Buf(f"x1_{m}_{h}") for h in range(2)] for m in range(KD)]
                    b_h2 = [Buf(f"h2{k}") for k in range(KD)]
                    b_act = [[Buf(f"a{f}_{h}") for h in range(2)] for f in range(FGC)]
                    b_sq2 = _ring(2)
                    b_tmp = [[Buf(f"tf{p}{i}") for i in range(2)] for p in range(2)]
                    b_rstd2, b_lnb = Buf("rstd2"), Buf("lnb")
                    b_mg = Buf("mg")
                    s_x1 = ph.dsem()
                    jobs = [(w_o[m], KD * 128) for m in range(KD)]
                    for g in range(FG):
                        for f in range(FGC):
                            jobs += [(w_fg[g * FGC + f], KD * 128), (w_fu[g * FGC + f], KD * 128)]
                        jobs += [(w_fd[g * 16 + m], FGC * 128) for m in range(KD)]
                    W = WStream(ph, jobs, pf=4)
                    W.get(0)
                    for m in range(KD):
                        P.op("sp", lambda e, m=m: e.dma_start(out=x1[:, m, :], in_=xTs[m * 128:(m + 1) * 128, :]), dma_sem=s_x1)
                    P.op("sp", lambda e: e.nop(), writes=[b for bb in b_x1 for b in bb], extra=[(s_x1, sem_state[id(s_x1)])])
                    ji = 0
                    for m in range(KD):
                        sl, bw = W.get(ji)
                        ji += 1
                        for hf in range(2):
                            hs_ = slice(hf * TT, (hf + 1) * TT)
                            bk = nb()
                            for k in range(KD):
                                P.op("pe", lambda e, sl=sl, k=k, bk=bk, hs_=hs_: e.matmul(ps[:, bk, :], lhsT=wr[:, sl, k * 128:(k + 1) * 128], rhs=merged[:, k, hs_], start=(k == 0), stop=(k == KD - 1)),
                                     reads=[bw, b_mg], writes=[psb[bk]])
                            P.op("dve", lambda e, m=m, bk=bk, hs_=hs_: e.tensor_tensor(out=x1[:, m, hs_], in0=x1[:, m, hs_], in1=ps[:, bk, :], op=ALU.add),
                                 reads=[psb[bk], b_x1[m][hf]], writes=[b_x1[m][hf]])
                    if debug:
                        P.op("sp", lambda e: e.dma_start(out=dbg["x1"], in_=x1[:]), reads=[b for bb in b_x1 for b in bb], dma_sem=ph.dsem())
                    sb0, sb1 = nb(), nb()
                    for k in range(KD):
                        sl = k % 2
                        P.op("act", lambda e, k=k, sl=sl: e.activation(out=sq2[:, sl, :], in_=x1[:, k, :], func=AF.Square), reads=b_x1[k], writes=[b_sq2[sl]])
                        for hf, bk in ((0, sb0), (1, sb1)):
                            P.op("pe", lambda e, k=k, sl=sl, hf=hf, bk=bk: e.matmul(ps[:, bk, :], lhsT=ones[:], rhs=sq2[:, sl, hf * TT:(hf + 1) * TT], start=(k == 0), stop=(k == KD - 1)),
                                 reads=[b_sq2[sl]], writes=[psb[bk]])
                    for hf, bk in ((0, sb0), (1, sb1)):
                        P.op("act", lambda e, hf=hf, bk=bk: e.activation(out=lnb[:, hf * TT:(hf + 1) * TT], in_=ps[:, bk, :], func=AF.Ln, scale=1.0 / D, bias=EPS), reads=[psb[bk]], writes=[b_lnb])
                    P.op("act", lambda e: e.activation(out=rstd2[:], in_=lnb[:], func=AF.Exp, scale=-0.5), reads=[b_lnb], writes=[b_rstd2])
                    for k in range(KD):
                        P.op("dve", lambda e, k=k: e.scalar_tensor_tensor(out=h2b[:, k, :], in0=x1[:, k, :], scalar=vc(V_N2 + k), in1=rstd2[:], op0=ALU.mult, op1=ALU.mult),
                             reads=b_x1[k] + [b_rstd2], writes=[b_h2[k]])
                    it = 0
                    for g in range(FG):
                        for f in range(FGC):
                            (slg, bwg), (slu, bwu) = W.get(ji), W.get(ji + 1)
                            ji += 2
                            for hf in range(2):
                                tp = it % 2
                                it += 1
                                hs_ = slice(hf * TT, (hf + 1) * TT)
                                bg, bu = nb(), nb()
                                for sl, bw, bk in ((slg, bwg, bg), (slu, bwu, bu)):
                                    for k in range(KD):
                                        P.op("pe", lambda e, sl=sl, k=k, bk=bk, hs_=hs_: e.matmul(ps[:, bk, :], lhsT=wr[:, sl, k * 128:(k + 1) * 128], rhs=h2b[:, k, hs_], start=(k == 0), stop=(k == KD - 1)),
                                             reads=[bw, b_h2[k]], writes=[psb[bk]])
                                P.op("act", lambda e, tp=tp, bg=bg: e.activation(out=tmp[:, tp, 0, :], in_=ps[:, bg, :], func=AF.Tanh, scale=0.5), reads=[psb[bg]], writes=[b_tmp[tp][0]])
                                P.op("dve", lambda e, tp=tp, bg=bg: e.scalar_tensor_tensor(out=tmp[:, tp, 1, :], in0=tmp[:, tp, 0, :], scalar=1.0, in1=ps[:, bg, :], op0=ALU.add, op1=ALU.mult),
                                     reads=[b_tmp[tp][0], psb[bg]], writes=[b_tmp[tp][1]])
                                P.op("dve", lambda e, tp=tp, bu=bu, f=f, hs_=hs_: e.scalar_tensor_tensor(out=actT[:, f, hs_], in0=tmp[:, tp, 1, :], scalar=0.5, in1=ps[:, bu, :], op0=ALU.mult, op1=ALU.mult),
                                     reads=[b_tmp[tp][1], psb[bu]], writes=[b_act[f][hf]])
                        for m in range(KD):
                            sl, bw = W.get(ji)
                            ji += 1
                            for hf in range(2):
                                hs_ = slice(hf * TT, (hf + 1) * TT)
                                bk = nb()
                                for f in range(FGC):
                                    P.op("pe", lambda e, sl=sl, f=f, bk=bk, hs_=hs_: e.matmul(ps[:, bk, :], lhsT=wr[:, sl, f * 128:(f + 1) * 128], rhs=actT[:, f, hs_], start=(f == 0), stop=(f == FGC - 1)),
                                         reads=[bw, b_act[f][hf]], writes=[psb[bk]])
                                P.op("dve", lambda e, m=m, bk=bk, hs_=hs_: e.tensor_tensor(out=x1[:, m, hs_], in0=x1[:, m, hs_], in1=ps[:, bk, :], op=ALU.add),
                                     reads=[psb[bk], b_x1[m][hf]], writes=[b_x1[m][hf]])
                    s_out = ph.dsem()
                    for m in range(KD):
                        P.op("sp", lambda e, m=m: e.dma_start(out=outT[m * 128:(m + 1) * 128, :], in_=x1[:, m, :]), reads=b_x1[m], dma_sem=s_out)
                    ph.finish()

        phase_A("lru")
        with contextlib.ExitStack() as sa:
            A = Ctx()
            TA = lambda name, shape, dt: sa.enter_context(nc.sbuf_tensor(name, shape, dt))
            A.QT = [TA(f"QT{h}", [128, S], BF16) for h in range(2)]
            A.KT = [TA(f"KT{h}", [128, S], BF16) for h in range(2)]
            A.Vext = [TA(f"V{h}", [128, 64, 130], BF16) for h in range(2)]
            A.selb = [TA(f"sel{h}", [128, 64, 32], BF16) for h in range(2)]
            A.kmean = TA("kmean", [128, 2, 32], F32)
            A.Ttab = TA("Ttab", [128, 2, 64], F32)
            A.Bdiag = TA("Bdiag", [128, 2, 128], F32)
            A.Bfull = TA("Bfull", [128, 2, 128], F32)
            A.b_QT = [Buf("QT0"), Buf("QT1")]
            A.b_KT = [Buf("KT0"), Buf("KT1")]
            A.b_V = [Buf("V0"), Buf("V1")]
            A.b_sel = [Buf("sel0"), Buf("sel1")]
            A.b_kmean = [Buf("km0"), Buf("km1")]
            A.b_tab = Buf("tab")
            phase_A("qkv", A)
            for lst in (A.b_QT, A.b_KT, A.b_V, A.b_sel, A.b_kmean, [A.b_tab]):
                for b_ in lst:
                    b_.last_w, b_.readers = None, []
            phase_att(A)
        phase_B()
    return nc


def _slabs(W):
    K, N = W.shape
    return np.ascontiguousarray(W.reshape(K // 128, 128, N // 128, 128).transpose(2, 1, 0, 3).reshape(N // 128, 128, (K // 128) * 128))


def _const_tables():
    ct = np.zeros((128, NCT), np.float32)
    q = np.arange(128, dtype=np.float32)[:, None]
    for j in range(2):
        i = np.arange(32, dtype=np.float32)[None, :]
        ct[:, C_D + j * 32:C_D + (j + 1) * 32] = 256.0 * (31 - i) + 128.0 * j + q - 255.0
    ct[:, C_D + 31] = 0.0
    ct[:, C_D + 63] = 0.0
    for kc in range(2):
        ct[:, C_KB + kc] = 128.0 * kc + q[:, 0] - 255.0
    p = np.arange(128, dtype=np.float32)[:, None]
    qq = np.arange(128, dtype=np.float32)[None, :]
    ct[:, C_DD:C_DD + 128] = np.where(p <= qq, p - qq, -1.0e9)
    ct[:, C_DF:C_DF + 128] = p - qq - 128.0
    return ct


def make_in_maps(inp):
    f32 = lambda a: np.ascontiguousarray(np.asarray(a, dtype=np.float32))
    x = f32(inp["x"])[0]
    xT = np.ascontiguousarray(x.T)
    w_in = f32(inp["w_in"])[0]
    ctab = _const_tables()
    shared = dict(
        xT=xT, ctab=ctab,
        w_g=_slabs(w_in[:, 10240:14336]),
        w_pa=_slabs(f32(inp["w_proj_attn"])[0]),
        w_pl=_slabs(f32(inp["w_proj_lru"])[0]),
        w_o=_slabs(f32(inp["w_out"])[0]),
        w_fg=_slabs(f32(inp["w_ffn_gate"])[0]),
        w_fu=_slabs(f32(inp["w_ffn_up"])[0]),
    )
    wfd = f32(inp["w_ffn_down"])[0]
    shared["w_fd"] = np.ascontiguousarray(
        wfd.reshape(FG, FGC, 128, KD, 128).transpose(0, 3, 2, 1, 4).reshape(FG * KD, 128, FGC * 128))
    n1, n2 = f32(inp["norm1_w"])[0], f32(inp["norm2_w"])[0]
    cw, cb = f32(inp["conv_w"])[0], f32(inp["conv_b"])[0]
    ba, bx, lam = f32(inp["b_rg_a"])[0], f32(inp["b_rg_x"])[0], f32(inp["lru_lambda"])[0]
    qw, kw = f32(inp["q_norm_w"])[0], f32(inp["k_norm_w"])[0]
    wra, wrx = f32(inp["w_rg_a"])[0], f32(inp["w_rg_x"])[0]
    maps = []
    for c in range(NC):
        cols = np.concatenate([np.arange(256) + base + 256 * c for base in (6144, 8192, 2048, 4096, 0)])
        w_a = np.ascontiguousarray(w_in[:, cols].reshape(KD, 128, 1280).transpose(1, 0, 2))
        vecs = np.zeros((128, NV), np.float32)
        vecs[:, V_N1:V_N1 + KD] = n1.reshape(KD, 128).T
        vecs[:, V_N2:V_N2 + KD] = n2.reshape(KD, 128).T
        for ch in range(2):
            sl = slice(256 * c + 128 * ch, 256 * c + 128 * ch + 128)
            for tap in range(4):
                vecs[:, V_CW + ch * 4 + tap] = cw[tap, sl]
            vecs[:, V_CB + ch] = cb[sl]
            vecs[:, V_BA + ch] = ba[sl]
            vecs[:, V_BX + ch] = bx[sl]
            vecs[:, V_LAM + ch] = lam[sl]
            vecs[:, V_HI + ch] = float(2 * c + ch + 1)
        vecs[:, V_QW] = qw
        vecs[:, V_KW] = kw
        w_rg = np.zeros((128, 2, 2, 128), np.float32)
        for ch in range(2):
            w_rg[:, 0, ch, :] = wra[2 * c + ch]
            w_rg[:, 1, ch, :] = wrx[2 * c + ch]
        m = dict(shared)
        m.update(w_a=w_a, vecs=vecs, w_rg=w_rg, xTs=np.ascontiguousarray(xT[:, c * TB:(c + 1) * TB]),
                 cidx=np.array([[c]], np.int32))
        maps.append(m)
    return maps


_NC_CACHE = {}


def kernel(**inputs):
    if "nc" not in _NC_CACHE:
        _NC_CACHE["nc"] = build_program(debug=True)
    nc = _NC_CACHE["nc"]
    in_maps = make_in_maps(inputs)
    res = run_bass_kernel_spmd(nc, in_maps, core_ids=list(range(NC)))
    out = np.empty((1, S, D), np.float32)
    for c in range(NC):
        out[0, c * TB:(c + 1) * TB, :] = res.results[c]["outT"].T
    return out
```
